# Optimizing a Trainium2 kernel written in Bass

```python
import jax, jax.numpy as jnp
from jax import lax
import numpy as np

D_MODEL = 1024
BATCH = 16
SEQ = 2048
DEPTH = 4

HEAD_DIM = 64
ATT_WIDTH = D_MODEL // 2
ML_WIDTH = D_MODEL // 4
GM_WIDTH = D_MODEL // 4
MIX_WIDTH = ATT_WIDTH + ML_WIDTH + GM_WIDTH
N_ATT_HEADS = ATT_WIDTH // HEAD_DIM
N_KV_HEADS = max(1, N_ATT_HEADS // 4)
KV_WIDTH = N_KV_HEADS * HEAD_DIM
N_ML_HEADS = ML_WIDTH // HEAD_DIM
N_GM_GROUPS = GM_WIDTH // HEAD_DIM
WINDOW = 128
ATT_BLOCK = 128
N_REL_BUCKETS = 32
REL_MAX_DIST = 128
ML_CHUNK = 128
ML_CONV = 3
N_ML_GATES = 4 * N_ML_HEADS
GM_CHUNK = 128
N_EXPERTS = 16
EC_CAPACITY_FACTOR = 2
D_EXPERT = 2 * D_MODEL
DN_ALPHA = (2 * DEPTH) ** 0.25
DN_BETA = (8 * DEPTH) ** -0.25
LN_EPS = 1e-5
NEG_INF = -1e30
PROJ_SPLITS = (ATT_WIDTH, KV_WIDTH, KV_WIDTH, ML_WIDTH, ML_WIDTH, ML_WIDTH, ML_WIDTH, N_ML_GATES, GM_WIDTH, GM_WIDTH)
PROJ_WIDTH = ATT_WIDTH + 2 * KV_WIDTH + 4 * ML_WIDTH + N_ML_GATES + 2 * GM_WIDTH

kernel_name = "hybrid_parallel_groups_ec_moe_encoder"


def layer_norm(x, g=None, b=None):
    xf = x.astype(jnp.float32)
    mu = xf.mean(-1, keepdims=True)
    var = jnp.mean(jnp.square(xf - mu), -1, keepdims=True)
    y = (xf - mu) * lax.rsqrt(var + LN_EPS)
    if g is not None:
        y = y * g.astype(jnp.float32) + b.astype(jnp.float32)
    return y.astype(x.dtype)


def t5_buckets(rel):
    nb = N_REL_BUCKETS // 2
    max_exact = nb // 2
    ret = np.where(rel > 0, nb, 0)
    n = np.abs(rel)
    large = max_exact + (np.log(np.maximum(n, 1) / max_exact) / np.log(REL_MAX_DIST / max_exact) * (nb - max_exact)).astype(np.int32)
    large = np.minimum(large, nb - 1)
    return (ret + np.where(n < max_exact, n, large)).astype(np.int32)


def windowed_gqa(q, k, v, sink, rel_bias):
    B, S, Hq, dh = q.shape
    Hkv = k.shape[2]
    G = Hq // Hkv
    nb = S // ATT_BLOCK
    band = 3 * ATT_BLOCK

    def to_band(t):
        tp = jnp.pad(t, ((0, 0), (ATT_BLOCK, ATT_BLOCK), (0, 0), (0, 0)))
        blocks = tp.reshape(B, nb + 2, ATT_BLOCK, Hkv, dh)
        return jnp.concatenate([blocks[:, :-2], blocks[:, 1:-1], blocks[:, 2:]], axis=2)

    kb, vb = to_band(k), to_band(v)
    qb = q.reshape(B, nb, ATT_BLOCK, Hkv, G, dh)
    logits = jnp.einsum('bnqhgd,bnshd->bhgnqs', qb, kb, preferred_element_type=jnp.float32) * (dh ** -0.5)
    qi = np.arange(ATT_BLOCK)[:, None]
    sj = np.arange(band)[None, :]
    rel = sj - ATT_BLOCK - qi
    bias = rel_bias[t5_buckets(rel)].astype(jnp.float32)
    bias = bias.transpose(2, 0, 1).reshape(Hkv, G, 1, ATT_BLOCK, band)
    kpos = np.arange(nb)[:, None] * ATT_BLOCK - ATT_BLOCK + sj
    mask = (np.abs(rel) <= WINDOW)[None] & ((kpos >= 0) & (kpos < S))[:, None, :]
    logits = jnp.where(mask, logits + bias, NEG_INF)
    sink_col = jnp.broadcast_to(sink.astype(jnp.float32).reshape(Hkv, G, 1, 1, 1), logits.shape[:-1] + (1,))
    probs = jax.nn.softmax(jnp.concatenate([logits, sink_col], axis=-1), axis=-1)[..., :-1]
    out = jnp.einsum('bhgnqs,bnshd->bnqhgd', probs.astype(vb.dtype), vb)
    return out.reshape(B, S, Hq * dh)


def short_conv(t, w, b):
    C = t.shape[-1]
    y = lax.conv_general_dilated(t, w[:, None, :], window_strides=(1,), padding=[(ML_CONV // 2, ML_CONV // 2)],
                                 dimension_numbers=('NWC', 'WIO', 'NWC'), feature_group_count=C)
    return y + b


def mlstm_direction(q, k, v, ig, lf):
    B, H, S, dh = q.shape
    nc = S // ML_CHUNK

    def chunks(t):
        return jnp.moveaxis(t.reshape((B, H, nc, ML_CHUNK) + t.shape[3:]), 2, 0)

    tril = jnp.tril(jnp.ones((ML_CHUNK, ML_CHUNK), dtype=bool))

    def step(carry, inp):
        C, n, m = carry
        qc, kc, vc, ic, fc = inp
        b = jnp.cumsum(fc, axis=-1)
        dmat = jnp.where(tril, b[..., :, None] - b[..., None, :] + ic[..., None, :], -jnp.inf)
        inter = b + m[..., None]
        m_t = jnp.maximum(inter, dmat.max(-1))
        s = jnp.einsum('bhtd,bhsd->bhts', qc, kc) * jnp.exp(dmat - m_t[..., None])
        iw = jnp.exp(inter - m_t)
        num = jnp.einsum('bhts,bhsd->bhtd', s, vc) + iw[..., None] * jnp.einsum('bhed,bhtd->bhte', C, qc)
        den = s.sum(-1) + iw * jnp.einsum('bhd,bhtd->bht', n, qc)
        h = num / jnp.maximum(jnp.abs(den), jnp.exp(-m_t))[..., None]
        bL = b[..., -1]
        ws = bL[..., None] - b + ic
        m_new = jnp.maximum(bL + m, ws.max(-1))
        wexp = jnp.exp(ws - m_new[..., None])
        decay = jnp.exp(bL + m - m_new)
        C_new = decay[..., None, None] * C + jnp.einsum('bhs,bhse,bhsd->bhed', wexp, vc, kc)
        n_new = decay[..., None] * n + jnp.einsum('bhs,bhsd->bhd', wexp, kc)
        return (C_new, n_new, m_new), h

    init = (jnp.zeros((B, H, dh, dh), jnp.float32), jnp.zeros((B, H, dh), jnp.float32), jnp.zeros((B, H), jnp.float32))
    _, h = lax.scan(step, init, (chunks(q), chunks(k), chunks(v), chunks(ig), chunks(lf)))
    return jnp.moveaxis(h, 0, 2).reshape(B, H, S, dh)


def mlstm_bidir(mq, mk, mv, mo, gates):
    B, S, _ = mq.shape
    H = N_ML_HEADS

    def heads(t):
        return t.astype(jnp.float32).reshape(B, S, H, HEAD_DIM).transpose(0, 2, 1, 3)

    q, k, v = heads(mq), heads(mk) * (HEAD_DIM ** -0.5), heads(mv)
    g = gates.astype(jnp.float32).reshape(B, S, 4, H).transpose(2, 0, 3, 1)
    flip = lambda t: jnp.flip(t, axis=2)
    h_f = mlstm_direction(q, k, v, g[0], jax.nn.log_sigmoid(g[1]))
    h_b = flip(mlstm_direction(flip(q), flip(k), flip(v), flip(g[2]), flip(jax.nn.log_sigmoid(g[3]))))
    h = (h_f + h_b).transpose(0, 2, 1, 3).reshape(B, S, ML_WIDTH)
    return jax.nn.sigmoid(mo) * h.astype(mo.dtype)


def spatial_gating(u, v, w_s, b_s):
    B, S, _ = u.shape
    u = jax.nn.gelu(u, approximate=False)
    v = layer_norm(jax.nn.gelu(v, approximate=False))
    nc = S // GM_CHUNK
    vb = v.reshape(B, nc, GM_CHUNK, N_GM_GROUPS, HEAD_DIM)
    s = jnp.einsum('gts,bnsgc->bntgc', w_s, vb) + b_s.T[:, :, None]
    return u * s.reshape(B, S, GM_WIDTH)


def expert_choice_ffn(h, w_router, w_gate, w_up, w_down):
    B, S, D = h.shape
    cap = max(1, min(S, EC_CAPACITY_FACTOR * S // N_EXPERTS))
    aff = jax.nn.softmax(jnp.einsum('bsd,de->bse', h, w_router, preferred_element_type=jnp.float32), axis=-1)
    gate, idx = lax.top_k(aff.transpose(0, 2, 1), cap)
    xg = jax.vmap(lambda hb, ib: hb[ib])(h, idx)
    hid = jax.nn.silu(jnp.einsum('becd,edf->becf', xg, w_gate)) * jnp.einsum('becd,edf->becf', xg, w_up)
    ye = jnp.einsum('becf,efd->becd', hid, w_down) * gate.astype(h.dtype)[..., None]
    scatter = lambda yb, ib: jnp.zeros((S, D), yb.dtype).at[ib.reshape(-1)].add(yb.reshape(-1, D))
    return jax.vmap(scatter)(ye, idx)


def hybrid_layer(x, c, w_ada, b_ada, w_in, conv_w, conv_b, gate_b, sink, rel_bias, w_s, b_s, w_out,
                 w_router, w_gate, w_up, w_down, ln_g, ln_b):
    B, S, D = x.shape
    mod = jax.nn.silu(c) @ w_ada + b_ada
    sh1, sc1, g1, sh2, sc2, g2 = [m[:, None, :] for m in jnp.split(mod, 6, axis=-1)]
    h = layer_norm(x) * (1 + sc1) + sh1
    proj = h @ w_in
    aq, ak, av, mq, mk, mv, mo, mg, gu, gv = jnp.split(proj, np.cumsum(PROJ_SPLITS)[:-1].tolist(), axis=-1)
    att = windowed_gqa(aq.reshape(B, S, N_ATT_HEADS, HEAD_DIM), ak.reshape(B, S, N_KV_HEADS, HEAD_DIM),
                       av.reshape(B, S, N_KV_HEADS, HEAD_DIM), sink, rel_bias)
    mqk = jax.nn.silu(short_conv(jnp.concatenate([mq, mk], axis=-1), conv_w, conv_b))
    mq, mk = jnp.split(mqk, 2, axis=-1)
    ml = mlstm_bidir(mq, mk, mv, mo, mg + gate_b)
    gm = spatial_gating(gu, gv, w_s, b_s)
    mix = jnp.concatenate([att, ml, gm], axis=-1) @ w_out
    x = layer_norm(DN_ALPHA * x + (1 + g1) * mix, ln_g[0], ln_b[0])
    h2 = layer_norm(x) * (1 + sc2) + sh2
    y = expert_choice_ffn(h2, w_router, w_gate, w_up, w_down)
    return layer_norm(DN_ALPHA * x + (1 + g2) * y, ln_g[1], ln_b[1])


def setup_inputs(seed: int = 0) -> dict:
    key = jax.random.key(seed)
    ks = jax.random.split(key, 20)
    nrm = lambda k, shape, scale: jax.random.normal(k, shape, jnp.float32) * scale
    D = D_MODEL
    lin = jnp.linspace(3.0, 6.0, N_ML_HEADS, dtype=jnp.float32)
    zer = jnp.zeros((N_ML_HEADS,), jnp.float32)
    gate_base = jnp.concatenate([zer, lin, zer, lin])
    return {
        'x': nrm(ks[0], (BATCH, SEQ, D), 1.0),
        'c': nrm(ks[1], (BATCH, D), 1.0),
        'w_ada': nrm(ks[2], (DEPTH, D, 6 * D), 0.2 * D ** -0.5),
        'b_ada': nrm(ks[3], (DEPTH, 6 * D), 0.02),
        'w_in': nrm(ks[4], (DEPTH, D, PROJ_WIDTH), D ** -0.5),
        'conv_w': nrm(ks[5], (DEPTH, ML_CONV, 2 * ML_WIDTH), ML_CONV ** -0.5),
        'conv_b': nrm(ks[6], (DEPTH, 2 * ML_WIDTH), 0.02),
        'gate_b': gate_base + nrm(ks[7], (DEPTH, N_ML_GATES), 0.1),
        'sink': nrm(ks[8], (DEPTH, N_ATT_HEADS), 0.5),
        'rel_bias': nrm(ks[9], (N_REL_BUCKETS, N_ATT_HEADS), 0.1),
        'w_s': nrm(ks[10], (DEPTH, N_GM_GROUPS, GM_CHUNK, GM_CHUNK), GM_CHUNK ** -0.5),
        'b_s': 1.0 + nrm(ks[11], (DEPTH, N_GM_GROUPS, GM_CHUNK), 0.02),
        'w_out': nrm(ks[12], (DEPTH, MIX_WIDTH, D), DN_BETA * MIX_WIDTH ** -0.5),
        'w_router': nrm(ks[13], (DEPTH, D, N_EXPERTS), D ** -0.5),
        'w_gate': nrm(ks[14], (DEPTH, N_EXPERTS, D, D_EXPERT), D ** -0.5),
        'w_up': nrm(ks[15], (DEPTH, N_EXPERTS, D, D_EXPERT), D ** -0.5),
        'w_down': nrm(ks[16], (DEPTH, N_EXPERTS, D_EXPERT, D), DN_BETA * D_EXPERT ** -0.5),
        'ln_g': 1.0 + nrm(ks[17], (DEPTH, 2, D), 0.02),
        'ln_b': nrm(ks[18], (DEPTH, 2, D), 0.02),
    }


def reference(x, c, w_ada, b_ada, w_in, conv_w, conv_b, gate_b, sink, rel_bias, w_s, b_s, w_out,
              w_router, w_gate, w_up, w_down, ln_g, ln_b):
    for l in range(DEPTH):
        x = hybrid_layer(x, c, w_ada[l], b_ada[l], w_in[l], conv_w[l], conv_b[l], gate_b[l], sink[l], rel_bias,
                         w_s[l], b_s[l], w_out[l], w_router[l], w_gate[l], w_up[l], w_down[l], ln_g[l], ln_b[l])
    return x
```

```python
import numpy as np
import concourse.bass as bass
import concourse.mybir as mybir
from concourse.bass_utils import run_bass_kernel_spmd
from contextlib import ExitStack

F32 = mybir.dt.float32
BF16 = mybir.dt.bfloat16
AF = mybir.ActivationFunctionType
ALU = mybir.AluOpType
AX = mybir.AxisListType

ENGS = ["pe", "act", "dve", "pool", "sp"]
N_DMA_SEMS = 24
D = 1024
S = 2048
NB = 16
DEPTH = 4
ALPHA = float((2 * DEPTH) ** 0.25)
EPS = 1e-5


class Res:
    __slots__ = ("name", "w", "rs", "excl")

    def __init__(self, name="", excl=False):
        self.name = name
        self.w = None
        self.rs = []
        self.excl = excl


class Op:
    __slots__ = ("eng", "key", "pos", "fn", "waits", "signal", "sigval", "is_dma")


class Prog:
    def __init__(self, nc):
        self.nc = nc
        self.ops = {e: [] for e in ENGS}
        self.waited = {e: {} for e in ENGS}
        self.cnt = {}
        self.dma_rr = 0
        self.dma_last = [None] * N_DMA_SEMS
        self.last = {e: None for e in ENGS}

    def _dep(self, o, d):
        if d is None or d is o:
            return
        E = o.eng
        if (not d.is_dma) and d.eng == E and E == "pe":
            return
        w = self.waited[E]
        if w.get(d.key, 0) >= d.pos:
            return
        w[d.key] = d.pos
        d.signal = True
        o.waits.append(d)

    def op(self, eng, fn, reads=(), writes=(), dma=False, extra_deps=()):
        o = Op()
        o.eng = eng
        o.fn = fn
        o.waits = []
        o.signal = False
        o.sigval = None
        o.is_dma = dma
        prev = None
        if dma:
            si = self.dma_rr
            self.dma_rr = (self.dma_rr + 1) % N_DMA_SEMS
            o.key = ("dma", si)
            o.signal = True
            prev = self.dma_last[si]
            self.dma_last[si] = o
        else:
            o.key = eng
        self.cnt[o.key] = self.cnt.get(o.key, 0) + 1
        o.pos = self.cnt[o.key]
        if prev is not None:
            self._dep(o, prev)
        for d in extra_deps:
            self._dep(o, d)
        for r in reads:
            self._dep(o, r.w)
            if r.excl:
                for rd in r.rs:
                    if rd.eng != eng:
                        self._dep(o, rd)
        for wr in writes:
            self._dep(o, wr.w)
            for rd in wr.rs:
                self._dep(o, rd)
        for wr in writes:
            wr.w = o
            wr.rs = []
        for r in reads:
            if r.w is not o:
                r.rs.append(o)
        self.ops[eng].append(o)
        if fn is not None and not dma:
            self.last[eng] = o
        return o

    def barrier(self):
        lasts = [self.last[e] for e in ENGS if self.last[e] is not None]
        dmas = [d for d in self.dma_last if d is not None]
        for e in ENGS:
            self.op(e, None, extra_deps=lasts + dmas)

    def emit(self):
        nc = self.nc
        with ExitStack() as es:
            sems = {}
            for e in ENGS:
                sems[e] = es.enter_context(nc.semaphore("s_" + e))
            for i in range(N_DMA_SEMS):
                sems[("dma", i)] = es.enter_context(nc.semaphore("s_dma%d" % i))
            for e in ENGS:
                c = 0
                for o in self.ops[e]:
                    if o.is_dma:
                        o.sigval = 16 * o.pos
                    elif o.signal:
                        c += 1
                        o.sigval = c
            import sys as _s
            print("SIGCOUNTS", {e: (len(self.ops[e]), max([o.sigval or 0 for o in self.ops[e] if not o.is_dma] + [0])) for e in ENGS},
                  "dma", max([o.sigval or 0 for e in ENGS for o in self.ops[e] if o.is_dma] + [0]), file=_s.stderr)
            block = es.enter_context(nc.Block())

            def run(ename):
                def body(eng):
                    for o in self.ops[ename]:
                        for d in o.waits:
                            eng.wait_ge(sems[d.key], d.sigval)
                        if o.fn is None:
                            continue
                        inst = o.fn(eng)
                        if o.signal:
                            inst.then_inc(sems[o.key], 16 if o.is_dma else 1)
                return body

            block.sync(run("sp"))
            block.tensor(run("pe"))
            block.scalar(run("act"))
            block.vector(run("dve"))
            block.gpsimd(run("pool"))


def t5_buckets(rel):
    nb = 16
    max_exact = 8
    ret = np.where(rel > 0, nb, 0)
    n = np.abs(rel)
    large = max_exact + (np.log(np.maximum(n, 1) / max_exact) / np.log(128 / max_exact) * (nb - max_exact)).astype(np.int32)
    large = np.minimum(large, nb - 1)
    return (ret + np.where(n < max_exact, n, large)).astype(np.int32)


C_ID, C_TRF, C_TRB, C_ONE, C_TRS, C_IOTA, C_MNEG, C_END = 0, 128, 256, 384, 512, 640, 896, 1280


def const_pack():
    c = np.zeros((128, C_END), np.float32)
    u = np.arange(128)[:, None]
    t = np.arange(128)[None, :]
    c[:, C_ID:C_ID + 128] = (u == t)
    c[:, C_TRF:C_TRF + 128] = (u <= t)
    c[:, C_TRB:C_TRB + 128] = (u >= t)
    c[:, C_ONE:C_ONE + 128] = 1.0
    c[:, C_TRS:C_TRS + 128] = (u < t)
    c[:, C_IOTA:C_IOTA + 256] = np.arange(256)[None, :]
    q = np.arange(128)[:, None]
    sj = np.arange(384)[None, :]
    rel = sj - 128 - q
    c[:, C_MNEG:C_MNEG + 384] = np.where(np.abs(rel) <= 128, 0.0, -1e30)
    return c


class Stop(Exception):
    pass


def build(n_layers=DEPTH, n_seq=2, dbg=(), stop=None):
    nc = bass.Bass("TRN2", target_bir_lowering=False)

    def din(name, shape):
        return nc.dram_tensor(name, list(shape), F32, kind="ExternalInput").ap()

    x_d = din("x", [2, S, D])
    cT_d = din("cT", [128, 8, 2])
    wada_d = din("w_ada", [DEPTH, D, 6 * D])
    bada_d = din("b_ada", [DEPTH, 6 * D])
    win_d = din("w_in", [DEPTH, D, 2320])
    cw_d = din("conv_wT", [DEPTH, 512, 3])
    cb_d = din("conv_b", [128, DEPTH, 4])
    gb_d = din("gate_b", [DEPTH, 16])
    sink_d = din("sink", [DEPTH, 8])
    btab_d = din("bias_tab", [128, 8, 384])
    ws_d = din("w_s", [DEPTH, 4, 128, 128])
    bsT_d = din("b_sT", [DEPTH, 128, 4])
    wout_d = din("w_out", [DEPTH, D, D])
    wr_d = din("w_router", [DEPTH, D, 16])
    wg_d = din("w_gate", [DEPTH, 16, D, 2 * D])
    wu_d = din("w_up", [DEPTH, 16, D, 2 * D])
    wd_d = din("w_down", [DEPTH, 16, 2 * D, D])
    lng_d = din("ln_g", [DEPTH, 2, D])
    lnb_d = din("ln_b", [DEPTH, 2, D])
    cst_d = din("cst", [128, C_END])
    out_d = nc.dram_tensor("out", [2, S, D], F32, kind="ExternalOutput").ap()
    xs_d = nc.dram_tensor("xs", [S, D], F32, kind="Internal").ap()
    modd = nc.dram_tensor("modd", [2, DEPTH * 6 * D], F32, kind="Internal").ap()
    dbg_d = {}
    for nm in dbg:
        dbg_d[nm] = nc.dram_tensor("dbg_" + nm, [S, D], F32, kind="ExternalOutput").ap()

    es = ExitStack()
    P = Prog(nc)

    def sb(name, shape, dt):
        return es.enter_context(nc.sbuf_tensor(name, list(shape), dt))

    XT = sb("XT", [128, NB, D], F32)
    AA = sb("AA", [128, 16384], BF16)
    BB = sb("BB", [128, 16384], BF16)
    WW = sb("WW", [128, 20480], BF16)
    MOD = [sb("MOD%d" % i, [128, D], F32) for i in range(3)]
    CST = sb("CST", [128, C_END], F32)
    IDB = sb("IDB", [128, 128], BF16)
    ONEB = sb("ONEB", [128, 128], BF16)
    SM = sb("SM", [128, 1024], F32)
    CWA = sb("CWA", [128, DEPTH, 4, 3], F32)
    CBA = sb("CBA", [128, DEPTH, 4], F32)
    BST = sb("BST", [128, DEPTH, 4], F32)
    GBB = sb("GBB", [128, DEPTH, 16], F32)
    SNK = sb("SNK", [128, DEPTH, 8], F32)
    AFF = sb("AFF", [128, NB, 16], F32)
    MSK = sb("MSK", [128, NB, 16], F32)
    GTM = sb("GTM", [128, NB, 16], F32)
    SLT = sb("SLT", [128, NB, 16], F32)
    TOT = sb("TOT", [128, NB, 16], F32)
    AFT = sb("AFT", [16, S], F32)
    WRT = sb("WRT", [128, 8, 16], F32)
    PS = [es.enter_context(nc.psum_tensor("ps%d" % i, [128, 512], F32)) for i in range(8)]
    PR = [Res("ps%d" % i, excl=True) for i in range(8)]
    pstate = {"i": 0}

    def bank():
        i = pstate["i"]
        pstate["i"] = (i + 1) % 8
        return PS[i], PR[i]

    def carve(arena, off, dt, pat=None, **kw):
        return arena

    ident_f = CST[:, C_ID:C_ID + 128]
    tri_f = CST[:, C_TRF:C_TRF + 128]
    tri_b = CST[:, C_TRB:C_TRB + 128]
    ones_f = CST[:, C_ONE:C_ONE + 128]
    tri_s = CST[:, C_TRS:C_TRS + 128]
    iota_f = CST[:, C_IOTA:C_IOTA + 256]
    mneg = CST[:, C_MNEG:C_MNEG + 384]

    sm_off = {"i": 0}
    smr = {}

    def smslot(n):
        o = sm_off["i"]
        sm_off["i"] += n
        assert sm_off["i"] <= 1024
        return SM[:, o:o + n]

    EPSV = smslot(1)
    STAT = [smslot(12) for _ in range(4)]
    MVv = [smslot(2) for _ in range(4)]
    RSTD = [smslot(1) for _ in range(4)]
    NMR = [smslot(1) for _ in range(4)]
    r_stat = [Res("stat%d" % i) for i in range(4)]
    stat_i = {"i": 0}
    r_cst = Res("cst")
    r_mod = [Res("mod%d" % i) for i in range(3)]
    r_x = [Res("x%d" % b) for b in range(NB)]
    r_modd = Res("modd")
    r_xs = Res("xs")

    def mm(out, lhsT, rhs, start, stop, R, W):
        return P.op("pe", lambda e: e.matmul(out, lhsT=lhsT, rhs=rhs, start=start, stop=stop), R, W)

    def tr(out, in_, ident, R, W):
        return P.op("pe", lambda e: e.transpose(out=out, in_=in_, identity=ident), R, W)

    def act(out, in_, func, R, W, bias=None, scale=None, accum_out=None):
        kw = {}
        if bias is not None:
            kw["bias"] = bias
        if scale is not None:
            kw["scale"] = scale
        if accum_out is not None:
            kw["accum_out"] = accum_out
        return P.op("act", lambda e: e.activation(out=out, in_=in_, func=func, **kw), R, W)

    def ts(eng, out, in0, s1, s2, op0, op1, R, W):
        if op1 is None:
            return P.op(eng, lambda e: e.tensor_scalar(out=out, in0=in0, scalar1=s1, scalar2=None, op0=op0), R, W)
        return P.op(eng, lambda e: e.tensor_scalar(out=out, in0=in0, scalar1=s1, scalar2=s2, op0=op0, op1=op1), R, W)

    def tt(eng, out, in0, in1, op, R, W):
        return P.op(eng, lambda e: e.tensor_tensor(out=out, in0=in0, in1=in1, op=op), R, W)

    def stt(out, in0, scalar, in1, op0, op1, R, W):
        return P.op("dve", lambda e: e.scalar_tensor_tensor(out=out, in0=in0, scalar=scalar, in1=in1, op0=op0, op1=op1), R, W)

    def cp(eng, out, in_, R, W):
        if eng == "act":
            return act(out, in_, AF.Copy, R, W)
        return P.op(eng, lambda e: e.tensor_copy(out=out, in_=in_), R, W)

    def dma(q, out, in_, R, W):
        return P.op(q, lambda e: e.dma_start(out=out, in_=in_), R, W, dma=True)

    def ln_stats(xap, Rx, width=1024):
        i = stat_i["i"]
        stat_i["i"] = (i + 1) % 4
        rs = r_stat[i]
        nchunk = width // 512 if width >= 512 else 1
        cw = width // nchunk
        for j in range(nchunk):
            P.op("dve", lambda e, j=j: e.bn_stats(out=STAT[i][:, j * 6:(j + 1) * 6], in_=xap[:, j * cw:(j + 1) * cw]), [Rx], [rs])
        P.op("dve", lambda e: e.bn_aggr(out=MVv[i], in_=STAT[i][:, 0:6 * nchunk]), [rs], [rs])
        act(RSTD[i], MVv[i][:, 1:2], AF.Sqrt, [rs, r_cst], [rs], bias=EPSV, scale=1.0)
        P.op("dve", lambda e: e.reciprocal(out=RSTD[i], in_=RSTD[i]), [rs], [rs])
        stt(NMR[i], MVv[i][:, 0:1], -1.0, RSTD[i], ALU.mult, ALU.mult, [rs], [rs])
        return RSTD[i], NMR[i], rs

    def load_mod(slot, seq, l, which):
        off = l * 6 * D + which * D
        return dma("sp", MOD[slot][:], modd[seq:seq + 1, off:off + D].broadcast_to([128, D]), [r_modd], [r_mod[slot]])

    def load_row(slot, row_ap):
        return dma("sp", MOD[slot][:], row_ap.broadcast_to([128, D]), [], [r_mod[slot]])

    dma("sp", CST[:], cst_d, [], [r_cst])
    P.op("dve", lambda e: e.memset(EPSV, EPS), [], [r_cst])
    cp("dve", IDB[:], ident_f, [r_cst], [r_cst])
    cp("dve", ONEB[:], ones_f, [r_cst], [r_cst])
    r_small = Res("small")
    dma("act", CWA[:], cw_d.rearrange("l (f p) j -> p l f j", p=128), [], [r_small])
    dma("act", CBA[:], cb_d, [], [r_small])
    dma("act", BST[:], bsT_d.rearrange("l p g -> p l g"), [], [r_small])
    dma("act", GBB[:].rearrange("p l g -> p (l g)"), gb_d.rearrange("l g -> (l g)").unsqueeze(0).broadcast_to([128, DEPTH * 16]), [], [r_small])
    dma("act", SNK[:].rearrange("p l g -> p (l g)"), sink_d.rearrange("l g -> (l g)").unsqueeze(0).broadcast_to([128, DEPTH * 8]), [], [r_small])

    CT = sb("CTs", [128, 8, 2], F32)
    CTB = sb("CTB", [128, 8, 2], BF16)
    r_ct = Res("ct")
    dma("sp", CT[:], cT_d, [], [r_ct])
    act(CTB[:], CT[:], AF.Silu, [r_ct], [r_ct])
    WAD = [WW[:, i * 4096:(i + 1) * 4096].rearrange("p (k n) -> p k n", k=8) for i in range(4)]
    r_wad = [Res("wad%d" % i) for i in range(4)]
    MROW = XT[0:2, 0, :]
    BROW = XT[0:2, 1, 0:512]
    r_mrow = Res("mrow")
    r_brow = Res("brow")
    wi = 0
    for l in range(n_layers):
        for j in range(12):
            w = WAD[wi % 4]
            rw = r_wad[wi % 4]
            wi += 1
            dma("pool", w, wada_d[l, :, j * 512:(j + 1) * 512].rearrange("(k p) n -> p k n", p=128), [], [rw])
            pb, pr = bank()
            for k in range(8):
                mm(pb[0:2, :], CTB[:, k, :], w[:, k, :], k == 0, k == 7, [r_ct, rw], [pr])
            dma("sp", BROW, bada_d[l:l + 1, j * 512:(j + 1) * 512].broadcast_to([2, 512]), [], [r_brow])
            plus1 = 1.0 if (j // 2) in (1, 2, 4, 5) else 0.0
            stt(MROW[:, (j % 2) * 512:(j % 2) * 512 + 512], pb[0:2, :], plus1, BROW, ALU.add, ALU.add, [pr, r_brow], [r_mrow])
            if j % 2 == 1:
                dma("sp", modd[:, l * 6 * D + (j // 2) * D: l * 6 * D + (j // 2 + 1) * D], MROW, [r_mrow], [r_modd])
    P.barrier()
    def chk(name):
        if stop == name:
            raise Stop()

    HT = AA[:].rearrange("p (k t) -> p k t", k=8)
    H2B = AA[:].rearrange("p (b d) -> p b d", b=NB)
    MIX = BB[:].rearrange("p (b d) -> p b d", b=NB)
    r_ht = [Res("ht%d" % b) for b in range(NB)]
    r_mix_a = [Res("mixa%d" % b) for b in range(NB)]
    r_mix_m = [Res("mixm%d" % b) for b in range(NB)]
    r_mix_g = [Res("mixg%d" % b) for b in range(NB)]
    XTB = XT[:].bitcast(BF16).rearrange("p b d -> p (b d)")
    XTF = XT[:].rearrange("p b d -> p (b d)")

    LNX = [WW[:, 14336 + i * 2048: 14336 + (i + 1) * 2048].bitcast(F32) for i in range(2)]
    LNH = [WW[:, 18432 + i * 1024: 18432 + (i + 1) * 1024] for i in range(2)]
    r_lnx = [Res("lnx0"), Res("lnx1")]
    r_lnh = [Res("lnh0"), Res("lnh1")]
    WR = [WW[:, i * 4224:(i + 1) * 4224].rearrange("p (k n) -> p k n", k=8) for i in range(2)]
    r_wr = [Res("wr0"), Res("wr1")]

    def x_src(seq, l):
        src = x_d[seq] if l == 0 else xs_d
        return src.rearrange("(b p) d -> p b d", p=128), ([] if l == 0 else [r_xs])

    def mixer(seq, l):
        xsrc, xsr = x_src(seq, l)
        load_mod(0, seq, l, 1)
        load_mod(1, seq, l, 0)
        for b in range(NB):
            j = b % 2
            dma("sp", LNX[j], xsrc[:, b, :], xsr, [r_lnx[j]])
            rstd, nmr, rs = ln_stats(LNX[j], r_lnx[j])
            act(LNX[j], LNX[j], AF.Identity, [rs, r_lnx[j]], [r_lnx[j]], bias=nmr, scale=rstd)
            tt("dve", LNX[j], LNX[j], MOD[0][:], ALU.mult, [r_lnx[j], r_mod[0]], [r_lnx[j]])
            tt("pool", LNH[j], LNX[j], MOD[1][:], ALU.add, [r_lnx[j], r_mod[1]], [r_lnh[j]])
            pb, pr = bank()
            pbb = pb[:].bitcast(BF16)
            for k in range(8):
                tr(pbb[:, k * 128:(k + 1) * 128], LNH[j][:, k * 128:(k + 1) * 128], IDB[:], [r_lnh[j], r_cst], [pr])
            cp("act", HT[:, :, b * 128:(b + 1) * 128], pbb.rearrange("p (k t) -> p k t", k=8), [pr], [r_ht[b]])

        chk("A")

        def load_piece(slot, c0, c1):
            return dma("pool", WR[slot][:, :, 0:c1 - c0], win_d[l, :, c0:c1].rearrange("(k p) n -> p k n", p=128), [], [r_wr[slot]])

        def proj_fm(slot, lhs_fn, evac):
            for tq in range(4):
                pb, pr = bank()
                for k in range(8):
                    mm(pb[:, :], lhs_fn(k), HT[:, k, tq * 512:(tq + 1) * 512], k == 0, k == 7,
                       [r_wr[slot]] + r_ht[tq * 4:(tq + 1) * 4], [pr])
                evac(tq, pb, pr)

        def proj_tm(slot, b, c0, n, pb, pr):
            for k in range(8):
                mm(pb[:, 0:n], HT[:, k, b * 128:(b + 1) * 128], WR[slot][:, k, c0:c0 + n], k == 0, k == 7, [r_wr[slot], r_ht[b]], [pr])

        AQT = XTB[:, 0:8192].rearrange("p (c t) -> p c t", c=4)
        AKT = XTB[:, 8192:10240]
        AV1 = XTB[:, 10240:12320].rearrange("p (b g e) -> p b g e", b=NB, g=2)
        BIASM = XTF[:, 6400:9472].rearrange("p (h s) -> p h s", h=8)
        S1 = [XTF[:, 9472 + i * 384: 9472 + (i + 1) * 384] for i in range(2)]
        PBF = [XTB[:, 20480 + i * 384: 20480 + (i + 1) * 384] for i in range(2)]
        PTB = [XTB[:, 21248 + i * 384: 21248 + (i + 1) * 384] for i in range(2)]
        AVEC = XTF[:, 11008:11072]
        r_aq = [Res("aq%d" % i) for i in range(4)]
        r_ak = [Res("ak%d" % i) for i in range(4)]
        r_av = [Res("av%d" % b) for b in range(NB)]
        r_bm = Res("biasm")
        r_s1 = [Res("s1a"), Res("s1b")]
        r_pb = [Res("pba"), Res("pbb")]
        r_pt = [Res("pta"), Res("ptb")]
        r_avec = [Res("avec%d" % i) for i in range(4)]
        dma("act", BIASM, btab_d, [], [r_bm])
        for h in range(8):
            tt("pool", BIASM[:, h, :], BIASM[:, h, :], mneg, ALU.add, [r_bm, r_cst], [r_bm])
        P.op("pool", lambda e: e.memset(AV1[:, :, :, 64:65], 1.0), [], r_av)
        for g_ in range(2):
            for c_ in range(4):
                dma("pool", WR[0][:, :, c_ * 128 + g_ * 64: c_ * 128 + (g_ + 1) * 64],
                    win_d[l, :, (g_ * 4 + c_) * 64:(g_ * 4 + c_ + 1) * 64].rearrange("(k p) e -> p k e", p=128), [], [r_wr[0]])
        load_piece(1, 512, 768)
        for c in range(4):
            def ev(tq, pb, pr, c=c):
                act(AQT[:, c, tq * 512:(tq + 1) * 512], pb[:, :], AF.Copy, [pr], [r_aq[tq]], scale=0.125)
            proj_fm(0, lambda k, c=c: WR[0][:, k, c * 128:(c + 1) * 128], ev)

        def evk(tq, pb, pr):
            cp("dve", AKT[:, tq * 512:(tq + 1) * 512], pb[:, :], [pr], [r_ak[tq]])
        proj_fm(1, lambda k: WR[1][:, k, 0:128], evk)
        for b in range(NB):
            pb, pr = bank()
            proj_tm(1, b, 128, 128, pb, pr)
            cp("dve", AV1[:, b, :, 0:64], pb[:, 0:128].rearrange("p (g e) -> p g e", g=2), [pr], [r_av[b]])
        u = 0
        for n in range(NB):
            lo, hi = max(n - 1, 0), min(n + 1, NB - 1)
            nblk = hi - lo + 1
            ncol = nblk * 128
            j0 = (lo - (n - 1)) * 128
            for h in range(8):
                g, c = h // 4, h % 4
                i2 = u % 2
                i4 = u % 4
                u += 1
                vec = AVEC[:, i4 * 8:(i4 + 1) * 8]
                ps, prs = bank()
                mm(ps[:, 0:ncol], AQT[g * 64:(g + 1) * 64, c, n * 128:(n + 1) * 128], AKT[g * 64:(g + 1) * 64, lo * 128:(hi + 1) * 128],
                   True, True, [r_aq[n // 4]] + [r_ak[bb // 4] for bb in range(lo, hi + 1)], [prs])
                tt("dve", S1[i2][:, 0:ncol], ps[:, 0:ncol], BIASM[:, h, j0:j0 + ncol], ALU.add, [prs, r_bm], [r_s1[i2]])
                P.op("dve", lambda e, i2=i2, ncol=ncol, vec=vec: e.tensor_reduce(out=vec[:, 0:1], in_=S1[i2][:, 0:ncol], axis=AX.X, op=ALU.max),
                     [r_s1[i2]], [r_avec[i4]])
                ts("dve", vec[:, 1:2], vec[:, 0:1], SNK[:, l, h:h + 1], -1.0, ALU.max, ALU.mult, [r_avec[i4], r_small], [r_avec[i4]])
                act(PBF[i2][:, 0:ncol], S1[i2][:, 0:ncol], AF.Exp, [r_s1[i2], r_avec[i4]], [r_pb[i2]], bias=vec[:, 1:2], scale=1.0)
                act(vec[:, 2:3], SNK[:, l, h:h + 1], AF.Exp, [r_avec[i4], r_small], [r_avec[i4]], bias=vec[:, 1:2], scale=1.0)
                pt, prt = bank()
                ptb = pt[:].bitcast(BF16)
                for jb in range(nblk):
                    tr(ptb[:, jb * 128:(jb + 1) * 128], PBF[i2][:, jb * 128:(jb + 1) * 128], IDB[:], [r_pb[i2], r_cst], [prt])
                cp("act", PTB[i2][:, 0:ncol], ptb[:, 0:ncol], [prt], [r_pt[i2]])
                po, pro = bank()
                for jb in range(nblk):
                    mm(po[:, 0:65], PTB[i2][:, jb * 128:(jb + 1) * 128], AV1[:, lo + jb, g, :], jb == 0, jb == nblk - 1,
                       [r_pt[i2], r_av[lo + jb]], [pro])
                tt("dve", vec[:, 3:4], po[:, 64:65], vec[:, 2:3], ALU.add, [pro, r_avec[i4]], [r_avec[i4]])
                P.op("dve", lambda e, vec=vec: e.reciprocal(out=vec[:, 4:5], in_=vec[:, 3:4]), [r_avec[i4]], [r_avec[i4]])
                ts("dve", MIX[:, n, h * 64:(h + 1) * 64], po[:, 0:64], vec[:, 4:5], None, ALU.mult, None, [pro, r_avec[i4]], [r_mix_a[n]])
        P.barrier()
        chk("att")

        PRE = XTB[:, 0:8200].rearrange("p (c t) -> p c t", c=4)
        CQ = XTB[:, 8200:16392].rearrange("p (c t) -> p c t", c=4)
        KTOK = XTB[:, 16392:20488].rearrange("p (b d) -> p b d", b=NB)
        MV1 = XTB[:, 20488:24648].rearrange("p (b h e) -> p b h e", b=NB, h=4)
        MO = XTB[:, 24648:28744].rearrange("p (b d) -> p b d", b=NB)
        fo = 14400

        def falloc(n):
            nonlocal fo
            a = XTF[:, fo:fo + n]
            fo += n
            assert fo <= 16384
            return a
        GT = falloc(256).rearrange("p (b g) -> p b g", b=NB)
        LF = falloc(128).rearrange("p (d b h) -> p d b h", d=2, b=NB)
        EA = falloc(128).rearrange("p (d b h) -> p d b h", d=2, b=NB)
        EB = falloc(128).rearrange("p (d b h) -> p d b h", d=2, b=NB)
        EBL = falloc(128).rearrange("p (d b h) -> p d b h", d=2, b=NB)
        TMPG = falloc(128).rearrange("p (d b h) -> p d b h", d=2, b=NB)
        C32 = falloc(8 * 65).rearrange("p (c e) -> p c e", c=8)
        CTMP = falloc(2 * 65).rearrange("p (c e) -> p c e", c=2)
        ND = [falloc(65) for _ in range(4)]
        MVEC = falloc(32)
        HTMP = [falloc(64) for _ in range(2)]
        bo = 8448

        def balloc(n):
            nonlocal bo
            a = WW[:, bo:bo + n]
            bo += n
            assert bo <= 14336
            return a
        CONVT = [balloc(1024).bitcast(F32) for _ in range(2)]
        STM = [balloc(128) for _ in range(4)]
        VS2 = [balloc(130) for _ in range(2)]
        CBF = balloc(8 * 66).rearrange("p (c e) -> p c e", c=8)[:, :, 0:65]
        r_pre = [Res("pre%d" % i) for i in range(4)]
        r_cq = [[Res("cq%d_%d" % (f, i)) for i in range(4)] for f in range(4)]
        r_ktok = [Res("ktok%d" % b) for b in range(NB)]
        r_mv = [Res("mv%d" % b) for b in range(NB)]
        r_mo = [Res("mo%d" % b) for b in range(NB)]
        r_gt = Res("gt")
        r_gates = Res("gates")
        r_convt = [Res("cva"), Res("cvb")]
        r_stm = [Res("stm%d" % i) for i in range(4)]
        r_vs = [Res("vs%d" % i) for i in range(4)]
        r_c = [Res("c%d" % i) for i in range(8)]
        r_ctmp = [Res("ctmp0"), Res("ctmp1")]
        r_nd = [Res("nd%d" % i) for i in range(4)]
        r_mvec = [Res("mvec%d" % i) for i in range(4)]
        r_htmp = [Res("htmp0"), Res("htmp1")]
        load_piece(0, 768, 1280)
        load_piece(1, 1280, 1808)
        P.op("pool", lambda e: e.memset(PRE[:, :, 0:1], 0.0), [], r_pre)
        P.op("pool", lambda e: e.memset(PRE[:, :, 2049:2050], 0.0), [], r_pre)
        P.op("pool", lambda e: e.memset(MV1[:, :, :, 64:65], 1.0), [], r_mv)
        P.op("pool", lambda e: e.memset(C32[:, :, :], 0.0), [], r_c)
        P.op("pool", lambda e: e.memset(CBF, 0.0), [], r_c)
        for fc in range(4):
            def ev(tq, pb, pr, fc=fc):
                cp("act", PRE[:, fc, 1 + tq * 512: 1 + (tq + 1) * 512], pb[:, :], [pr], [r_pre[tq]])
            proj_fm(0, lambda k, fc=fc: WR[0][:, k, fc * 128:(fc + 1) * 128], ev)
        ci = 0
        for fc in range(4):
            for tq in range(4):
                t0 = tq * 512
                cv = CONVT[ci % 2]
                rc = r_convt[ci % 2]
                ci += 1
                rd = [r_pre[i] for i in range(max(tq - 1, 0), min(tq + 1, 3) + 1)] + [r_small]
                ts("dve", cv, PRE[:, fc, t0:t0 + 512], CWA[:, l, fc, 0:1], None, ALU.mult, None, rd, [rc])
                stt(cv, PRE[:, fc, t0 + 1:t0 + 513], CWA[:, l, fc, 1:2], cv, ALU.mult, ALU.add, rd + [rc], [rc])
                stt(cv, PRE[:, fc, t0 + 2:t0 + 514], CWA[:, l, fc, 2:3], cv, ALU.mult, ALU.add, rd + [rc], [rc])
                act(CQ[:, fc, t0:t0 + 512], cv, AF.Silu, [rc, r_small], [r_cq[fc][tq]], bias=CBA[:, l, fc:fc + 1], scale=1.0)
                if fc >= 2:
                    ts("pool", CQ[:, fc, t0:t0 + 512], CQ[:, fc, t0:t0 + 512], 0.125, None, ALU.mult, None, [r_cq[fc][tq]], [r_cq[fc][tq]])
        chk("m1")
        for b in range(NB):
            pb, pr = bank()
            pbb = pb[:].bitcast(BF16)
            for kc in range(2):
                tr(pbb[:, kc * 128:(kc + 1) * 128], CQ[:, 2 + kc, b * 128:(b + 1) * 128], IDB[:], [r_cq[2 + kc][b // 4], r_cst], [pr])
            cp("dve", KTOK[:, b, :], pbb[:, 0:256], [pr], [r_ktok[b]])
        chk("m1b")
        for b in range(NB):
            pb, pr = bank()
            proj_tm(1, b, 0, 512, pb, pr)
            pb2, pr2 = bank()
            proj_tm(1, b, 496, 32, pb2, pr2)
            cp("dve", MV1[:, b, :, 0:64], pb[:, 0:256].rearrange("p (h e) -> p h e", h=4), [pr], [r_mv[b]])
            sgt_ = CONVT[b % 2][:, 0:256]
            act(sgt_, pb[:, 256:512], AF.Exp, [pr], [r_convt[b % 2]], scale=-1.0)
            ts("dve", sgt_, sgt_, 1.0, None, ALU.add, None, [r_convt[b % 2]], [r_convt[b % 2]])
            P.op("dve", lambda e, sgt_=sgt_: e.reciprocal(out=sgt_, in_=sgt_), [r_convt[b % 2]], [r_convt[b % 2]])
            cp("dve", MO[:, b, :], sgt_, [r_convt[b % 2]], [r_mo[b]])
            tt("dve", GT[:, b, :], pb2[:, 16:32], GBB[:, l, :], ALU.add, [pr2, r_small], [r_gt])
        chk("m2")
        for d in range(2):
            act(TMPG[:, d], GT[:, :, (2 * d + 1) * 4:(2 * d + 2) * 4], AF.Exp, [r_gt], [r_gates], scale=-1.0)
            act(LF[:, d], TMPG[:, d], AF.Ln, [r_gates], [r_gates], bias=1.0, scale=1.0)
        for d in range(2):
            pb, pr = bank()
            mm(pb[:, 0:64], tri_f if d == 0 else tri_b, LF[:, d].rearrange("p b h -> p (b h)"), True, True, [r_gates, r_cst], [pr])
            pb2, pr2 = bank()
            mm(pb2[:, 0:64], ones_f, LF[:, d].rearrange("p b h -> p (b h)"), True, True, [r_gates, r_cst], [pr2])
            tt("dve", TMPG[:, d], pb[:, 0:64].rearrange("p (b h) -> p b h", b=NB), GT[:, :, (2 * d) * 4:(2 * d + 1) * 4], ALU.add, [pr, r_gt], [r_gates])
            act(EA[:, d], TMPG[:, d], AF.Exp, [r_gates], [r_gates])
            act(EB[:, d], pb[:, 0:64].rearrange("p (b h) -> p b h", b=NB), AF.Exp, [pr], [r_gates], scale=-1.0)
            act(EBL[:, d], pb2[:, 0:64].rearrange("p (b h) -> p b h", b=NB), AF.Exp, [pr2], [r_gates], scale=-1.0)
        chk("m3")
        uu = 0
        for ci_ in range(NB):
            for d in range(2):
                c = ci_ if d == 0 else NB - 1 - ci_
                msk = tri_f if d == 0 else tri_b
                for h in range(4):
                    kc, pbs = h // 2, (h % 2) * 64
                    ch = d * 4 + h
                    i4 = uu % 4
                    i2 = uu % 2
                    uu += 1
                    QT = CQ[pbs:pbs + 64, kc, c * 128:(c + 1) * 128]
                    KT = CQ[pbs:pbs + 64, 2 + kc, c * 128:(c + 1) * 128]
                    rq = [r_cq[kc][c // 4], r_cq[2 + kc][c // 4]]
                    ps, prs = bank()
                    mm(ps[:, 0:128], KT, QT, True, True, rq, [prs])
                    stt(STM[i4], ps[:, 0:128], EA[:, d, c, h:h + 1], msk, ALU.mult, ALU.mult, [prs, r_gates, r_cst], [r_stm[i4]])
                    pn, prn = bank()
                    mm(pn[:, 0:65], STM[i4], MV1[:, c, h, :], True, False, [r_stm[i4], r_mv[c]], [prn])
                    mm(pn[:, 0:65], CQ[:, kc, c * 128:(c + 1) * 128], CBF[:, ch, :], False, True, rq + [r_c[ch]], [prn])
                    ts("dve", ND[i4], pn[:, 0:65], EB[:, d, c, h:h + 1], None, ALU.mult, None, [prn, r_gates], [r_nd[i4]])
                    mv = MVEC[:, i4 * 8:(i4 + 1) * 8]
                    ts("dve", mv[:, 0:1], ND[i4][:, 64:65], -1.0, None, ALU.mult, None, [r_nd[i4]], [r_mvec[i4]])
                    ts("dve", mv[:, 0:1], mv[:, 0:1], ND[i4][:, 64:65], 1.0, ALU.max, ALU.max, [r_nd[i4], r_mvec[i4]], [r_mvec[i4]])
                    P.op("dve", lambda e, mv=mv: e.reciprocal(out=mv[:, 1:2], in_=mv[:, 0:1]), [r_mvec[i4]], [r_mvec[i4]])
                    dst = MIX[:, c, 512 + h * 64: 512 + (h + 1) * 64]
                    if (d == 0) == (c <= 7):
                        ts("dve", dst, ND[i4][:, 0:64], mv[:, 1:2], None, ALU.mult, None, [r_nd[i4], r_mvec[i4]], [r_mix_m[c]])
                    else:
                        stt(HTMP[i2], ND[i4][:, 0:64], mv[:, 1:2], dst, ALU.mult, ALU.add, [r_nd[i4], r_mvec[i4], r_mix_m[c]], [r_htmp[i2]])
                        tt("dve", dst, HTMP[i2], MO[:, c, h * 64:(h + 1) * 64], ALU.mult, [r_htmp[i2], r_mo[c]], [r_mix_m[c]])
                if ci_ < NB - 1:
                    for kc in range(2):
                        i2 = uu % 2
                        uu += 1
                        for hh in range(2):
                            h = kc * 2 + hh
                            ts("pool", VS2[i2][:, hh * 65:(hh + 1) * 65], MV1[:, c, h, :], EA[:, d, c, h:h + 1], None, ALU.mult, None, [r_mv[c], r_gates], [r_vs[i2]])
                        pu, pru = bank()
                        mm(pu[:, 0:130], KTOK[:, c, kc * 128:(kc + 1) * 128], VS2[i2], True, True, [r_ktok[c], r_vs[i2]], [pru])
                        for hh in range(2):
                            h = kc * 2 + hh
                            ch = d * 4 + h
                            pbs = hh * 64
                            tt("dve", CTMP[pbs:pbs + 64, i2, :], C32[pbs:pbs + 64, ch, :], pu[pbs:pbs + 64, hh * 65:(hh + 1) * 65], ALU.add, [r_c[ch], pru], [r_ctmp[i2]])
                            ts("dve", C32[pbs:pbs + 64, ch, :], CTMP[pbs:pbs + 64, i2, :], EBL[pbs:pbs + 64, d, c, h:h + 1], None, ALU.mult, None,
                               [r_ctmp[i2], r_gates], [r_c[ch]])
                            cp("act", CBF[pbs:pbs + 64, ch, :], C32[pbs:pbs + 64, ch, :], [r_c[ch]], [r_c[ch]])
        P.barrier()
        chk("mls")

        WSF = XTF[:, 0:512].rearrange("p (g s) -> p g s", g=4)
        WSB = XTB[:, 1024:1536].rearrange("p (g s) -> p g s", g=4)
        WST = XTB[:, 1536:2048].rearrange("p (g s) -> p g s", g=4)
        GV = [XTF[:, 1024 + i * 256: 1024 + (i + 1) * 256] for i in range(2)]
        VB = [XTB[:, 3072 + i * 256: 3072 + (i + 1) * 256] for i in range(2)]
        r_ws = Res("ws")
        r_gv = [Res("gv0"), Res("gv1")]
        r_vb = [Res("vb0"), Res("vb1")]
        load_piece(0, 1808, 2320)
        dma("act", WSF, ws_d[l].rearrange("g t s -> t g s"), [], [r_ws])
        cp("dve", WSB, WSF, [r_ws], [r_ws])
        pb, pr = bank()
        pbb = pb[:].bitcast(BF16)
        for g in range(4):
            tr(pbb[:, g * 128:(g + 1) * 128], WSB[:, g, :], IDB[:], [r_ws, r_cst], [pr])
        cp("dve", WST.rearrange("p g s -> p (g s)"), pbb[:, 0:512], [pr], [r_ws])
        for b in range(NB):
            j = b % 2
            pb, pr = bank()
            proj_tm(0, b, 0, 512, pb, pr)
            act(MIX[:, b, 768:1024], pb[:, 0:256], AF.Gelu, [pr], [r_mix_g[b]])
            act(GV[j], pb[:, 256:512], AF.Gelu, [pr], [r_gv[j]])
            rstd, nmr, rs = ln_stats(GV[j], r_gv[j], width=256)
            act(VB[j], GV[j], AF.Identity, [rs, r_gv[j]], [r_vb[j]], bias=nmr, scale=rstd)
            pg, prg = bank()
            for g in range(4):
                mm(pg[:, g * 64:(g + 1) * 64], WST[:, g, :], VB[j][:, g * 64:(g + 1) * 64], True, True, [r_ws, r_vb[j]], [prg])
            for g in range(4):
                dst = MIX[:, b, 768 + g * 64: 768 + (g + 1) * 64]
                stt(dst, pg[:, g * 64:(g + 1) * 64], BST[:, l, g:g + 1], dst, ALU.add, ALU.mult, [prg, r_small, r_mix_g[b]], [r_mix_g[b]])
        P.barrier()
        chk("gml")

        WO = WW[:, 0:8192].rearrange("p (k n) -> p k n", k=8)
        r_wo = Res("wo")
        MTB = [AA[:, i * 1024:(i + 1) * 1024].rearrange("p (k t) -> p k t", k=8) for i in range(2)]
        TMPF = [AA[:, 2048 + i * 1024: 2048 + (i + 1) * 1024].bitcast(F32) for i in range(2)]
        r_mtb = [Res("mtb0"), Res("mtb1")]
        r_tmpf = [Res("tf0"), Res("tf1")]
        dma("pool", WO, wout_d[l].rearrange("(k p) n -> p k n", p=128), [], [r_wo])
        load_mod(0, seq, l, 2)
        load_row(1, lng_d[l, 0:1, :])
        load_row(2, lnb_d[l, 0:1, :])
        for b in range(NB):
            j = b % 2
            dma("sp", XT[:, b, :], xsrc[:, b, :], xsr, [r_x[b]])
            pb, pr = bank()
            pbb = pb[:].bitcast(BF16)
            for k in range(8):
                tr(pbb[:, k * 128:(k + 1) * 128], MIX[:, b, k * 128:(k + 1) * 128], IDB[:], [r_mix_a[b], r_mix_m[b], r_mix_g[b], r_cst], [pr])
            cp("act", MTB[j].rearrange("p k t -> p (k t)"), pbb, [pr], [r_mtb[j]])
            for dh in range(2):
                po, pro = bank()
                for k in range(8):
                    mm(po[:, :], MTB[j][:, k, :], WO[:, k, dh * 512:(dh + 1) * 512], k == 0, k == 7, [r_mtb[j], r_wo], [pro])
                tt("dve", TMPF[dh], po[:, :], MOD[0][:, dh * 512:(dh + 1) * 512], ALU.mult, [pro, r_mod[0]], [r_tmpf[dh]])
                stt(XT[:, b, dh * 512:(dh + 1) * 512], XT[:, b, dh * 512:(dh + 1) * 512], ALPHA, TMPF[dh], ALU.mult, ALU.add,
                    [r_tmpf[dh], r_x[b]], [r_x[b]])
            rstd, nmr, rs = ln_stats(XT[:, b, :], r_x[b])
            act(XT[:, b, :], XT[:, b, :], AF.Identity, [rs, r_x[b]], [r_x[b]], bias=nmr, scale=rstd)
            tt("dve", XT[:, b, :], XT[:, b, :], MOD[1][:], ALU.mult, [r_x[b], r_mod[1]], [r_x[b]])
            tt("pool", XT[:, b, :], XT[:, b, :], MOD[2][:], ALU.add, [r_x[b], r_mod[2]], [r_x[b]])
        P.barrier()

    def moe(seq, l, last):
        load_mod(0, seq, l, 4)
        load_mod(1, seq, l, 3)
        r_wrt = Res("wrt")
        dma("act", WRT[:], wr_d[l].rearrange("(k p) e -> p k e", p=128), [], [r_wrt])
        r_h2b = [Res("h2b%d" % b) for b in range(NB)]
        r_aff = Res("aff")
        H2T = WW[:, 0:2048].bitcast(F32).rearrange("p (k t) -> p k t", k=8)
        r_h2t = Res("h2t")
        LVEC = SM[:, 200:264]
        r_lvec = [Res("lvec%d" % i) for i in range(4)]
        LG = [SM[:, 264 + i * 16: 264 + (i + 1) * 16] for i in range(4)]
        r_lg = [Res("lg%d" % i) for i in range(4)]
        for b in range(NB):
            j = b % 2
            i4 = b % 4
            rstd, nmr, rs = ln_stats(XT[:, b, :], r_x[b])
            act(LNX[j], XT[:, b, :], AF.Identity, [rs, r_x[b]], [r_lnx[j]], bias=nmr, scale=rstd)
            tt("dve", LNX[j], LNX[j], MOD[0][:], ALU.mult, [r_lnx[j], r_mod[0]], [r_lnx[j]])
            tt("pool", LNX[j], LNX[j], MOD[1][:], ALU.add, [r_lnx[j], r_mod[1]], [r_lnx[j]])
            cp("act", H2B[:, b, :], LNX[j], [r_lnx[j]], [r_h2b[b]])
            ts("pool", XT[:, b, :], XT[:, b, :], ALPHA, None, ALU.mult, None, [r_x[b]], [r_x[b]])
            for half in range(2):
                pb, pr = bank()
                for kk in range(4):
                    k = half * 4 + kk
                    tr(pb[:, kk * 128:(kk + 1) * 128], LNX[j][:, k * 128:(k + 1) * 128], ident_f, [r_lnx[j], r_cst], [pr])
                cp("dve", H2T[:, half * 4:(half + 1) * 4, :].rearrange("p k t -> p (k t)"), pb[:, :], [pr], [r_h2t])
            pl, prl = bank()
            for k in range(8):
                mm(pl[:, 0:16], H2T[:, k, :], WRT[:, k, :], k == 0, k == 7, [r_h2t, r_wrt], [prl])
            vec = LVEC[:, i4 * 8:(i4 + 1) * 8]
            P.op("dve", lambda e, vec=vec, pl=pl: e.tensor_reduce(out=vec[:, 0:1], in_=pl[:, 0:16], axis=AX.X, op=ALU.max, negate=True), [prl], [r_lvec[i4]])
            act(LG[i4], pl[:, 0:16], AF.Exp, [prl, r_lvec[i4]], [r_lg[i4]], bias=vec[:, 0:1], scale=1.0)
            P.op("dve", lambda e, vec=vec, i4=i4: e.tensor_reduce(out=vec[:, 1:2], in_=LG[i4], axis=AX.X, op=ALU.add), [r_lg[i4]], [r_lvec[i4]])
            P.op("dve", lambda e, vec=vec: e.reciprocal(out=vec[:, 2:3], in_=vec[:, 1:2]), [r_lvec[i4]], [r_lvec[i4]])
            ts("dve", AFF[:, b, :], LG[i4], vec[:, 2:3], None, ALU.mult, None, [r_lg[i4], r_lvec[i4]], [r_aff])
        chk("E")
        r_aft = Res("aft")
        for q4 in range(4):
            pb, pr = bank()
            for bb in range(4):
                b = q4 * 4 + bb
                tr(pb[0:16, bb * 128:(bb + 1) * 128], AFF[:, b, :], ident_f, [r_aff, r_cst], [pr])
            cp("dve", AFT[:, q4 * 512:(q4 + 1) * 512], pb[0:16, :], [pr], [r_aft])
        M8 = SM[0:16, 400:408]
        r_m8 = Res("m8")
        WORK = WW[0:16, 4096:8192].bitcast(F32)
        r_work = Res("work")
        for r in range(32):
            src = AFT[:, :] if r == 0 else WORK
            P.op("dve", lambda e, src=src: e.max(out=M8, in_=src), [r_aft, r_work], [r_m8])
            if r < 31:
                P.op("dve", lambda e, src=src: e.match_replace(out=WORK, in_to_replace=M8, in_values=src, imm_value=-1.0), [r_aft, r_m8, r_work], [r_work])
        ts("dve", WORK, AFT[:, :], M8[:, 7:8], None, ALU.is_ge, None, [r_aft, r_m8, r_work], [r_work])
        r_sel_meta = Res("selmeta")
        for q4 in range(4):
            pb, pr = bank()
            for bb in range(4):
                b = q4 * 4 + bb
                tr(pb[:, bb * 16:(bb + 1) * 16], WORK[:, b * 128:(b + 1) * 128], ident_f[0:16, 0:16], [r_work, r_cst], [pr])
            cp("dve", MSK[:, q4 * 4:(q4 + 1) * 4, :].rearrange("p b e -> p (b e)"), pb[:, 0:64], [pr], [r_sel_meta])
        tt("dve", GTM[:].rearrange("p b e -> p (b e)"), MSK[:].rearrange("p b e -> p (b e)"), AFF[:].rearrange("p b e -> p (b e)"), ALU.mult,
           [r_sel_meta, r_aff], [r_sel_meta])
        pp, prp = bank()
        mm(pp[:, 0:256], tri_s, MSK[:].rearrange("p b e -> p (b e)"), True, True, [r_sel_meta, r_cst], [prp])
        pq, prq = bank()
        mm(pq[:, 0:256], ones_f, MSK[:].rearrange("p b e -> p (b e)"), True, True, [r_sel_meta, r_cst], [prq])
        cp("dve", TOT[:].rearrange("p b e -> p (b e)"), pq[:, 0:256], [prq], [r_sel_meta])
        CAR = SLT
        P.op("dve", lambda e: e.memset(CAR[:, 0, :], 0.0), [], [r_sel_meta])
        for b in range(1, NB):
            tt("dve", CAR[:, b, :], CAR[:, b - 1, :], TOT[:, b - 1, :], ALU.add, [r_sel_meta], [r_sel_meta])
        tt("dve", SLT[:].rearrange("p b e -> p (b e)"), SLT[:].rearrange("p b e -> p (b e)"), pp[:, 0:256], ALU.add, [r_sel_meta, prp], [r_sel_meta])
        P.barrier()
        chk("F")
        load_mod(0, seq, l, 5)
        SEL = BB[:, 0:4096].rearrange("p (b j) -> p b j", b=NB)
        SELT = BB[:, 4096:8192].rearrange("p (c t) -> p c t", c=2)
        XGT = BB[:, 8192:10240].rearrange("p (k j) -> p k j", k=8)
        HID = BB[:, 10240:14336].rearrange("p (f j) -> p f j", f=16)
        YE = BB[:, 14336:16384].rearrange("p (c d) -> p c d", c=2)
        WSL = [WW[:, i * 4096:(i + 1) * 4096] for i in range(4)]
        SGT = [WW[:, 16384 + i * 512: 16384 + (i + 1) * 512].bitcast(F32) for i in range(2)]
        r_wsl = [Res("wsl%d" % i) for i in range(4)]
        r_sgt = [Res("sgt0"), Res("sgt1")]
        r_sel = Res("sel")
        r_selt = Res("selt")
        r_xgt = Res("xgt")
        r_hid = [Res("hid%d" % i) for i in range(16)]
        r_ye = Res("ye")
        wsi = {"i": 0}

        def wslice(src_ap, pat, **kw):
            i = wsi["i"] % 4
            wsi["i"] += 1
            v = WSL[i].rearrange(pat, **kw)
            dma("pool", v, src_ap, [], [r_wsl[i]])
            return v, r_wsl[i]

        for e_ in range(16):
            for b in range(NB):
                ts("pool" if b % 2 else "dve", SEL[:, b, :], iota_f, SLT[:, b, e_:e_ + 1], MSK[:, b, e_:e_ + 1],
                   ALU.is_equal, ALU.mult, [r_sel_meta, r_cst], [r_sel])
            for kp in range(4):
                pb, pr = bank()
                for kk in range(2):
                    k = kp * 2 + kk
                    for b in range(NB):
                        mm(pb[:, kk * 256:(kk + 1) * 256], H2B[:, b, k * 128:(k + 1) * 128], SEL[:, b, :], b == 0, b == NB - 1, [r_h2b[b], r_sel], [pr])
                cp("act" if kp % 2 else "dve", XGT[:, kp * 2:(kp + 1) * 2, :].rearrange("p k j -> p (k j)"), pb[:, :], [pr], [r_xgt])
            for c in range(2):
                for half in range(2):
                    pb, pr = bank()
                    pbb = pb[:].bitcast(BF16)
                    for bb in range(8):
                        b = half * 8 + bb
                        tr(pbb[:, bb * 128:(bb + 1) * 128], SEL[:, b, c * 128:(c + 1) * 128], IDB[:], [r_sel, r_cst], [pr])
                    cp("act", SELT[:, c, half * 1024:(half + 1) * 1024], pbb, [pr], [r_selt])
            for js in range(4):
                wgv, rwg = wslice(wg_d[l, e_, :, js * 512:(js + 1) * 512].rearrange("(k p) n -> p k n", p=128), "p (k n) -> p k n", k=8)
                wuv, rwu = wslice(wu_d[l, e_, :, js * 512:(js + 1) * 512].rearrange("(k p) n -> p k n", p=128), "p (k n) -> p k n", k=8)
                for fl in range(4):
                    fc = js * 4 + fl
                    pb, pr = bank()
                    for k in range(8):
                        mm(pb[:, 0:256], wgv[:, k, fl * 128:(fl + 1) * 128], XGT[:, k, :], k == 0, k == 7, [rwg, r_xgt], [pr])
                    for k in range(8):
                        mm(pb[:, 256:512], wuv[:, k, fl * 128:(fl + 1) * 128], XGT[:, k, :], k == 0, k == 7, [rwu, r_xgt], [pr])
                    j2 = fc % 2
                    act(SGT[j2], pb[:, 0:256], AF.Silu, [pr], [r_sgt[j2]])
                    tt("dve", HID[:, fc, :], SGT[j2], pb[:, 256:512], ALU.mult, [r_sgt[j2], pr], [r_hid[fc]])
            yb = [bank() for _ in range(4)]
            for js in range(4):
                wdv, rwd = wslice(wd_d[l, e_, js * 512:(js + 1) * 512, :].rearrange("(f p) n -> p f n", p=128), "p (f n) -> p f n", f=4)
                for fl in range(4):
                    fc = js * 4 + fl
                    for c in range(2):
                        for dh in range(2):
                            pb, pr = yb[c * 2 + dh]
                            mm(pb[:, :], HID[:, fc, c * 128:(c + 1) * 128], wdv[:, fl, dh * 512:(dh + 1) * 512], fc == 0, fc == 15, [r_hid[fc], rwd], [pr])
            for c in range(2):
                for dh in range(2):
                    pb, pr = yb[c * 2 + dh]
                    tt("dve", YE[:, c, dh * 512:(dh + 1) * 512], pb[:, :], MOD[0][:, dh * 512:(dh + 1) * 512], ALU.mult, [pr, r_mod[0]], [r_ye])
            for b in range(NB):
                for dh in range(2):
                    pb, pr = bank()
                    for c in range(2):
                        mm(pb[:, :], SELT[:, c, b * 128:(b + 1) * 128], YE[:, c, dh * 512:(dh + 1) * 512], c == 0, c == 1, [r_selt, r_ye], [pr])
                    stt(XT[:, b, dh * 512:(dh + 1) * 512], pb[:, :], GTM[:, b, e_:e_ + 1], XT[:, b, dh * 512:(dh + 1) * 512], ALU.mult, ALU.add,
                        [pr, r_sel_meta, r_x[b]], [r_x[b]])
        load_row(1, lng_d[l, 1:2, :])
        load_row(2, lnb_d[l, 1:2, :])
        outs = []
        dst = (out_d[seq] if last else xs_d).rearrange("(b p) d -> p b d", p=128)
        for b in range(NB):
            rstd, nmr, rs = ln_stats(XT[:, b, :], r_x[b])
            act(XT[:, b, :], XT[:, b, :], AF.Identity, [rs, r_x[b]], [r_x[b]], bias=nmr, scale=rstd)
            tt("dve", XT[:, b, :], XT[:, b, :], MOD[1][:], ALU.mult, [r_x[b], r_mod[1]], [r_x[b]])
            tt("pool", XT[:, b, :], XT[:, b, :], MOD[2][:], ALU.add, [r_x[b], r_mod[2]], [r_x[b]])
            outs.append(dma("sp", dst[:, b, :], XT[:, b, :], [r_x[b]], [] if last else [r_xs]))
        P.barrier()
        return outs

    final = []
    try:
        chk("pro")
        for seq in range(n_seq):
            for l in range(n_layers):
                mixer(seq, l)
                if "x1" in dbg_d and seq == 0 and l == 0:
                    for b in range(NB):
                        final.append(dma("sp", dbg_d["x1"].rearrange("(b p) d -> p b d", p=128)[:, b, :], XT[:, b, :], [r_x[b]], []))
                    P.barrier()
                chk("D")
                final += moe(seq, l, l == n_layers - 1)
    except Stop:
        P.barrier()
    P.op("sp", None, extra_deps=final)
    P.emit()
    es.close()
    return nc


_CACHE = {}


def host_inputs(inp, core):
    f = lambda a: np.ascontiguousarray(np.asarray(a, dtype=np.float32))
    sl = slice(2 * core, 2 * core + 2)
    q = np.arange(128)[:, None]
    sj = np.arange(384)[None, :]
    bk = t5_buckets(sj - 128 - q)
    btab = np.asarray(inp["rel_bias"], np.float32)[bk]
    m = {
        "x": f(inp["x"][sl]),
        "cT": f(np.asarray(inp["c"])[sl].T.reshape(8, 128, 2).transpose(1, 0, 2)),
        "w_ada": f(inp["w_ada"]), "b_ada": f(inp["b_ada"]), "w_in": f(inp["w_in"]),
        "conv_wT": f(np.asarray(inp["conv_w"]).transpose(0, 2, 1)), "conv_b": f(np.asarray(inp["conv_b"]).reshape(DEPTH, 4, 128).transpose(2, 0, 1)),
        "gate_b": f(inp["gate_b"]), "sink": f(inp["sink"]),
        "bias_tab": f(btab.transpose(0, 2, 1)),
        "w_s": f(inp["w_s"]), "b_sT": f(np.asarray(inp["b_s"]).transpose(0, 2, 1)),
        "w_out": f(inp["w_out"]), "w_router": f(inp["w_router"]),
        "w_gate": f(inp["w_gate"]), "w_up": f(inp["w_up"]), "w_down": f(inp["w_down"]),
        "ln_g": f(inp["ln_g"]), "ln_b": f(inp["ln_b"]),
        "cst": const_pack(),
    }
    return m


def kernel(**inputs):
    if "nc" not in _CACHE:
        _CACHE["nc"] = build()
    nc = _CACHE["nc"]
    shared = host_inputs(inputs, 0)
    in_maps = []
    for core in range(8):
        m = dict(shared)
        sl = slice(2 * core, 2 * core + 2)
        m["x"] = np.ascontiguousarray(np.asarray(inputs["x"], np.float32)[sl])
        m["cT"] = np.ascontiguousarray(np.asarray(inputs["c"], np.float32)[sl].T.reshape(8, 128, 2).transpose(1, 0, 2))
        in_maps.append(m)
    res = run_bass_kernel_spmd(nc, in_maps, core_ids=list(range(8)))
    return np.concatenate([r["out"] for r in res.results], axis=0).astype(np.float32)
```

```python
import numpy as np
import concourse.bass as bass
import concourse.mybir as mybir
from concourse.bass_utils import run_bass_kernel_spmd
from contextlib import ExitStack

F32 = mybir.dt.float32
BF16 = mybir.dt.bfloat16
AF = mybir.ActivationFunctionType
ALU = mybir.AluOpType
AX = mybir.AxisListType

ENGS = ["pe", "act", "dve", "pool", "sp"]
N_DMA_SEMS = 24
D = 1024
S = 2048
NB = 16
DEPTH = 4
ALPHA = float((2 * DEPTH) ** 0.25)
EPS = 1e-5


class Res:
    __slots__ = ("name", "w", "rs", "excl")

    def __init__(self, name="", excl=False):
        self.name = name
        self.w = None
        self.rs = []
        self.excl = excl


class Op:
    __slots__ = ("eng", "key", "pos", "fn", "waits", "signal", "sigval", "is_dma")


class Prog:
    def __init__(self, nc):
        self.nc = nc
        self.ops = {e: [] for e in ENGS}
        self.waited = {e: {} for e in ENGS}
        self.cnt = {}
        self.dma_rr = 0
        self.dma_last = [None] * N_DMA_SEMS
        self.last = {e: None for e in ENGS}

    def _dep(self, o, d):
        if d is None or d is o:
            return
        E = o.eng
        if (not d.is_dma) and d.eng == E and E == "pe":
            return
        w = self.waited[E]
        if w.get(d.key, 0) >= d.pos:
            return
        w[d.key] = d.pos
        d.signal = True
        o.waits.append(d)

    def op(self, eng, fn, reads=(), writes=(), dma=False, extra_deps=()):
        o = Op()
        o.eng = eng
        o.fn = fn
        o.waits = []
        o.signal = False
        o.sigval = None
        o.is_dma = dma
        prev = None
        if dma:
            si = self.dma_rr
            self.dma_rr = (self.dma_rr + 1) % N_DMA_SEMS
            o.key = ("dma", si)
            o.signal = True
            prev = self.dma_last[si]
            self.dma_last[si] = o
        else:
            o.key = eng
        self.cnt[o.key] = self.cnt.get(o.key, 0) + 1
        o.pos = self.cnt[o.key]
        if prev is not None:
            self._dep(o, prev)
        for d in extra_deps:
            self._dep(o, d)
        for r in reads:
            self._dep(o, r.w)
            if r.excl:
                for rd in r.rs:
                    if rd.eng != eng:
                        self._dep(o, rd)
        for wr in writes:
            self._dep(o, wr.w)
            for rd in wr.rs:
                self._dep(o, rd)
        for wr in writes:
            wr.w = o
            wr.rs = []
        for r in reads:
            if r.w is not o:
                r.rs.append(o)
        self.ops[eng].append(o)
        if fn is not None and not dma:
            self.last[eng] = o
        return o

    def barrier(self):
        lasts = [self.last[e] for e in ENGS if self.last[e] is not None]
        dmas = [d for d in self.dma_last if d is not None]
        for e in ENGS:
            self.op(e, None, extra_deps=lasts + dmas)

    def emit(self):
        nc = self.nc
        with ExitStack() as es:
            sems = {}
            for e in ENGS:
                sems[e] = es.enter_context(nc.semaphore("s_" + e))
            for i in range(N_DMA_SEMS):
                sems[("dma", i)] = es.enter_context(nc.semaphore("s_dma%d" % i))
            for e in ENGS:
                c = 0
                for o in self.ops[e]:
                    if o.is_dma:
                        o.sigval = 16 * o.pos
                    elif o.signal:
                        c += 1
                        o.sigval = c
            import sys as _s
            print("SIGCOUNTS", {e: (len(self.ops[e]), max([o.sigval or 0 for o in self.ops[e] if not o.is_dma] + [0])) for e in ENGS},
                  "dma", max([o.sigval or 0 for e in ENGS for o in self.ops[e] if o.is_dma] + [0]), file=_s.stderr)
            block = es.enter_context(nc.Block())

            def run(ename):
                def body(eng):
                    for o in self.ops[ename]:
                        for d in o.waits:
                            eng.wait_ge(sems[d.key], d.sigval)
                        if o.fn is None:
                            continue
                        inst = o.fn(eng)
                        if o.signal:
                            inst.then_inc(sems[o.key], 16 if o.is_dma else 1)
                return body

            block.sync(run("sp"))
            block.tensor(run("pe"))
            block.scalar(run("act"))
            block.vector(run("dve"))
            block.gpsimd(run("pool"))


def t5_buckets(rel):
    nb = 16
    max_exact = 8
    ret = np.where(rel > 0, nb, 0)
    n = np.abs(rel)
    large = max_exact + (np.log(np.maximum(n, 1) / max_exact) / np.log(128 / max_exact) * (nb - max_exact)).astype(np.int32)
    large = np.minimum(large, nb - 1)
    return (ret + np.where(n < max_exact, n, large)).astype(np.int32)


C_ID, C_TRF, C_TRB, C_ONE, C_TRS, C_IOTA, C_MNEG, C_END = 0, 128, 256, 384, 512, 640, 896, 1280


def const_pack():
    c = np.zeros((128, C_END), np.float32)
    u = np.arange(128)[:, None]
    t = np.arange(128)[None, :]
    c[:, C_ID:C_ID + 128] = (u == t)
    c[:, C_TRF:C_TRF + 128] = (u <= t)
    c[:, C_TRB:C_TRB + 128] = (u >= t)
    c[:, C_ONE:C_ONE + 128] = 1.0
    c[:, C_TRS:C_TRS + 128] = (u < t)
    c[:, C_IOTA:C_IOTA + 256] = np.arange(256)[None, :]
    q = np.arange(128)[:, None]
    sj = np.arange(384)[None, :]
    rel = sj - 128 - q
    c[:, C_MNEG:C_MNEG + 384] = np.where(np.abs(rel) <= 128, 0.0, -1e30)
    return c


class Stop(Exception):
    pass


def build(n_layers=DEPTH, n_seq=2, dbg=(), stop=None):
    nc = bass.Bass("TRN2", target_bir_lowering=False)

    def din(name, shape):
        return nc.dram_tensor(name, list(shape), F32, kind="ExternalInput").ap()

    x_d = din("x", [2, S, D])
    cT_d = din("cT", [128, 8, 2])
    wada_d = din("w_ada", [DEPTH, D, 6 * D])
    bada_d = din("b_ada", [DEPTH, 6 * D])
    win_d = din("w_in", [DEPTH, D, 2320])
    cw_d = din("conv_wT", [DEPTH, 512, 3])
    cb_d = din("conv_b", [128, DEPTH, 4])
    gb_d = din("gate_b", [DEPTH, 16])
    sink_d = din("sink", [DEPTH, 8])
    btab_d = din("bias_tab", [128, 8, 384])
    ws_d = din("w_s", [DEPTH, 4, 128, 128])
    bsT_d = din("b_sT", [DEPTH, 128, 4])
    wout_d = din("w_out", [DEPTH, D, D])
    wr_d = din("w_router", [DEPTH, D, 16])
    wg_d = din("w_gate", [DEPTH, 16, D, 2 * D])
    wu_d = din("w_up", [DEPTH, 16, D, 2 * D])
    wd_d = din("w_down", [DEPTH, 16, 2 * D, D])
    lng_d = din("ln_g", [DEPTH, 2, D])
    lnb_d = din("ln_b", [DEPTH, 2, D])
    cst_d = din("cst", [128, C_END])
    out_d = nc.dram_tensor("out", [2, S, D], F32, kind="ExternalOutput").ap()
    xs_d = nc.dram_tensor("xs", [S, D], F32, kind="Internal").ap()
    modd = nc.dram_tensor("modd", [2, DEPTH * 6 * D], F32, kind="Internal").ap()
    dbg_d = {}
    for nm in dbg:
        dbg_d[nm] = nc.dram_tensor("dbg_" + nm, [S, D], F32, kind="ExternalOutput").ap()

    es = ExitStack()
    P = Prog(nc)

    def sb(name, shape, dt):
        return es.enter_context(nc.sbuf_tensor(name, list(shape), dt))

    XT = sb("XT", [128, NB, D], F32)
    AA = sb("AA", [128, 16384], BF16)
    BB = sb("BB", [128, 16384], BF16)
    WW = sb("WW", [128, 20480], BF16)
    MOD = [sb("MOD%d" % i, [128, D], F32) for i in range(3)]
    CST = sb("CST", [128, C_END], F32)
    IDB = sb("IDB", [128, 128], BF16)
    ONEB = sb("ONEB", [128, 128], BF16)
    SM = sb("SM", [128, 1024], F32)
    CWA = sb("CWA", [128, DEPTH, 4, 3], F32)
    CBA = sb("CBA", [128, DEPTH, 4], F32)
    BST = sb("BST", [128, DEPTH, 4], F32)
    GBB = sb("GBB", [128, DEPTH, 16], F32)
    SNK = sb("SNK", [128, DEPTH, 8], F32)
    AFF = sb("AFF", [128, NB, 16], F32)
    MSK = sb("MSK", [128, NB, 16], F32)
    GTM = sb("GTM", [128, NB, 16], F32)
    SLT = sb("SLT", [128, NB, 16], F32)
    TOT = sb("TOT", [128, NB, 16], F32)
    AFT = sb("AFT", [16, S], F32)
    WRT = sb("WRT", [128, 8, 16], F32)
    PS = [es.enter_context(nc.psum_tensor("ps%d" % i, [128, 512], F32)) for i in range(8)]
    PR = [Res("ps%d" % i, excl=True) for i in range(8)]
    pstate = {"i": 0}

    def bank():
        i = pstate["i"]
        pstate["i"] = (i + 1) % 8
        return PS[i], PR[i]

    def carve(arena, off, dt, pat=None, **kw):
        return arena

    ident_f = CST[:, C_ID:C_ID + 128]
    tri_f = CST[:, C_TRF:C_TRF + 128]
    tri_b = CST[:, C_TRB:C_TRB + 128]
    ones_f = CST[:, C_ONE:C_ONE + 128]
    tri_s = CST[:, C_TRS:C_TRS + 128]
    iota_f = CST[:, C_IOTA:C_IOTA + 256]
    mneg = CST[:, C_MNEG:C_MNEG + 384]

    sm_off = {"i": 0}
    smr = {}

    def smslot(n):
        o = sm_off["i"]
        sm_off["i"] += n
        assert sm_off["i"] <= 1024
        return SM[:, o:o + n]

    EPSV = smslot(1)
    STAT = [smslot(12) for _ in range(4)]
    MVv = [smslot(2) for _ in range(4)]
    RSTD = [smslot(1) for _ in range(4)]
    NMR = [smslot(1) for _ in range(4)]
    r_stat = [Res("stat%d" % i) for i in range(4)]
    stat_i = {"i": 0}
    r_cst = Res("cst")
    r_mod = [Res("mod%d" % i) for i in range(3)]
    r_x = [Res("x%d" % b) for b in range(NB)]
    r_modd = Res("modd")
    r_xs = Res("xs")

    def mm(out, lhsT, rhs, start, stop, R, W):
        return P.op("pe", lambda e: e.matmul(out, lhsT=lhsT, rhs=rhs, start=start, stop=stop), R, W)

    def tr(out, in_, ident, R, W):
        return P.op("pe", lambda e: e.transpose(out=out, in_=in_, identity=ident), R, W)

    def act(out, in_, func, R, W, bias=None, scale=None, accum_out=None):
        kw = {}
        if bias is not None:
            kw["bias"] = bias
        if scale is not None:
            kw["scale"] = scale
        if accum_out is not None:
            kw["accum_out"] = accum_out
        return P.op("act", lambda e: e.activation(out=out, in_=in_, func=func, **kw), R, W)

    def ts(eng, out, in0, s1, s2, op0, op1, R, W):
        if op1 is None:
            return P.op(eng, lambda e: e.tensor_scalar(out=out, in0=in0, scalar1=s1, scalar2=None, op0=op0), R, W)
        return P.op(eng, lambda e: e.tensor_scalar(out=out, in0=in0, scalar1=s1, scalar2=s2, op0=op0, op1=op1), R, W)

    def tt(eng, out, in0, in1, op, R, W):
        return P.op(eng, lambda e: e.tensor_tensor(out=out, in0=in0, in1=in1, op=op), R, W)

    def stt(out, in0, scalar, in1, op0, op1, R, W):
        return P.op("dve", lambda e: e.scalar_tensor_tensor(out=out, in0=in0, scalar=scalar, in1=in1, op0=op0, op1=op1), R, W)

    def cp(eng, out, in_, R, W):
        if eng == "act":
            return act(out, in_, AF.Copy, R, W)
        return P.op(eng, lambda e: e.tensor_copy(out=out, in_=in_), R, W)

    def dma(q, out, in_, R, W):
        return P.op(q, lambda e: e.dma_start(out=out, in_=in_), R, W, dma=True)

    def ln_stats(xap, Rx, width=1024):
        i = stat_i["i"]
        stat_i["i"] = (i + 1) % 4
        rs = r_stat[i]
        nchunk = width // 512 if width >= 512 else 1
        cw = width // nchunk
        for j in range(nchunk):
            P.op("dve", lambda e, j=j: e.bn_stats(out=STAT[i][:, j * 6:(j + 1) * 6], in_=xap[:, j * cw:(j + 1) * cw]), [Rx], [rs])
        P.op("dve", lambda e: e.bn_aggr(out=MVv[i], in_=STAT[i][:, 0:6 * nchunk]), [rs], [rs])
        act(RSTD[i], MVv[i][:, 1:2], AF.Sqrt, [rs, r_cst], [rs], bias=EPSV, scale=1.0)
        P.op("dve", lambda e: e.reciprocal(out=RSTD[i], in_=RSTD[i]), [rs], [rs])
        stt(NMR[i], MVv[i][:, 0:1], -1.0, RSTD[i], ALU.mult, ALU.mult, [rs], [rs])
        return RSTD[i], NMR[i], rs

    def load_mod(slot, seq, l, which):
        off = l * 6 * D + which * D
        return dma("sp", MOD[slot][:], modd[seq:seq + 1, off:off + D].broadcast_to([128, D]), [r_modd], [r_mod[slot]])

    def load_row(slot, row_ap):
        return dma("sp", MOD[slot][:], row_ap.broadcast_to([128, D]), [], [r_mod[slot]])

    dma("sp", CST[:], cst_d, [], [r_cst])
    P.op("dve", lambda e: e.memset(EPSV, EPS), [], [r_cst])
    cp("dve", IDB[:], ident_f, [r_cst], [r_cst])
    cp("dve", ONEB[:], ones_f, [r_cst], [r_cst])
    r_small = Res("small")
    dma("act", CWA[:], cw_d.rearrange("l (f p) j -> p l f j", p=128), [], [r_small])
    dma("act", CBA[:], cb_d, [], [r_small])
    dma("act", BST[:], bsT_d.rearrange("l p g -> p l g"), [], [r_small])
    dma("act", GBB[:].rearrange("p l g -> p (l g)"), gb_d.rearrange("l g -> (l g)").unsqueeze(0).broadcast_to([128, DEPTH * 16]), [], [r_small])
    dma("act", SNK[:].rearrange("p l g -> p (l g)"), sink_d.rearrange("l g -> (l g)").unsqueeze(0).broadcast_to([128, DEPTH * 8]), [], [r_small])

    CT = sb("CTs", [128, 8, 2], F32)
    CTB = sb("CTB", [128, 8, 2], BF16)
    r_ct = Res("ct")
    dma("sp", CT[:], cT_d, [], [r_ct])
    act(CTB[:], CT[:], AF.Silu, [r_ct], [r_ct])
    WAD = [WW[:, i * 4096:(i + 1) * 4096].rearrange("p (k n) -> p k n", k=8) for i in range(4)]
    r_wad = [Res("wad%d" % i) for i in range(4)]
    MROW = XT[0:2, 0, :]
    BROW = XT[0:2, 1, 0:512]
    r_mrow = Res("mrow")
    r_brow = Res("brow")
    wi = 0
    for l in range(n_layers):
        for j in range(12):
            w = WAD[wi % 4]
            rw = r_wad[wi % 4]
            wi += 1
            dma("pool", w, wada_d[l, :, j * 512:(j + 1) * 512].rearrange("(k p) n -> p k n", p=128), [], [rw])
            pb, pr = bank()
            for k in range(8):
                mm(pb[0:2, :], CTB[:, k, :], w[:, k, :], k == 0, k == 7, [r_ct, rw], [pr])
            dma("sp", BROW, bada_d[l:l + 1, j * 512:(j + 1) * 512].broadcast_to([2, 512]), [], [r_brow])
            plus1 = 1.0 if (j // 2) in (1, 2, 4, 5) else 0.0
            stt(MROW[:, (j % 2) * 512:(j % 2) * 512 + 512], pb[0:2, :], plus1, BROW, ALU.add, ALU.add, [pr, r_brow], [r_mrow])
            if j % 2 == 1:
                dma("sp", modd[:, l * 6 * D + (j // 2) * D: l * 6 * D + (j // 2 + 1) * D], MROW, [r_mrow], [r_modd])
    P.barrier()
    def chk(name):
        if stop == name:
            raise Stop()

    HT = AA[:].rearrange("p (k t) -> p k t", k=8)
    H2B = AA[:].rearrange("p (b d) -> p b d", b=NB)
    MIX = BB[:].rearrange("p (b d) -> p b d", b=NB)
    r_ht = [Res("ht%d" % b) for b in range(NB)]
    r_mix_a = [Res("mixa%d" % b) for b in range(NB)]
    r_mix_m = [Res("mixm%d" % b) for b in range(NB)]
    r_mix_g = [Res("mixg%d" % b) for b in range(NB)]
    XTB = XT[:].bitcast(BF16).rearrange("p b d -> p (b d)")
    XTF = XT[:].rearrange("p b d -> p (b d)")

    LNX = [WW[:, 14336 + i * 2048: 14336 + (i + 1) * 2048].bitcast(F32) for i in range(2)]
    LNH = [WW[:, 18432 + i * 1024: 18432 + (i + 1) * 1024] for i in range(2)]
    r_lnx = [Res("lnx0"), Res("lnx1")]
    r_lnh = [Res("lnh0"), Res("lnh1")]
    WR = [WW[:, i * 4224:(i + 1) * 4224].rearrange("p (k n) -> p k n", k=8) for i in range(2)]
    r_wr = [Res("wr0"), Res("wr1")]

    def x_src(seq, l):
        src = x_d[seq] if l == 0 else xs_d
        return src.rearrange("(b p) d -> p b d", p=128), ([] if l == 0 else [r_xs])

    def mixer(seq, l):
        xsrc, xsr = x_src(seq, l)
        load_mod(0, seq, l, 1)
        load_mod(1, seq, l, 0)
        for b in range(NB):
            j = b % 2
            dma("sp", LNX[j], xsrc[:, b, :], xsr, [r_lnx[j]])
            rstd, nmr, rs = ln_stats(LNX[j], r_lnx[j])
            act(LNX[j], LNX[j], AF.Identity, [rs, r_lnx[j]], [r_lnx[j]], bias=nmr, scale=rstd)
            tt("dve", LNX[j], LNX[j], MOD[0][:], ALU.mult, [r_lnx[j], r_mod[0]], [r_lnx[j]])
            tt("pool", LNH[j], LNX[j], MOD[1][:], ALU.add, [r_lnx[j], r_mod[1]], [r_lnh[j]])
            pb, pr = bank()
            pbb = pb[:].bitcast(BF16)
            for k in range(8):
                tr(pbb[:, k * 128:(k + 1) * 128], LNH[j][:, k * 128:(k + 1) * 128], IDB[:], [r_lnh[j], r_cst], [pr])
            cp("act", HT[:, :, b * 128:(b + 1) * 128], pbb.rearrange("p (k t) -> p k t", k=8), [pr], [r_ht[b]])

        chk("A")

        def load_piece(slot, c0, c1):
            return dma("pool", WR[slot][:, :, 0:c1 - c0], win_d[l, :, c0:c1].rearrange("(k p) n -> p k n", p=128), [], [r_wr[slot]])

        def proj_fm(slot, lhs_fn, evac):
            for tq in range(4):
                pb, pr = bank()
                for k in range(8):
                    mm(pb[:, :], lhs_fn(k), HT[:, k, tq * 512:(tq + 1) * 512], k == 0, k == 7,
                       [r_wr[slot]] + r_ht[tq * 4:(tq + 1) * 4], [pr])
                evac(tq, pb, pr)

        def proj_tm(slot, b, c0, n, pb, pr):
            for k in range(8):
                mm(pb[:, 0:n], HT[:, k, b * 128:(b + 1) * 128], WR[slot][:, k, c0:c0 + n], k == 0, k == 7, [r_wr[slot], r_ht[b]], [pr])

        AQT = XTB[:, 0:8192].rearrange("p (c t) -> p c t", c=4)
        AKT = XTB[:, 8192:10240]
        AV1 = XTB[:, 10240:12320].rearrange("p (b g e) -> p b g e", b=NB, g=2)
        BIASM = XTF[:, 6400:9472].rearrange("p (h s) -> p h s", h=8)
        r_aq = [Res("aq%d" % i) for i in range(4)]
        r_ak = [Res("ak%d" % i) for i in range(4)]
        r_av = [Res("av%d" % b) for b in range(NB)]
        r_bm = Res("biasm")
        dma("act", BIASM, btab_d, [], [r_bm])
        for h in range(8):
            tt("pool", BIASM[:, h, :], BIASM[:, h, :], mneg, ALU.add, [r_bm, r_cst], [r_bm])
        P.op("pool", lambda e: e.memset(AV1[:, :, :, 64:65], 1.0), [], r_av)
        for g_ in range(2):
            for c_ in range(4):
                dma("pool", WR[0][:, :, c_ * 128 + g_ * 64: c_ * 128 + (g_ + 1) * 64],
                    win_d[l, :, (g_ * 4 + c_) * 64:(g_ * 4 + c_ + 1) * 64].rearrange("(k p) e -> p k e", p=128), [], [r_wr[0]])
        load_piece(1, 512, 768)
        for c in range(4):
            def ev(tq, pb, pr, c=c):
                act(AQT[:, c, tq * 512:(tq + 1) * 512], pb[:, :], AF.Copy, [pr], [r_aq[tq]], scale=0.125)
            proj_fm(0, lambda k, c=c: WR[0][:, k, c * 128:(c + 1) * 128], ev)

        def evk(tq, pb, pr):
            cp("dve", AKT[:, tq * 512:(tq + 1) * 512], pb[:, :], [pr], [r_ak[tq]])
        proj_fm(1, lambda k: WR[1][:, k, 0:128], evk)
        for b in range(NB):
            pb, pr = bank()
            proj_tm(1, b, 128, 128, pb, pr)
            cp("dve", AV1[:, b, :, 0:64], pb[:, 0:128].rearrange("p (g e) -> p g e", g=2), [pr], [r_av[b]])
        S1 = [XTF[:, 9472 + i * 384: 9472 + (i + 1) * 384] for i in range(8)]
        AVEC = XTF[:, 12544:12672]
        PBF = [XTB[:, 25344 + i * 384: 25344 + (i + 1) * 384] for i in range(8)]
        PTB = [XTB[:, 28416 + i * 384: 28416 + (i + 1) * 384] for i in range(8)]
        r_s1 = [Res("s1_%d" % i) for i in range(8)]
        r_pb = [Res("pb_%d" % i) for i in range(8)]
        r_pt = [Res("pt_%d" % i) for i in range(8)]
        r_vec = [Res("vec0"), Res("vec1")]
        bi = 0
        for n in range(NB):
            lo, hi = max(n - 1, 0), min(n + 1, NB - 1)
            nblk = hi - lo + 1
            ncol = nblk * 128
            j0 = (lo - (n - 1)) * 128
            for g in range(2):
                st = bi % 2
                bi += 1
                V = AVEC[:, st * 64:(st + 1) * 64].rearrange("p (r c) -> p r c", c=4)
                rv = r_vec[st]
                snk = SNK[:, l, g * 4:(g + 1) * 4]
                pss = []
                for c in range(4):
                    ps, prs = bank()
                    pss.append((ps, prs))
                    mm(ps[:, 0:ncol], AQT[g * 64:(g + 1) * 64, c, n * 128:(n + 1) * 128], AKT[g * 64:(g + 1) * 64, lo * 128:(hi + 1) * 128],
                       True, True, [r_aq[n // 4]] + [r_ak[bb // 4] for bb in range(lo, hi + 1)], [prs])
                for c in range(4):
                    s_ = st * 4 + c
                    h = g * 4 + c
                    ps, prs = pss[c]
                    tt("dve", S1[s_][:, 0:ncol], ps[:, 0:ncol], BIASM[:, h, j0:j0 + ncol], ALU.add, [prs, r_bm], [r_s1[s_]])
                    P.op("dve", lambda e, s_=s_, ncol=ncol, V=V, c=c: e.tensor_reduce(out=V[:, 0, c:c + 1], in_=S1[s_][:, 0:ncol], axis=AX.X, op=ALU.max),
                         [r_s1[s_]], [rv])
                tt("dve", V[:, 1, :], V[:, 0, :], snk, ALU.max, [rv, r_small], [rv])
                ts("dve", V[:, 2, :], V[:, 1, :], -1.0, None, ALU.mult, None, [rv], [rv])
                tt("dve", V[:, 3, :], snk, V[:, 1, :], ALU.subtract, [rv, r_small], [rv])
                for c in range(4):
                    s_ = st * 4 + c
                    act(PBF[s_][:, 0:ncol], S1[s_][:, 0:ncol], AF.Exp, [r_s1[s_], rv], [r_pb[s_]], bias=V[:, 2, c:c + 1], scale=1.0)
                act(V[:, 4, :], V[:, 3, :], AF.Exp, [rv], [rv])
                for half in range(2):
                    pt, prt = bank()
                    ptb = pt[:].bitcast(BF16)
                    for cc in range(2):
                        s_ = st * 4 + half * 2 + cc
                        for jb in range(nblk):
                            tr(ptb[:, cc * 384 + jb * 128: cc * 384 + (jb + 1) * 128], PBF[s_][:, jb * 128:(jb + 1) * 128], IDB[:], [r_pb[s_], r_cst], [prt])
                    s0 = st * 4 + half * 2
                    for cc in range(2):
                        cp("act", PTB[s0 + cc][:, 0:ncol], ptb[:, cc * 384: cc * 384 + ncol], [prt], [r_pt[s0 + cc]])
                po, pro = bank()
                for c in range(4):
                    s_ = st * 4 + c
                    for jb in range(nblk):
                        mm(po[:, c * 65:(c + 1) * 65], PTB[s_][:, jb * 128:(jb + 1) * 128], AV1[:, lo + jb, g, :], jb == 0, jb == nblk - 1,
                           [r_pt[s_], r_av[lo + jb]], [pro])
                tt("dve", V[:, 5, :], po[:, 0:260].rearrange("p (c e) -> p c e", c=4)[:, :, 64], V[:, 4, :], ALU.add, [pro, rv], [rv])
                P.op("dve", lambda e, V=V: e.reciprocal(out=V[:, 6, :], in_=V[:, 5, :]), [rv], [rv])
                for c in range(4):
                    h = g * 4 + c
                    ts("dve", MIX[:, n, h * 64:(h + 1) * 64], po[:, c * 65: c * 65 + 64], V[:, 6, c:c + 1], None, ALU.mult, None, [pro, rv], [r_mix_a[n]])
        P.barrier()
        chk("att")

        PRE = XTB[:, 0:8200].rearrange("p (c t) -> p c t", c=4)
        CQ = XTB[:, 8200:16392].rearrange("p (c t) -> p c t", c=4)
        KTOK = XTB[:, 16392:20488].rearrange("p (b d) -> p b d", b=NB)
        MV1 = XTB[:, 20488:24648].rearrange("p (b h e) -> p b h e", b=NB, h=4)
        MO = XTB[:, 24648:28744].rearrange("p (b d) -> p b d", b=NB)
        fo = 14400

        def falloc(n):
            nonlocal fo
            a = XTF[:, fo:fo + n]
            fo += n
            assert fo <= 16384
            return a
        GT = falloc(256).rearrange("p (b g) -> p b g", b=NB)
        LF = falloc(128).rearrange("p (d b h) -> p d b h", d=2, b=NB)
        EA = falloc(128).rearrange("p (d b h) -> p d b h", d=2, b=NB)
        EB = falloc(128).rearrange("p (d b h) -> p d b h", d=2, b=NB)
        EBL = falloc(128).rearrange("p (d b h) -> p d b h", d=2, b=NB)
        TMPG = falloc(128).rearrange("p (d b h) -> p d b h", d=2, b=NB)
        C32 = falloc(8 * 65).rearrange("p (c e) -> p c e", c=8)
        CTMP = falloc(2 * 65).rearrange("p (c e) -> p c e", c=2)
        ND = [falloc(65) for _ in range(4)]
        MVEC = falloc(32)
        HTMP = [falloc(64) for _ in range(2)]
        bo = 8448

        def balloc(n):
            nonlocal bo
            a = WW[:, bo:bo + n]
            bo += n
            assert bo <= 14336
            return a
        CONVT = [balloc(1024).bitcast(F32) for _ in range(2)]
        STM = [balloc(128) for _ in range(4)]
        VS2 = [balloc(130) for _ in range(2)]
        CBF = balloc(8 * 66).rearrange("p (c e) -> p c e", c=8)[:, :, 0:65]
        r_pre = [Res("pre%d" % i) for i in range(4)]
        r_cq = [[Res("cq%d_%d" % (f, i)) for i in range(4)] for f in range(4)]
        r_ktok = [Res("ktok%d" % b) for b in range(NB)]
        r_mv = [Res("mv%d" % b) for b in range(NB)]
        r_mo = [Res("mo%d" % b) for b in range(NB)]
        r_gt = Res("gt")
        r_gates = Res("gates")
        r_convt = [Res("cva"), Res("cvb")]
        r_stm = [Res("stm%d" % i) for i in range(4)]
        r_vs = [Res("vs%d" % i) for i in range(4)]
        r_c = [Res("c%d" % i) for i in range(8)]
        r_ctmp = [Res("ctmp0"), Res("ctmp1")]
        r_nd = [Res("nd%d" % i) for i in range(4)]
        r_mvec = [Res("mvec%d" % i) for i in range(4)]
        r_htmp = [Res("htmp0"), Res("htmp1")]
        load_piece(0, 768, 1280)
        load_piece(1, 1280, 1808)
        P.op("pool", lambda e: e.memset(PRE[:, :, 0:1], 0.0), [], r_pre)
        P.op("pool", lambda e: e.memset(PRE[:, :, 2049:2050], 0.0), [], r_pre)
        P.op("pool", lambda e: e.memset(MV1[:, :, :, 64:65], 1.0), [], r_mv)
        P.op("pool", lambda e: e.memset(C32[:, :, :], 0.0), [], r_c)
        P.op("pool", lambda e: e.memset(CBF, 0.0), [], r_c)
        for fc in range(4):
            def ev(tq, pb, pr, fc=fc):
                cp("act", PRE[:, fc, 1 + tq * 512: 1 + (tq + 1) * 512], pb[:, :], [pr], [r_pre[tq]])
            proj_fm(0, lambda k, fc=fc: WR[0][:, k, fc * 128:(fc + 1) * 128], ev)
        ci = 0
        for fc in range(4):
            for tq in range(4):
                t0 = tq * 512
                cv = CONVT[ci % 2]
                rc = r_convt[ci % 2]
                ci += 1
                rd = [r_pre[i] for i in range(max(tq - 1, 0), min(tq + 1, 3) + 1)] + [r_small]
                ts("dve", cv, PRE[:, fc, t0:t0 + 512], CWA[:, l, fc, 0:1], None, ALU.mult, None, rd, [rc])
                stt(cv, PRE[:, fc, t0 + 1:t0 + 513], CWA[:, l, fc, 1:2], cv, ALU.mult, ALU.add, rd + [rc], [rc])
                stt(cv, PRE[:, fc, t0 + 2:t0 + 514], CWA[:, l, fc, 2:3], cv, ALU.mult, ALU.add, rd + [rc], [rc])
                act(CQ[:, fc, t0:t0 + 512], cv, AF.Silu, [rc, r_small], [r_cq[fc][tq]], bias=CBA[:, l, fc:fc + 1], scale=1.0)
                if fc >= 2:
                    ts("dve", CQ[:, fc, t0:t0 + 512], CQ[:, fc, t0:t0 + 512], 0.125, None, ALU.mult, None, [r_cq[fc][tq]], [r_cq[fc][tq]])
        chk("m1")
        for b in range(NB):
            pb, pr = bank()
            pbb = pb[:].bitcast(BF16)
            for kc in range(2):
                tr(pbb[:, kc * 128:(kc + 1) * 128], CQ[:, 2 + kc, b * 128:(b + 1) * 128], IDB[:], [r_cq[2 + kc][b // 4], r_cst], [pr])
            cp("dve", KTOK[:, b, :], pbb[:, 0:256], [pr], [r_ktok[b]])
        chk("m1b")
        for b in range(NB):
            pb, pr = bank()
            proj_tm(1, b, 0, 512, pb, pr)
            pb2, pr2 = bank()
            proj_tm(1, b, 496, 32, pb2, pr2)
            cp("dve", MV1[:, b, :, 0:64], pb[:, 0:256].rearrange("p (h e) -> p h e", h=4), [pr], [r_mv[b]])
            sgt_ = CONVT[b % 2][:, 0:256]
            act(sgt_, pb[:, 256:512], AF.Exp, [pr], [r_convt[b % 2]], scale=-1.0)
            ts("dve", sgt_, sgt_, 1.0, None, ALU.add, None, [r_convt[b % 2]], [r_convt[b % 2]])
            P.op("dve", lambda e, sgt_=sgt_: e.reciprocal(out=sgt_, in_=sgt_), [r_convt[b % 2]], [r_convt[b % 2]])
            cp("dve", MO[:, b, :], sgt_, [r_convt[b % 2]], [r_mo[b]])
            tt("dve", GT[:, b, :], pb2[:, 16:32], GBB[:, l, :], ALU.add, [pr2, r_small], [r_gt])
        chk("m2")
        for d in range(2):
            act(TMPG[:, d], GT[:, :, (2 * d + 1) * 4:(2 * d + 2) * 4], AF.Exp, [r_gt], [r_gates], scale=-1.0)
            act(LF[:, d], TMPG[:, d], AF.Ln, [r_gates], [r_gates], bias=1.0, scale=1.0)
        for d in range(2):
            pb, pr = bank()
            mm(pb[:, 0:64], tri_f if d == 0 else tri_b, LF[:, d].rearrange("p b h -> p (b h)"), True, True, [r_gates, r_cst], [pr])
            pb2, pr2 = bank()
            mm(pb2[:, 0:64], ones_f, LF[:, d].rearrange("p b h -> p (b h)"), True, True, [r_gates, r_cst], [pr2])
            tt("dve", TMPG[:, d], pb[:, 0:64].rearrange("p (b h) -> p b h", b=NB), GT[:, :, (2 * d) * 4:(2 * d + 1) * 4], ALU.add, [pr, r_gt], [r_gates])
            act(EA[:, d], TMPG[:, d], AF.Exp, [r_gates], [r_gates])
            act(EB[:, d], pb[:, 0:64].rearrange("p (b h) -> p b h", b=NB), AF.Exp, [pr], [r_gates], scale=-1.0)
            act(EBL[:, d], pb2[:, 0:64].rearrange("p (b h) -> p b h", b=NB), AF.Exp, [pr2], [r_gates], scale=-1.0)
        chk("m3")
        uu = 0
        for ci_ in range(NB):
            for d in range(2):
                c = ci_ if d == 0 else NB - 1 - ci_
                msk = tri_f if d == 0 else tri_b
                for h in range(4):
                    kc, pbs = h // 2, (h % 2) * 64
                    ch = d * 4 + h
                    i4 = uu % 4
                    i2 = uu % 2
                    uu += 1
                    QT = CQ[pbs:pbs + 64, kc, c * 128:(c + 1) * 128]
                    KT = CQ[pbs:pbs + 64, 2 + kc, c * 128:(c + 1) * 128]
                    rq = [r_cq[kc][c // 4], r_cq[2 + kc][c // 4]]
                    ps, prs = bank()
                    mm(ps[:, 0:128], KT, QT, True, True, rq, [prs])
                    stt(STM[i4], ps[:, 0:128], EA[:, d, c, h:h + 1], msk, ALU.mult, ALU.mult, [prs, r_gates, r_cst], [r_stm[i4]])
                    pn, prn = bank()
                    mm(pn[:, 0:65], STM[i4], MV1[:, c, h, :], True, False, [r_stm[i4], r_mv[c]], [prn])
                    mm(pn[:, 0:65], CQ[:, kc, c * 128:(c + 1) * 128], CBF[:, ch, :], False, True, rq + [r_c[ch]], [prn])
                    ts("dve", ND[i4], pn[:, 0:65], EB[:, d, c, h:h + 1], None, ALU.mult, None, [prn, r_gates], [r_nd[i4]])
                    mv = MVEC[:, i4 * 8:(i4 + 1) * 8]
                    ts("dve", mv[:, 0:1], ND[i4][:, 64:65], -1.0, None, ALU.mult, None, [r_nd[i4]], [r_mvec[i4]])
                    ts("dve", mv[:, 0:1], mv[:, 0:1], ND[i4][:, 64:65], 1.0, ALU.max, ALU.max, [r_nd[i4], r_mvec[i4]], [r_mvec[i4]])
                    P.op("dve", lambda e, mv=mv: e.reciprocal(out=mv[:, 1:2], in_=mv[:, 0:1]), [r_mvec[i4]], [r_mvec[i4]])
                    dst = MIX[:, c, 512 + h * 64: 512 + (h + 1) * 64]
                    if (d == 0) == (c <= 7):
                        ts("dve", dst, ND[i4][:, 0:64], mv[:, 1:2], None, ALU.mult, None, [r_nd[i4], r_mvec[i4]], [r_mix_m[c]])
                    else:
                        stt(HTMP[i2], ND[i4][:, 0:64], mv[:, 1:2], dst, ALU.mult, ALU.add, [r_nd[i4], r_mvec[i4], r_mix_m[c]], [r_htmp[i2]])
                        tt("dve", dst, HTMP[i2], MO[:, c, h * 64:(h + 1) * 64], ALU.mult, [r_htmp[i2], r_mo[c]], [r_mix_m[c]])
                if ci_ < NB - 1:
                    for kc in range(2):
                        i2 = uu % 2
                        uu += 1
                        for hh in range(2):
                            h = kc * 2 + hh
                            ts("dve", VS2[i2][:, hh * 65:(hh + 1) * 65], MV1[:, c, h, :], EA[:, d, c, h:h + 1], None, ALU.mult, None, [r_mv[c], r_gates], [r_vs[i2]])
                        pu, pru = bank()
                        mm(pu[:, 0:130], KTOK[:, c, kc * 128:(kc + 1) * 128], VS2[i2], True, True, [r_ktok[c], r_vs[i2]], [pru])
                        for hh in range(2):
                            h = kc * 2 + hh
                            ch = d * 4 + h
                            pbs = hh * 64
                            tt("dve", CTMP[pbs:pbs + 64, i2, :], C32[pbs:pbs + 64, ch, :], pu[pbs:pbs + 64, hh * 65:(hh + 1) * 65], ALU.add, [r_c[ch], pru], [r_ctmp[i2]])
                            ts("dve", C32[pbs:pbs + 64, ch, :], CTMP[pbs:pbs + 64, i2, :], EBL[pbs:pbs + 64, d, c, h:h + 1], None, ALU.mult, None,
                               [r_ctmp[i2], r_gates], [r_c[ch]])
                            cp("act", CBF[pbs:pbs + 64, ch, :], C32[pbs:pbs + 64, ch, :], [r_c[ch]], [r_c[ch]])
        P.barrier()
        chk("mls")

        WSF = XTF[:, 0:512].rearrange("p (g s) -> p g s", g=4)
        WSB = XTB[:, 1024:1536].rearrange("p (g s) -> p g s", g=4)
        WST = XTB[:, 1536:2048].rearrange("p (g s) -> p g s", g=4)
        GV = [XTF[:, 1024 + i * 256: 1024 + (i + 1) * 256] for i in range(2)]
        VB = [XTB[:, 3072 + i * 256: 3072 + (i + 1) * 256] for i in range(2)]
        r_ws = Res("ws")
        r_gv = [Res("gv0"), Res("gv1")]
        r_vb = [Res("vb0"), Res("vb1")]
        load_piece(0, 1808, 2320)
        dma("act", WSF, ws_d[l].rearrange("g t s -> t g s"), [], [r_ws])
        cp("dve", WSB, WSF, [r_ws], [r_ws])
        pb, pr = bank()
        pbb = pb[:].bitcast(BF16)
        for g in range(4):
            tr(pbb[:, g * 128:(g + 1) * 128], WSB[:, g, :], IDB[:], [r_ws, r_cst], [pr])
        cp("dve", WST.rearrange("p g s -> p (g s)"), pbb[:, 0:512], [pr], [r_ws])
        for b in range(NB):
            j = b % 2
            pb, pr = bank()
            proj_tm(0, b, 0, 512, pb, pr)
            act(MIX[:, b, 768:1024], pb[:, 0:256], AF.Gelu, [pr], [r_mix_g[b]])
            act(GV[j], pb[:, 256:512], AF.Gelu, [pr], [r_gv[j]])
            rstd, nmr, rs = ln_stats(GV[j], r_gv[j], width=256)
            act(VB[j], GV[j], AF.Identity, [rs, r_gv[j]], [r_vb[j]], bias=nmr, scale=rstd)
            pg, prg = bank()
            for g in range(4):
                mm(pg[:, g * 64:(g + 1) * 64], WST[:, g, :], VB[j][:, g * 64:(g + 1) * 64], True, True, [r_ws, r_vb[j]], [prg])
            for g in range(4):
                dst = MIX[:, b, 768 + g * 64: 768 + (g + 1) * 64]
                stt(dst, pg[:, g * 64:(g + 1) * 64], BST[:, l, g:g + 1], dst, ALU.add, ALU.mult, [prg, r_small, r_mix_g[b]], [r_mix_g[b]])
        P.barrier()
        chk("gml")

        WO = WW[:, 0:8192].rearrange("p (k n) -> p k n", k=8)
        r_wo = Res("wo")
        MTB = [AA[:, i * 1024:(i + 1) * 1024].rearrange("p (k t) -> p k t", k=8) for i in range(2)]
        TMPF = [AA[:, 2048 + i * 1024: 2048 + (i + 1) * 1024].bitcast(F32) for i in range(2)]
        r_mtb = [Res("mtb0"), Res("mtb1")]
        r_tmpf = [Res("tf0"), Res("tf1")]
        dma("pool", WO, wout_d[l].rearrange("(k p) n -> p k n", p=128), [], [r_wo])
        load_mod(0, seq, l, 2)
        load_row(1, lng_d[l, 0:1, :])
        load_row(2, lnb_d[l, 0:1, :])
        for b in range(NB):
            j = b % 2
            dma("sp", XT[:, b, :], xsrc[:, b, :], xsr, [r_x[b]])
            pb, pr = bank()
            pbb = pb[:].bitcast(BF16)
            for k in range(8):
                tr(pbb[:, k * 128:(k + 1) * 128], MIX[:, b, k * 128:(k + 1) * 128], IDB[:], [r_mix_a[b], r_mix_m[b], r_mix_g[b], r_cst], [pr])
            cp("act", MTB[j].rearrange("p k t -> p (k t)"), pbb, [pr], [r_mtb[j]])
            for dh in range(2):
                po, pro = bank()
                for k in range(8):
                    mm(po[:, :], MTB[j][:, k, :], WO[:, k, dh * 512:(dh + 1) * 512], k == 0, k == 7, [r_mtb[j], r_wo], [pro])
                tt("dve", TMPF[dh], po[:, :], MOD[0][:, dh * 512:(dh + 1) * 512], ALU.mult, [pro, r_mod[0]], [r_tmpf[dh]])
                stt(XT[:, b, dh * 512:(dh + 1) * 512], XT[:, b, dh * 512:(dh + 1) * 512], ALPHA, TMPF[dh], ALU.mult, ALU.add,
                    [r_tmpf[dh], r_x[b]], [r_x[b]])
            rstd, nmr, rs = ln_stats(XT[:, b, :], r_x[b])
            act(XT[:, b, :], XT[:, b, :], AF.Identity, [rs, r_x[b]], [r_x[b]], bias=nmr, scale=rstd)
            tt("dve", XT[:, b, :], XT[:, b, :], MOD[1][:], ALU.mult, [r_x[b], r_mod[1]], [r_x[b]])
            tt("pool", XT[:, b, :], XT[:, b, :], MOD[2][:], ALU.add, [r_x[b], r_mod[2]], [r_x[b]])
        P.barrier()

    def moe(seq, l, last):
        load_mod(0, seq, l, 4)
        load_mod(1, seq, l, 3)
        r_wrt = Res("wrt")
        dma("act", WRT[:], wr_d[l].rearrange("(k p) e -> p k e", p=128), [], [r_wrt])
        r_h2b = [Res("h2b%d" % b) for b in range(NB)]
        r_aff = Res("aff")
        H2T = WW[:, 0:2048].bitcast(F32).rearrange("p (k t) -> p k t", k=8)
        r_h2t = Res("h2t")
        LVEC = SM[:, 200:264]
        r_lvec = [Res("lvec%d" % i) for i in range(4)]
        LG = [SM[:, 264 + i * 16: 264 + (i + 1) * 16] for i in range(4)]
        r_lg = [Res("lg%d" % i) for i in range(4)]
        for b in range(NB):
            j = b % 2
            i4 = b % 4
            rstd, nmr, rs = ln_stats(XT[:, b, :], r_x[b])
            act(LNX[j], XT[:, b, :], AF.Identity, [rs, r_x[b]], [r_lnx[j]], bias=nmr, scale=rstd)
            tt("dve", LNX[j], LNX[j], MOD[0][:], ALU.mult, [r_lnx[j], r_mod[0]], [r_lnx[j]])
            tt("pool", LNX[j], LNX[j], MOD[1][:], ALU.add, [r_lnx[j], r_mod[1]], [r_lnx[j]])
            cp("act", H2B[:, b, :], LNX[j], [r_lnx[j]], [r_h2b[b]])
            act(XT[:, b, :], XT[:, b, :], AF.Copy, [r_x[b]], [r_x[b]], scale=ALPHA)
            for half in range(2):
                pb, pr = bank()
                for kk in range(4):
                    k = half * 4 + kk
                    tr(pb[:, kk * 128:(kk + 1) * 128], LNX[j][:, k * 128:(k + 1) * 128], ident_f, [r_lnx[j], r_cst], [pr])
                cp("dve", H2T[:, half * 4:(half + 1) * 4, :].rearrange("p k t -> p (k t)"), pb[:, :], [pr], [r_h2t])
            pl, prl = bank()
            for k in range(8):
                mm(pl[:, 0:16], H2T[:, k, :], WRT[:, k, :], k == 0, k == 7, [r_h2t, r_wrt], [prl])
            vec = LVEC[:, i4 * 8:(i4 + 1) * 8]
            P.op("dve", lambda e, vec=vec, pl=pl: e.tensor_reduce(out=vec[:, 0:1], in_=pl[:, 0:16], axis=AX.X, op=ALU.max, negate=True), [prl], [r_lvec[i4]])
            act(LG[i4], pl[:, 0:16], AF.Exp, [prl, r_lvec[i4]], [r_lg[i4]], bias=vec[:, 0:1], scale=1.0)
            P.op("dve", lambda e, vec=vec, i4=i4: e.tensor_reduce(out=vec[:, 1:2], in_=LG[i4], axis=AX.X, op=ALU.add), [r_lg[i4]], [r_lvec[i4]])
            P.op("dve", lambda e, vec=vec: e.reciprocal(out=vec[:, 2:3], in_=vec[:, 1:2]), [r_lvec[i4]], [r_lvec[i4]])
            ts("dve", AFF[:, b, :], LG[i4], vec[:, 2:3], None, ALU.mult, None, [r_lg[i4], r_lvec[i4]], [r_aff])
        chk("E")
        r_aft = Res("aft")
        for q4 in range(4):
            pb, pr = bank()
            for bb in range(4):
                b = q4 * 4 + bb
                tr(pb[0:16, bb * 128:(bb + 1) * 128], AFF[:, b, :], ident_f, [r_aff, r_cst], [pr])
            cp("dve", AFT[:, q4 * 512:(q4 + 1) * 512], pb[0:16, :], [pr], [r_aft])
        M8 = SM[0:16, 400:408]
        r_m8 = Res("m8")
        WORK = WW[0:16, 4096:8192].bitcast(F32)
        r_work = Res("work")
        for r in range(32):
            src = AFT[:, :] if r == 0 else WORK
            P.op("dve", lambda e, src=src: e.max(out=M8, in_=src), [r_aft, r_work], [r_m8])
            if r < 31:
                P.op("dve", lambda e, src=src: e.match_replace(out=WORK, in_to_replace=M8, in_values=src, imm_value=-1.0), [r_aft, r_m8, r_work], [r_work])
        ts("dve", WORK, AFT[:, :], M8[:, 7:8], None, ALU.is_ge, None, [r_aft, r_m8, r_work], [r_work])
        r_sel_meta = Res("selmeta")
        for q4 in range(4):
            pb, pr = bank()
            for bb in range(4):
                b = q4 * 4 + bb
                tr(pb[:, bb * 16:(bb + 1) * 16], WORK[:, b * 128:(b + 1) * 128], ident_f[0:16, 0:16], [r_work, r_cst], [pr])
            cp("dve", MSK[:, q4 * 4:(q4 + 1) * 4, :].rearrange("p b e -> p (b e)"), pb[:, 0:64], [pr], [r_sel_meta])
        tt("dve", GTM[:].rearrange("p b e -> p (b e)"), MSK[:].rearrange("p b e -> p (b e)"), AFF[:].rearrange("p b e -> p (b e)"), ALU.mult,
           [r_sel_meta, r_aff], [r_sel_meta])
        pp, prp = bank()
        mm(pp[:, 0:256], tri_s, MSK[:].rearrange("p b e -> p (b e)"), True, True, [r_sel_meta, r_cst], [prp])
        pq, prq = bank()
        mm(pq[:, 0:256], ones_f, MSK[:].rearrange("p b e -> p (b e)"), True, True, [r_sel_meta, r_cst], [prq])
        cp("dve", TOT[:].rearrange("p b e -> p (b e)"), pq[:, 0:256], [prq], [r_sel_meta])
        CAR = SLT
        P.op("dve", lambda e: e.memset(CAR[:, 0, :], 0.0), [], [r_sel_meta])
        for b in range(1, NB):
            tt("dve", CAR[:, b, :], CAR[:, b - 1, :], TOT[:, b - 1, :], ALU.add, [r_sel_meta], [r_sel_meta])
        tt("dve", SLT[:].rearrange("p b e -> p (b e)"), SLT[:].rearrange("p b e -> p (b e)"), pp[:, 0:256], ALU.add, [r_sel_meta, prp], [r_sel_meta])
        P.barrier()
        chk("F")
        load_mod(0, seq, l, 5)
        SEL = BB[:, 0:4096].rearrange("p (b j) -> p b j", b=NB)
        SELT = BB[:, 4096:8192].rearrange("p (c t) -> p c t", c=2)
        XGT = BB[:, 8192:10240].rearrange("p (k j) -> p k j", k=8)
        HID = BB[:, 10240:14336].rearrange("p (f j) -> p f j", f=16)
        YE = BB[:, 14336:16384].rearrange("p (c d) -> p c d", c=2)
        WSL = [WW[:, i * 4096:(i + 1) * 4096] for i in range(4)]
        SGT = [WW[:, 16384 + i * 512: 16384 + (i + 1) * 512].bitcast(F32) for i in range(2)]
        r_wsl = [Res("wsl%d" % i) for i in range(4)]
        r_sgt = [Res("sgt0"), Res("sgt1")]
        r_sel = Res("sel")
        r_selt = Res("selt")
        r_xgt = Res("xgt")
        r_hid = [Res("hid%d" % i) for i in range(16)]
        r_ye = Res("ye")
        wsi = {"i": 0}

        def wslice(src_ap, pat, **kw):
            i = wsi["i"] % 4
            wsi["i"] += 1
            v = WSL[i].rearrange(pat, **kw)
            dma("pool", v, src_ap, [], [r_wsl[i]])
            return v, r_wsl[i]

        for e_ in range(16):
            for b in range(NB):
                ts("dve", SEL[:, b, :], iota_f, SLT[:, b, e_:e_ + 1], MSK[:, b, e_:e_ + 1],
                   ALU.is_equal, ALU.mult, [r_sel_meta, r_cst], [r_sel])
            for kp in range(4):
                pb, pr = bank()
                for kk in range(2):
                    k = kp * 2 + kk
                    for b in range(NB):
                        mm(pb[:, kk * 256:(kk + 1) * 256], H2B[:, b, k * 128:(k + 1) * 128], SEL[:, b, :], b == 0, b == NB - 1, [r_h2b[b], r_sel], [pr])
                cp("act" if kp % 2 else "dve", XGT[:, kp * 2:(kp + 1) * 2, :].rearrange("p k j -> p (k j)"), pb[:, :], [pr], [r_xgt])
            for c in range(2):
                for half in range(2):
                    pb, pr = bank()
                    pbb = pb[:].bitcast(BF16)
                    for bb in range(8):
                        b = half * 8 + bb
                        tr(pbb[:, bb * 128:(bb + 1) * 128], SEL[:, b, c * 128:(c + 1) * 128], IDB[:], [r_sel, r_cst], [pr])
                    cp("act", SELT[:, c, half * 1024:(half + 1) * 1024], pbb, [pr], [r_selt])
            for js in range(4):
                wgv, rwg = wslice(wg_d[l, e_, :, js * 512:(js + 1) * 512].rearrange("(k p) n -> p k n", p=128), "p (k n) -> p k n", k=8)
                wuv, rwu = wslice(wu_d[l, e_, :, js * 512:(js + 1) * 512].rearrange("(k p) n -> p k n", p=128), "p (k n) -> p k n", k=8)
                for fl in range(4):
                    fc = js * 4 + fl
                    pb, pr = bank()
                    for k in range(8):
                        mm(pb[:, 0:256], wgv[:, k, fl * 128:(fl + 1) * 128], XGT[:, k, :], k == 0, k == 7, [rwg, r_xgt], [pr])
                    for k in range(8):
                        mm(pb[:, 256:512], wuv[:, k, fl * 128:(fl + 1) * 128], XGT[:, k, :], k == 0, k == 7, [rwu, r_xgt], [pr])
                    j2 = fc % 2
                    act(SGT[j2], pb[:, 0:256], AF.Silu, [pr], [r_sgt[j2]])
                    tt("dve", HID[:, fc, :], SGT[j2], pb[:, 256:512], ALU.mult, [r_sgt[j2], pr], [r_hid[fc]])
            yb = [bank() for _ in range(4)]
            for js in range(4):
                wdv, rwd = wslice(wd_d[l, e_, js * 512:(js + 1) * 512, :].rearrange("(f p) n -> p f n", p=128), "p (f n) -> p f n", f=4)
                for fl in range(4):
                    fc = js * 4 + fl
                    for c in range(2):
                        for dh in range(2):
                            pb, pr = yb[c * 2 + dh]
                            mm(pb[:, :], HID[:, fc, c * 128:(c + 1) * 128], wdv[:, fl, dh * 512:(dh + 1) * 512], fc == 0, fc == 15, [r_hid[fc], rwd], [pr])
            for c in range(2):
                for dh in range(2):
                    pb, pr = yb[c * 2 + dh]
                    tt("dve", YE[:, c, dh * 512:(dh + 1) * 512], pb[:, :], MOD[0][:, dh * 512:(dh + 1) * 512], ALU.mult, [pr, r_mod[0]], [r_ye])
            for b in range(NB):
                for dh in range(2):
                    pb, pr = bank()
                    for c in range(2):
                        mm(pb[:, :], SELT[:, c, b * 128:(b + 1) * 128], YE[:, c, dh * 512:(dh + 1) * 512], c == 0, c == 1, [r_selt, r_ye], [pr])
                    stt(XT[:, b, dh * 512:(dh + 1) * 512], pb[:, :], GTM[:, b, e_:e_ + 1], XT[:, b, dh * 512:(dh + 1) * 512], ALU.mult, ALU.add,
                        [pr, r_sel_meta, r_x[b]], [r_x[b]])
        load_row(1, lng_d[l, 1:2, :])
        load_row(2, lnb_d[l, 1:2, :])
        outs = []
        dst = (out_d[seq] if last else xs_d).rearrange("(b p) d -> p b d", p=128)
        for b in range(NB):
            rstd, nmr, rs = ln_stats(XT[:, b, :], r_x[b])
            act(XT[:, b, :], XT[:, b, :], AF.Identity, [rs, r_x[b]], [r_x[b]], bias=nmr, scale=rstd)
            tt("dve", XT[:, b, :], XT[:, b, :], MOD[1][:], ALU.mult, [r_x[b], r_mod[1]], [r_x[b]])
            tt("pool", XT[:, b, :], XT[:, b, :], MOD[2][:], ALU.add, [r_x[b], r_mod[2]], [r_x[b]])
            outs.append(dma("sp", dst[:, b, :], XT[:, b, :], [r_x[b]], [] if last else [r_xs]))
        P.barrier()
        return outs

    final = []
    try:
        chk("pro")
        for seq in range(n_seq):
            for l in range(n_layers):
                mixer(seq, l)
                if "x1" in dbg_d and seq == 0 and l == 0:
                    for b in range(NB):
                        final.append(dma("sp", dbg_d["x1"].rearrange("(b p) d -> p b d", p=128)[:, b, :], XT[:, b, :], [r_x[b]], []))
                    P.barrier()
                chk("D")
                final += moe(seq, l, l == n_layers - 1)
    except Stop:
        P.barrier()
    P.op("sp", None, extra_deps=final)
    P.emit()
    es.close()
    return nc


_CACHE = {}


def host_inputs(inp, core):
    f = lambda a: np.ascontiguousarray(np.asarray(a, dtype=np.float32))
    sl = slice(2 * core, 2 * core + 2)
    q = np.arange(128)[:, None]
    sj = np.arange(384)[None, :]
    bk = t5_buckets(sj - 128 - q)
    btab = np.asarray(inp["rel_bias"], np.float32)[bk]
    m = {
        "x": f(inp["x"][sl]),
        "cT": f(np.asarray(inp["c"])[sl].T.reshape(8, 128, 2).transpose(1, 0, 2)),
        "w_ada": f(inp["w_ada"]), "b_ada": f(inp["b_ada"]), "w_in": f(inp["w_in"]),
        "conv_wT": f(np.asarray(inp["conv_w"]).transpose(0, 2, 1)), "conv_b": f(np.asarray(inp["conv_b"]).reshape(DEPTH, 4, 128).transpose(2, 0, 1)),
        "gate_b": f(inp["gate_b"]), "sink": f(inp["sink"]),
        "bias_tab": f(btab.transpose(0, 2, 1)),
        "w_s": f(inp["w_s"]), "b_sT": f(np.asarray(inp["b_s"]).transpose(0, 2, 1)),
        "w_out": f(inp["w_out"]), "w_router": f(inp["w_router"]),
        "w_gate": f(inp["w_gate"]), "w_up": f(inp["w_up"]), "w_down": f(inp["w_down"]),
        "ln_g": f(inp["ln_g"]), "ln_b": f(inp["ln_b"]),
        "cst": const_pack(),
    }
    return m


def kernel(**inputs):
    if "nc" not in _CACHE:
        _CACHE["nc"] = build()
    nc = _CACHE["nc"]
    shared = host_inputs(inputs, 0)
    in_maps = []
    for core in range(8):
        m = dict(shared)
        sl = slice(2 * core, 2 * core + 2)
        m["x"] = np.ascontiguousarray(np.asarray(inputs["x"], np.float32)[sl])
        m["cT"] = np.ascontiguousarray(np.asarray(inputs["c"], np.float32)[sl].T.reshape(8, 128, 2).transpose(1, 0, 2))
        in_maps.append(m)
    res = run_bass_kernel_spmd(nc, in_maps, core_ids=list(range(8)))
    return np.concatenate([r["out"] for r in res.results], axis=0).astype(np.float32)
```

```python
import numpy as np
import concourse.bass as bass
import concourse.mybir as mybir
from concourse.bass_utils import run_bass_kernel_spmd
from contextlib import ExitStack

F32 = mybir.dt.float32
BF16 = mybir.dt.bfloat16
AF = mybir.ActivationFunctionType
ALU = mybir.AluOpType
AX = mybir.AxisListType

ENGS = ["pe", "act", "dve", "pool", "sp"]
N_DMA_SEMS = 24
D = 1024
S = 2048
NB = 16
DEPTH = 4
ALPHA = float((2 * DEPTH) ** 0.25)
EPS = 1e-5


class Res:
    __slots__ = ("name", "w", "rs", "excl")

    def __init__(self, name="", excl=False):
        self.name = name
        self.w = None
        self.rs = []
        self.excl = excl


class Op:
    __slots__ = ("eng", "key", "pos", "fn", "waits", "signal", "sigval", "is_dma")


class Prog:
    def __init__(self, nc):
        self.nc = nc
        self.ops = {e: [] for e in ENGS}
        self.waited = {e: {} for e in ENGS}
        self.cnt = {}
        self.dma_rr = 0
        self.dma_last = [None] * N_DMA_SEMS
        self.last = {e: None for e in ENGS}

    def _dep(self, o, d):
        if d is None or d is o:
            return
        E = o.eng
        if (not d.is_dma) and d.eng == E and E == "pe":
            return
        w = self.waited[E]
        if w.get(d.key, 0) >= d.pos:
            return
        w[d.key] = d.pos
        d.signal = True
        o.waits.append(d)

    def op(self, eng, fn, reads=(), writes=(), dma=False, extra_deps=()):
        o = Op()
        o.eng = eng
        o.fn = fn
        o.waits = []
        o.signal = False
        o.sigval = None
        o.is_dma = dma
        prev = None
        if dma:
            si = self.dma_rr
            self.dma_rr = (self.dma_rr + 1) % N_DMA_SEMS
            o.key = ("dma", si)
            o.signal = True
            prev = self.dma_last[si]
            self.dma_last[si] = o
        else:
            o.key = eng
        self.cnt[o.key] = self.cnt.get(o.key, 0) + 1
        o.pos = self.cnt[o.key]
        if prev is not None:
            self._dep(o, prev)
        for d in extra_deps:
            self._dep(o, d)
        for r in reads:
            self._dep(o, r.w)
            if r.excl:
                for rd in r.rs:
                    if rd.eng != eng:
                        self._dep(o, rd)
        for wr in writes:
            self._dep(o, wr.w)
            for rd in wr.rs:
                self._dep(o, rd)
        for wr in writes:
            wr.w = o
            wr.rs = []
        for r in reads:
            if r.w is not o:
                r.rs.append(o)
        self.ops[eng].append(o)
        if fn is not None and not dma:
            self.last[eng] = o
        return o

    def barrier(self):
        lasts = [self.last[e] for e in ENGS if self.last[e] is not None]
        dmas = [d for d in self.dma_last if d is not None]
        for e in ENGS:
            self.op(e, None, extra_deps=lasts + dmas)

    def emit(self):
        nc = self.nc
        with ExitStack() as es:
            sems = {}
            for e in ENGS:
                sems[e] = es.enter_context(nc.semaphore("s_" + e))
            for i in range(N_DMA_SEMS):
                sems[("dma", i)] = es.enter_context(nc.semaphore("s_dma%d" % i))
            for e in ENGS:
                c = 0
                for o in self.ops[e]:
                    if o.is_dma:
                        o.sigval = 16 * o.pos
                    elif o.signal:
                        c += 1
                        o.sigval = c
            import sys as _s
            print("SIGCOUNTS", {e: (len(self.ops[e]), max([o.sigval or 0 for o in self.ops[e] if not o.is_dma] + [0])) for e in ENGS},
                  "dma", max([o.sigval or 0 for e in ENGS for o in self.ops[e] if o.is_dma] + [0]), file=_s.stderr)
            block = es.enter_context(nc.Block())

            def run(ename):
                def body(eng):
                    for o in self.ops[ename]:
                        for d in o.waits:
                            eng.wait_ge(sems[d.key], d.sigval)
                        if o.fn is None:
                            continue
                        inst = o.fn(eng)
                        if o.signal:
                            inst.then_inc(sems[o.key], 16 if o.is_dma else 1)
                return body

            block.sync(run("sp"))
            block.tensor(run("pe"))
            block.scalar(run("act"))
            block.vector(run("dve"))
            block.gpsimd(run("pool"))


def t5_buckets(rel):
    nb = 16
    max_exact = 8
    ret = np.where(rel > 0, nb, 0)
    n = np.abs(rel)
    large = max_exact + (np.log(np.maximum(n, 1) / max_exact) / np.log(128 / max_exact) * (nb - max_exact)).astype(np.int32)
    large = np.minimum(large, nb - 1)
    return (ret + np.where(n < max_exact, n, large)).astype(np.int32)


C_ID, C_TRF, C_TRB, C_ONE, C_TRS, C_IOTA, C_MNEG, C_END = 0, 128, 256, 384, 512, 640, 896, 1280


def const_pack():
    c = np.zeros((128, C_END), np.float32)
    u = np.arange(128)[:, None]
    t = np.arange(128)[None, :]
    c[:, C_ID:C_ID + 128] = (u == t)
    c[:, C_TRF:C_TRF + 128] = (u <= t)
    c[:, C_TRB:C_TRB + 128] = (u >= t)
    c[:, C_ONE:C_ONE + 128] = 1.0
    c[:, C_TRS:C_TRS + 128] = (u < t)
    c[:, C_IOTA:C_IOTA + 256] = np.arange(256)[None, :]
    q = np.arange(128)[:, None]
    sj = np.arange(384)[None, :]
    rel = sj - 128 - q
    c[:, C_MNEG:C_MNEG + 384] = np.where(np.abs(rel) <= 128, 0.0, -1e30)
    return c


class Stop(Exception):
    pass


def build(n_layers=DEPTH, n_seq=2, dbg=(), stop=None):
    nc = bass.Bass("TRN2", target_bir_lowering=False)

    def din(name, shape):
        return nc.dram_tensor(name, list(shape), F32, kind="ExternalInput").ap()

    x_d = din("x", [2, S, D])
    cT_d = din("cT", [128, 8, 2])
    wada_d = din("w_ada", [DEPTH, D, 6 * D])
    bada_d = din("b_ada", [DEPTH, 6 * D])
    win_d = din("w_in", [DEPTH, D, 2320])
    cw_d = din("conv_wT", [DEPTH, 512, 3])
    cb_d = din("conv_b", [128, DEPTH, 4])
    gb_d = din("gate_b", [DEPTH, 16])
    sink_d = din("sink", [DEPTH, 8])
    btab_d = din("bias_tab", [128, 8, 384])
    ws_d = din("w_s", [DEPTH, 4, 128, 128])
    bsT_d = din("b_sT", [DEPTH, 128, 4])
    wout_d = din("w_out", [DEPTH, D, D])
    wr_d = din("w_router", [DEPTH, D, 16])
    wg_d = din("w_gate", [DEPTH, 16, D, 2 * D])
    wu_d = din("w_up", [DEPTH, 16, D, 2 * D])
    wd_d = din("w_down", [DEPTH, 16, 2 * D, D])
    lng_d = din("ln_g", [DEPTH, 2, D])
    lnb_d = din("ln_b", [DEPTH, 2, D])
    cst_d = din("cst", [128, C_END])
    out_d = nc.dram_tensor("out", [2, S, D], F32, kind="ExternalOutput").ap()
    xs_d = nc.dram_tensor("xs", [S, D], F32, kind="Internal").ap()
    modd = nc.dram_tensor("modd", [2, DEPTH * 6 * D], F32, kind="Internal").ap()
    dbg_d = {}
    for nm in dbg:
        dbg_d[nm] = nc.dram_tensor("dbg_" + nm, [S, D], F32, kind="ExternalOutput").ap()

    es = ExitStack()
    P = Prog(nc)

    def sb(name, shape, dt):
        return es.enter_context(nc.sbuf_tensor(name, list(shape), dt))

    XT = sb("XT", [128, NB, D], F32)
    AA = sb("AA", [128, 16384], BF16)
    BB = sb("BB", [128, 16384], BF16)
    WW = sb("WW", [128, 20480], BF16)
    MOD = [sb("MOD%d" % i, [128, D], F32) for i in range(3)]
    CST = sb("CST", [128, C_END], F32)
    IDB = sb("IDB", [128, 128], BF16)
    ONEB = sb("ONEB", [128, 128], BF16)
    SM = sb("SM", [128, 1024], F32)
    CWA = sb("CWA", [128, DEPTH, 4, 3], F32)
    CBA = sb("CBA", [128, DEPTH, 4], F32)
    BST = sb("BST", [128, DEPTH, 4], F32)
    GBB = sb("GBB", [128, DEPTH, 16], F32)
    SNK = sb("SNK", [128, DEPTH, 8], F32)
    AFF = sb("AFF", [128, NB, 16], F32)
    MSK = sb("MSK", [128, NB, 16], F32)
    GTM = sb("GTM", [128, NB, 16], F32)
    SLT = sb("SLT", [128, NB, 16], F32)
    TOT = sb("TOT", [128, NB, 16], F32)
    AFT = sb("AFT", [16, S], F32)
    WRT = sb("WRT", [128, 8, 16], F32)
    PS = [es.enter_context(nc.psum_tensor("ps%d" % i, [128, 512], F32)) for i in range(8)]
    PR = [Res("ps%d" % i, excl=True) for i in range(8)]
    pstate = {"i": 0}

    def bank():
        i = pstate["i"]
        pstate["i"] = (i + 1) % 8
        return PS[i], PR[i]

    def carve(arena, off, dt, pat=None, **kw):
        return arena

    ident_f = CST[:, C_ID:C_ID + 128]
    tri_f = CST[:, C_TRF:C_TRF + 128]
    tri_b = CST[:, C_TRB:C_TRB + 128]
    ones_f = CST[:, C_ONE:C_ONE + 128]
    tri_s = CST[:, C_TRS:C_TRS + 128]
    iota_f = CST[:, C_IOTA:C_IOTA + 256]
    mneg = CST[:, C_MNEG:C_MNEG + 384]

    sm_off = {"i": 0}
    smr = {}

    def smslot(n):
        o = sm_off["i"]
        sm_off["i"] += n
        assert sm_off["i"] <= 1024
        return SM[:, o:o + n]

    EPSV = smslot(1)
    STAT = [smslot(12) for _ in range(4)]
    MVv = [smslot(2) for _ in range(4)]
    RSTD = [smslot(1) for _ in range(4)]
    NMR = [smslot(1) for _ in range(4)]
    r_stat = [Res("stat%d" % i) for i in range(4)]
    stat_i = {"i": 0}
    r_cst = Res("cst")
    r_mod = [Res("mod%d" % i) for i in range(3)]
    r_x = [Res("x%d" % b) for b in range(NB)]
    r_modd = Res("modd")
    r_xs = Res("xs")

    def mm(out, lhsT, rhs, start, stop, R, W):
        return P.op("pe", lambda e: e.matmul(out, lhsT=lhsT, rhs=rhs, start=start, stop=stop), R, W)

    def tr(out, in_, ident, R, W):
        return P.op("pe", lambda e: e.transpose(out=out, in_=in_, identity=ident), R, W)

    def act(out, in_, func, R, W, bias=None, scale=None, accum_out=None):
        kw = {}
        if bias is not None:
            kw["bias"] = bias
        if scale is not None:
            kw["scale"] = scale
        if accum_out is not None:
            kw["accum_out"] = accum_out
        return P.op("act", lambda e: e.activation(out=out, in_=in_, func=func, **kw), R, W)

    def ts(eng, out, in0, s1, s2, op0, op1, R, W):
        if op1 is None:
            return P.op(eng, lambda e: e.tensor_scalar(out=out, in0=in0, scalar1=s1, scalar2=None, op0=op0), R, W)
        return P.op(eng, lambda e: e.tensor_scalar(out=out, in0=in0, scalar1=s1, scalar2=s2, op0=op0, op1=op1), R, W)

    def tt(eng, out, in0, in1, op, R, W):
        return P.op(eng, lambda e: e.tensor_tensor(out=out, in0=in0, in1=in1, op=op), R, W)

    def stt(out, in0, scalar, in1, op0, op1, R, W):
        return P.op("dve", lambda e: e.scalar_tensor_tensor(out=out, in0=in0, scalar=scalar, in1=in1, op0=op0, op1=op1), R, W)

    def cp(eng, out, in_, R, W):
        if eng == "act":
            return act(out, in_, AF.Copy, R, W)
        return P.op(eng, lambda e: e.tensor_copy(out=out, in_=in_), R, W)

    def dma(q, out, in_, R, W):
        return P.op(q, lambda e: e.dma_start(out=out, in_=in_), R, W, dma=True)

    def ln_stats(xap, Rx, width=1024):
        i = stat_i["i"]
        stat_i["i"] = (i + 1) % 4
        rs = r_stat[i]
        nchunk = width // 512 if width >= 512 else 1
        cw = width // nchunk
        for j in range(nchunk):
            P.op("dve", lambda e, j=j: e.bn_stats(out=STAT[i][:, j * 6:(j + 1) * 6], in_=xap[:, j * cw:(j + 1) * cw]), [Rx], [rs])
        P.op("dve", lambda e: e.bn_aggr(out=MVv[i], in_=STAT[i][:, 0:6 * nchunk]), [rs], [rs])
        act(RSTD[i], MVv[i][:, 1:2], AF.Sqrt, [rs, r_cst], [rs], bias=EPSV, scale=1.0)
        P.op("dve", lambda e: e.reciprocal(out=RSTD[i], in_=RSTD[i]), [rs], [rs])
        stt(NMR[i], MVv[i][:, 0:1], -1.0, RSTD[i], ALU.mult, ALU.mult, [rs], [rs])
        return RSTD[i], NMR[i], rs

    STATB = SM[:, 448:640].rearrange("p (b s) -> p b s", b=NB)
    MVB = SM[:, 640:672].rearrange("p (b s) -> p b s", b=NB)
    RSTDB = SM[:, 672:688]
    NMRB = SM[:, 688:704]
    r_sb = [Res("sb%d" % b) for b in range(NB)]
    r_stb = Res("stb")

    def ln_stats_batch():
        for b in range(NB):
            for j in range(2):
                P.op("dve", lambda e, b=b, j=j: e.bn_stats(out=STATB[:, b, j * 6:(j + 1) * 6], in_=XT[:, b, j * 512:(j + 1) * 512]), [r_x[b], r_stb], [r_sb[b]])
        for b in range(NB):
            P.op("dve", lambda e, b=b: e.bn_aggr(out=MVB[:, b, :], in_=STATB[:, b, :]), [r_sb[b]], [r_sb[b]])
        act(RSTDB, MVB[:, :, 1], AF.Sqrt, r_sb + [r_cst], [r_stb], bias=EPSV, scale=1.0)
        P.op("dve", lambda e: e.reciprocal(out=RSTDB, in_=RSTDB), [r_stb], [r_stb])
        stt(NMRB, MVB[:, :, 0], -1.0, RSTDB, ALU.mult, ALU.mult, r_sb + [r_stb], [r_stb])

    def load_mod(slot, seq, l, which):
        off = l * 6 * D + which * D
        return dma("sp", MOD[slot][:], modd[seq:seq + 1, off:off + D].broadcast_to([128, D]), [r_modd], [r_mod[slot]])

    def load_row(slot, row_ap):
        return dma("sp", MOD[slot][:], row_ap.broadcast_to([128, D]), [], [r_mod[slot]])

    dma("sp", CST[:], cst_d, [], [r_cst])
    P.op("dve", lambda e: e.memset(EPSV, EPS), [], [r_cst])
    cp("dve", IDB[:], ident_f, [r_cst], [r_cst])
    cp("dve", ONEB[:], ones_f, [r_cst], [r_cst])
    r_small = Res("small")
    dma("act", CWA[:], cw_d.rearrange("l (f p) j -> p l f j", p=128), [], [r_small])
    dma("act", CBA[:], cb_d, [], [r_small])
    dma("act", BST[:], bsT_d.rearrange("l p g -> p l g"), [], [r_small])
    dma("act", GBB[:].rearrange("p l g -> p (l g)"), gb_d.rearrange("l g -> (l g)").unsqueeze(0).broadcast_to([128, DEPTH * 16]), [], [r_small])
    dma("act", SNK[:].rearrange("p l g -> p (l g)"), sink_d.rearrange("l g -> (l g)").unsqueeze(0).broadcast_to([128, DEPTH * 8]), [], [r_small])

    CT = sb("CTs", [128, 8, 2], F32)
    CTB = sb("CTB", [128, 8, 2], BF16)
    r_ct = Res("ct")
    dma("sp", CT[:], cT_d, [], [r_ct])
    act(CTB[:], CT[:], AF.Silu, [r_ct], [r_ct])
    WAD = [WW[:, i * 4096:(i + 1) * 4096].rearrange("p (k n) -> p k n", k=8) for i in range(4)]
    r_wad = [Res("wad%d" % i) for i in range(4)]
    MROW = XT[0:2, 0, :]
    BROW = XT[0:2, 1, 0:512]
    r_mrow = Res("mrow")
    r_brow = Res("brow")
    wi = 0
    for l in range(n_layers):
        for j in range(12):
            w = WAD[wi % 4]
            rw = r_wad[wi % 4]
            wi += 1
            dma("pool", w, wada_d[l, :, j * 512:(j + 1) * 512].rearrange("(k p) n -> p k n", p=128), [], [rw])
            pb, pr = bank()
            for k in range(8):
                mm(pb[0:2, :], CTB[:, k, :], w[:, k, :], k == 0, k == 7, [r_ct, rw], [pr])
            dma("sp", BROW, bada_d[l:l + 1, j * 512:(j + 1) * 512].broadcast_to([2, 512]), [], [r_brow])
            plus1 = 1.0 if (j // 2) in (1, 2, 4, 5) else 0.0
            stt(MROW[:, (j % 2) * 512:(j % 2) * 512 + 512], pb[0:2, :], plus1, BROW, ALU.add, ALU.add, [pr, r_brow], [r_mrow])
            if j % 2 == 1:
                dma("sp", modd[:, l * 6 * D + (j // 2) * D: l * 6 * D + (j // 2 + 1) * D], MROW, [r_mrow], [r_modd])
    P.barrier()
    def chk(name):
        if stop == name:
            raise Stop()

    HT = AA[:].rearrange("p (k t) -> p k t", k=8)
    H2B = AA[:].rearrange("p (b d) -> p b d", b=NB)
    MIX = BB[:].rearrange("p (b d) -> p b d", b=NB)
    r_ht = [Res("ht%d" % b) for b in range(NB)]
    r_mix_a = [Res("mixa%d" % b) for b in range(NB)]
    r_mix_m = [Res("mixm%d" % b) for b in range(NB)]
    r_mix_g = [Res("mixg%d" % b) for b in range(NB)]
    XTB = XT[:].bitcast(BF16).rearrange("p b d -> p (b d)")
    XTF = XT[:].rearrange("p b d -> p (b d)")

    LNX = [WW[:, 14336 + i * 2048: 14336 + (i + 1) * 2048].bitcast(F32) for i in range(2)]
    LNH = [WW[:, 18432 + i * 1024: 18432 + (i + 1) * 1024] for i in range(2)]
    r_lnx = [Res("lnx0"), Res("lnx1")]
    r_lnh = [Res("lnh0"), Res("lnh1")]
    WR = [WW[:, i * 4224:(i + 1) * 4224].rearrange("p (k n) -> p k n", k=8) for i in range(2)]
    r_wr = [Res("wr0"), Res("wr1")]

    def x_src(seq, l):
        src = x_d[seq] if l == 0 else xs_d
        return src.rearrange("(b p) d -> p b d", p=128), ([] if l == 0 else [r_xs])

    def mixer(seq, l):
        xsrc, xsr = x_src(seq, l)
        load_mod(0, seq, l, 1)
        load_mod(1, seq, l, 0)
        for b in range(NB):
            j = b % 2
            dma("sp", LNX[j], xsrc[:, b, :], xsr, [r_lnx[j]])
            rstd, nmr, rs = ln_stats(LNX[j], r_lnx[j])
            act(LNX[j], LNX[j], AF.Identity, [rs, r_lnx[j]], [r_lnx[j]], bias=nmr, scale=rstd)
            tt("dve", LNX[j], LNX[j], MOD[0][:], ALU.mult, [r_lnx[j], r_mod[0]], [r_lnx[j]])
            tt("pool", LNH[j], LNX[j], MOD[1][:], ALU.add, [r_lnx[j], r_mod[1]], [r_lnh[j]])
            pb, pr = bank()
            pbb = pb[:].bitcast(BF16)
            for k in range(8):
                tr(pbb[:, k * 128:(k + 1) * 128], LNH[j][:, k * 128:(k + 1) * 128], IDB[:], [r_lnh[j], r_cst], [pr])
            cp("act", HT[:, :, b * 128:(b + 1) * 128], pbb.rearrange("p (k t) -> p k t", k=8), [pr], [r_ht[b]])

        chk("A")

        def load_piece(slot, c0, c1):
            return dma("pool", WR[slot][:, :, 0:c1 - c0], win_d[l, :, c0:c1].rearrange("(k p) n -> p k n", p=128), [], [r_wr[slot]])

        def proj_fm(slot, lhs_fn, evac):
            for tq in range(4):
                pb, pr = bank()
                for k in range(8):
                    mm(pb[:, :], lhs_fn(k), HT[:, k, tq * 512:(tq + 1) * 512], k == 0, k == 7,
                       [r_wr[slot]] + r_ht[tq * 4:(tq + 1) * 4], [pr])
                evac(tq, pb, pr)

        def proj_tm(slot, b, c0, n, pb, pr):
            for k in range(8):
                mm(pb[:, 0:n], HT[:, k, b * 128:(b + 1) * 128], WR[slot][:, k, c0:c0 + n], k == 0, k == 7, [r_wr[slot], r_ht[b]], [pr])

        AQT = XTB[:, 0:8192].rearrange("p (c t) -> p c t", c=4)
        AKT = XTB[:, 8192:10240]
        AV1 = XTB[:, 10240:12320].rearrange("p (b g e) -> p b g e", b=NB, g=2)
        BIASM = XTF[:, 6400:9472].rearrange("p (h s) -> p h s", h=8)
        r_aq = [Res("aq%d" % i) for i in range(4)]
        r_ak = [Res("ak%d" % i) for i in range(4)]
        r_av = [Res("av%d" % b) for b in range(NB)]
        r_bm = Res("biasm")
        dma("act", BIASM, btab_d, [], [r_bm])
        for h in range(8):
            tt("pool", BIASM[:, h, :], BIASM[:, h, :], mneg, ALU.add, [r_bm, r_cst], [r_bm])
        P.op("pool", lambda e: e.memset(AV1[:, :, :, 64:65], 1.0), [], r_av)
        for g_ in range(2):
            for c_ in range(4):
                dma("pool", WR[0][:, :, c_ * 128 + g_ * 64: c_ * 128 + (g_ + 1) * 64],
                    win_d[l, :, (g_ * 4 + c_) * 64:(g_ * 4 + c_ + 1) * 64].rearrange("(k p) e -> p k e", p=128), [], [r_wr[0]])
        load_piece(1, 512, 768)
        for c in range(4):
            def ev(tq, pb, pr, c=c):
                act(AQT[:, c, tq * 512:(tq + 1) * 512], pb[:, :], AF.Copy, [pr], [r_aq[tq]], scale=0.125)
            proj_fm(0, lambda k, c=c: WR[0][:, k, c * 128:(c + 1) * 128], ev)

        def evk(tq, pb, pr):
            cp("dve", AKT[:, tq * 512:(tq + 1) * 512], pb[:, :], [pr], [r_ak[tq]])
        proj_fm(1, lambda k: WR[1][:, k, 0:128], evk)
        for b in range(NB):
            pb, pr = bank()
            proj_tm(1, b, 128, 128, pb, pr)
            cp("dve", AV1[:, b, :, 0:64], pb[:, 0:128].rearrange("p (g e) -> p g e", g=2), [pr], [r_av[b]])
        S1 = [XTF[:, 9472 + i * 384: 9472 + (i + 1) * 384] for i in range(8)]
        AVEC = XTF[:, 12544:12672]
        PBF = [XTB[:, 25344 + i * 384: 25344 + (i + 1) * 384] for i in range(8)]
        PTB = [XTB[:, 28416 + i * 384: 28416 + (i + 1) * 384] for i in range(8)]
        r_s1 = [Res("s1_%d" % i) for i in range(8)]
        r_pb = [Res("pb_%d" % i) for i in range(8)]
        r_pt = [Res("pt_%d" % i) for i in range(8)]
        r_vec = [Res("vec0"), Res("vec1")]
        bi = 0
        for n in range(NB):
            lo, hi = max(n - 1, 0), min(n + 1, NB - 1)
            nblk = hi - lo + 1
            ncol = nblk * 128
            j0 = (lo - (n - 1)) * 128
            for g in range(2):
                st = bi % 2
                bi += 1
                V = AVEC[:, st * 64:(st + 1) * 64].rearrange("p (r c) -> p r c", c=4)
                rv = r_vec[st]
                snk = SNK[:, l, g * 4:(g + 1) * 4]
                pss = []
                for c in range(4):
                    ps, prs = bank()
                    pss.append((ps, prs))
                    mm(ps[:, 0:ncol], AQT[g * 64:(g + 1) * 64, c, n * 128:(n + 1) * 128], AKT[g * 64:(g + 1) * 64, lo * 128:(hi + 1) * 128],
                       True, True, [r_aq[n // 4]] + [r_ak[bb // 4] for bb in range(lo, hi + 1)], [prs])
                for c in range(4):
                    s_ = st * 4 + c
                    h = g * 4 + c
                    ps, prs = pss[c]
                    tt("dve", S1[s_][:, 0:ncol], ps[:, 0:ncol], BIASM[:, h, j0:j0 + ncol], ALU.add, [prs, r_bm], [r_s1[s_]])
                    P.op("dve", lambda e, s_=s_, ncol=ncol, V=V, c=c: e.tensor_reduce(out=V[:, 0, c:c + 1], in_=S1[s_][:, 0:ncol], axis=AX.X, op=ALU.max),
                         [r_s1[s_]], [rv])
                tt("dve", V[:, 1, :], V[:, 0, :], snk, ALU.max, [rv, r_small], [rv])
                ts("dve", V[:, 2, :], V[:, 1, :], -1.0, None, ALU.mult, None, [rv], [rv])
                tt("dve", V[:, 3, :], snk, V[:, 1, :], ALU.subtract, [rv, r_small], [rv])
                for c in range(4):
                    s_ = st * 4 + c
                    act(PBF[s_][:, 0:ncol], S1[s_][:, 0:ncol], AF.Exp, [r_s1[s_], rv], [r_pb[s_]], bias=V[:, 2, c:c + 1], scale=1.0)
                act(V[:, 4, :], V[:, 3, :], AF.Exp, [rv], [rv])
                for half in range(2):
                    pt, prt = bank()
                    ptb = pt[:].bitcast(BF16)
                    for cc in range(2):
                        s_ = st * 4 + half * 2 + cc
                        for jb in range(nblk):
                            tr(ptb[:, cc * 384 + jb * 128: cc * 384 + (jb + 1) * 128], PBF[s_][:, jb * 128:(jb + 1) * 128], IDB[:], [r_pb[s_], r_cst], [prt])
                    s0 = st * 4 + half * 2
                    for cc in range(2):
                        cp("act", PTB[s0 + cc][:, 0:ncol], ptb[:, cc * 384: cc * 384 + ncol], [prt], [r_pt[s0 + cc]])
                po, pro = bank()
                for c in range(4):
                    s_ = st * 4 + c
                    for jb in range(nblk):
                        mm(po[:, c * 65:(c + 1) * 65], PTB[s_][:, jb * 128:(jb + 1) * 128], AV1[:, lo + jb, g, :], jb == 0, jb == nblk - 1,
                           [r_pt[s_], r_av[lo + jb]], [pro])
                tt("dve", V[:, 5, :], po[:, 0:260].rearrange("p (c e) -> p c e", c=4)[:, :, 64], V[:, 4, :], ALU.add, [pro, rv], [rv])
                P.op("dve", lambda e, V=V: e.reciprocal(out=V[:, 6, :], in_=V[:, 5, :]), [rv], [rv])
                for c in range(4):
                    h = g * 4 + c
                    ts("dve", MIX[:, n, h * 64:(h + 1) * 64], po[:, c * 65: c * 65 + 64], V[:, 6, c:c + 1], None, ALU.mult, None, [pro, rv], [r_mix_a[n]])
        P.barrier()
        chk("att")

        PRE = XTB[:, 0:8200].rearrange("p (c t) -> p c t", c=4)
        CQ = XTB[:, 8200:16392].rearrange("p (c t) -> p c t", c=4)
        KTOK = XTB[:, 16392:20488].rearrange("p (b d) -> p b d", b=NB)
        MV1 = XTB[:, 20488:24648].rearrange("p (b h e) -> p b h e", b=NB, h=4)
        MO = XTB[:, 24648:28744].rearrange("p (b d) -> p b d", b=NB)
        fo = 14400

        def falloc(n):
            nonlocal fo
            a = XTF[:, fo:fo + n]
            fo += n
            assert fo <= 16384
            return a
        GT = falloc(256).rearrange("p (b g) -> p b g", b=NB)
        LF = falloc(128).rearrange("p (d b h) -> p d b h", d=2, b=NB)
        EA = falloc(128).rearrange("p (d b h) -> p d b h", d=2, b=NB)
        EB = falloc(128).rearrange("p (d b h) -> p d b h", d=2, b=NB)
        EBL = falloc(128).rearrange("p (d b h) -> p d b h", d=2, b=NB)
        TMPG = falloc(128).rearrange("p (d b h) -> p d b h", d=2, b=NB)
        C32 = falloc(8 * 65).rearrange("p (c e) -> p c e", c=8)
        CTMP = falloc(2 * 65).rearrange("p (c e) -> p c e", c=2)
        ND = [falloc(65) for _ in range(4)]
        MVEC = falloc(32)
        HTMP = [falloc(64) for _ in range(2)]
        bo = 8448

        def balloc(n):
            nonlocal bo
            a = WW[:, bo:bo + n]
            bo += n
            assert bo <= 14336
            return a
        CONVT = [balloc(1024).bitcast(F32) for _ in range(2)]
        STM = [balloc(128) for _ in range(4)]
        VS2 = [balloc(130) for _ in range(2)]
        CBF = balloc(8 * 66).rearrange("p (c e) -> p c e", c=8)[:, :, 0:65]
        r_pre = [Res("pre%d" % i) for i in range(4)]
        r_cq = [[Res("cq%d_%d" % (f, i)) for i in range(4)] for f in range(4)]
        r_ktok = [Res("ktok%d" % b) for b in range(NB)]
        r_mv = [Res("mv%d" % b) for b in range(NB)]
        r_mo = [Res("mo%d" % b) for b in range(NB)]
        r_gt = Res("gt")
        r_gates = Res("gates")
        r_convt = [Res("cva"), Res("cvb")]
        r_stm = [Res("stm%d" % i) for i in range(4)]
        r_vs = [Res("vs%d" % i) for i in range(4)]
        r_c = [Res("c%d" % i) for i in range(8)]
        r_ctmp = [Res("ctmp0"), Res("ctmp1")]
        r_nd = [Res("nd%d" % i) for i in range(4)]
        r_mvec = [Res("mvec%d" % i) for i in range(4)]
        r_htmp = [Res("htmp0"), Res("htmp1")]
        load_piece(0, 768, 1280)
        load_piece(1, 1280, 1808)
        P.op("pool", lambda e: e.memset(PRE[:, :, 0:1], 0.0), [], r_pre)
        P.op("pool", lambda e: e.memset(PRE[:, :, 2049:2050], 0.0), [], r_pre)
        P.op("pool", lambda e: e.memset(MV1[:, :, :, 64:65], 1.0), [], r_mv)
        P.op("pool", lambda e: e.memset(C32[:, :, :], 0.0), [], r_c)
        P.op("pool", lambda e: e.memset(CBF, 0.0), [], r_c)
        for fc in range(4):
            def ev(tq, pb, pr, fc=fc):
                cp("act", PRE[:, fc, 1 + tq * 512: 1 + (tq + 1) * 512], pb[:, :], [pr], [r_pre[tq]])
            proj_fm(0, lambda k, fc=fc: WR[0][:, k, fc * 128:(fc + 1) * 128], ev)
        ci = 0
        for fc in range(4):
            for tq in range(4):
                t0 = tq * 512
                cv = CONVT[ci % 2]
                rc = r_convt[ci % 2]
                ci += 1
                rd = [r_pre[i] for i in range(max(tq - 1, 0), min(tq + 1, 3) + 1)] + [r_small]
                ts("dve", cv, PRE[:, fc, t0:t0 + 512], CWA[:, l, fc, 0:1], None, ALU.mult, None, rd, [rc])
                stt(cv, PRE[:, fc, t0 + 1:t0 + 513], CWA[:, l, fc, 1:2], cv, ALU.mult, ALU.add, rd + [rc], [rc])
                stt(cv, PRE[:, fc, t0 + 2:t0 + 514], CWA[:, l, fc, 2:3], cv, ALU.mult, ALU.add, rd + [rc], [rc])
                act(CQ[:, fc, t0:t0 + 512], cv, AF.Silu, [rc, r_small], [r_cq[fc][tq]], bias=CBA[:, l, fc:fc + 1], scale=1.0)
                if fc >= 2:
                    ts("dve", CQ[:, fc, t0:t0 + 512], CQ[:, fc, t0:t0 + 512], 0.125, None, ALU.mult, None, [r_cq[fc][tq]], [r_cq[fc][tq]])
        chk("m1")
        for b in range(NB):
            pb, pr = bank()
            pbb = pb[:].bitcast(BF16)
            for kc in range(2):
                tr(pbb[:, kc * 128:(kc + 1) * 128], CQ[:, 2 + kc, b * 128:(b + 1) * 128], IDB[:], [r_cq[2 + kc][b // 4], r_cst], [pr])
            cp("dve", KTOK[:, b, :], pbb[:, 0:256], [pr], [r_ktok[b]])
        chk("m1b")
        for b in range(NB):
            pb, pr = bank()
            proj_tm(1, b, 0, 512, pb, pr)
            pb2, pr2 = bank()
            proj_tm(1, b, 496, 32, pb2, pr2)
            cp("dve", MV1[:, b, :, 0:64], pb[:, 0:256].rearrange("p (h e) -> p h e", h=4), [pr], [r_mv[b]])
            sgt_ = CONVT[b % 2][:, 0:256]
            act(sgt_, pb[:, 256:512], AF.Exp, [pr], [r_convt[b % 2]], scale=-1.0)
            ts("dve", sgt_, sgt_, 1.0, None, ALU.add, None, [r_convt[b % 2]], [r_convt[b % 2]])
            P.op("dve", lambda e, sgt_=sgt_: e.reciprocal(out=sgt_, in_=sgt_), [r_convt[b % 2]], [r_convt[b % 2]])
            cp("dve", MO[:, b, :], sgt_, [r_convt[b % 2]], [r_mo[b]])
            tt("dve", GT[:, b, :], pb2[:, 16:32], GBB[:, l, :], ALU.add, [pr2, r_small], [r_gt])
        chk("m2")
        for d in range(2):
            act(TMPG[:, d], GT[:, :, (2 * d + 1) * 4:(2 * d + 2) * 4], AF.Exp, [r_gt], [r_gates], scale=-1.0)
            act(LF[:, d], TMPG[:, d], AF.Ln, [r_gates], [r_gates], bias=1.0, scale=1.0)
        for d in range(2):
            pb, pr = bank()
            mm(pb[:, 0:64], tri_f if d == 0 else tri_b, LF[:, d].rearrange("p b h -> p (b h)"), True, True, [r_gates, r_cst], [pr])
            pb2, pr2 = bank()
            mm(pb2[:, 0:64], ones_f, LF[:, d].rearrange("p b h -> p (b h)"), True, True, [r_gates, r_cst], [pr2])
            tt("dve", TMPG[:, d], pb[:, 0:64].rearrange("p (b h) -> p b h", b=NB), GT[:, :, (2 * d) * 4:(2 * d + 1) * 4], ALU.add, [pr, r_gt], [r_gates])
            act(EA[:, d], TMPG[:, d], AF.Exp, [r_gates], [r_gates])
            act(EB[:, d], pb[:, 0:64].rearrange("p (b h) -> p b h", b=NB), AF.Exp, [pr], [r_gates], scale=-1.0)
            act(EBL[:, d], pb2[:, 0:64].rearrange("p (b h) -> p b h", b=NB), AF.Exp, [pr2], [r_gates], scale=-1.0)
        chk("m3")
        uu = 0
        for ci_ in range(NB):
            for d in range(2):
                c = ci_ if d == 0 else NB - 1 - ci_
                msk = tri_f if d == 0 else tri_b
                for h in range(4):
                    kc, pbs = h // 2, (h % 2) * 64
                    ch = d * 4 + h
                    i4 = uu % 4
                    i2 = uu % 2
                    uu += 1
                    QT = CQ[pbs:pbs + 64, kc, c * 128:(c + 1) * 128]
                    KT = CQ[pbs:pbs + 64, 2 + kc, c * 128:(c + 1) * 128]
                    rq = [r_cq[kc][c // 4], r_cq[2 + kc][c // 4]]
                    ps, prs = bank()
                    mm(ps[:, 0:128], KT, QT, True, True, rq, [prs])
                    stt(STM[i4], ps[:, 0:128], EA[:, d, c, h:h + 1], msk, ALU.mult, ALU.mult, [prs, r_gates, r_cst], [r_stm[i4]])
                    pn, prn = bank()
                    mm(pn[:, 0:65], STM[i4], MV1[:, c, h, :], True, False, [r_stm[i4], r_mv[c]], [prn])
                    mm(pn[:, 0:65], CQ[:, kc, c * 128:(c + 1) * 128], CBF[:, ch, :], False, True, rq + [r_c[ch]], [prn])
                    ts("dve", ND[i4], pn[:, 0:65], EB[:, d, c, h:h + 1], None, ALU.mult, None, [prn, r_gates], [r_nd[i4]])
                    mv = MVEC[:, i4 * 8:(i4 + 1) * 8]
                    ts("dve", mv[:, 0:1], ND[i4][:, 64:65], -1.0, None, ALU.mult, None, [r_nd[i4]], [r_mvec[i4]])
                    ts("dve", mv[:, 0:1], mv[:, 0:1], ND[i4][:, 64:65], 1.0, ALU.max, ALU.max, [r_nd[i4], r_mvec[i4]], [r_mvec[i4]])
                    P.op("dve", lambda e, mv=mv: e.reciprocal(out=mv[:, 1:2], in_=mv[:, 0:1]), [r_mvec[i4]], [r_mvec[i4]])
                    dst = MIX[:, c, 512 + h * 64: 512 + (h + 1) * 64]
                    if (d == 0) == (c <= 7):
                        ts("dve", dst, ND[i4][:, 0:64], mv[:, 1:2], None, ALU.mult, None, [r_nd[i4], r_mvec[i4]], [r_mix_m[c]])
                    else:
                        stt(HTMP[i2], ND[i4][:, 0:64], mv[:, 1:2], dst, ALU.mult, ALU.add, [r_nd[i4], r_mvec[i4], r_mix_m[c]], [r_htmp[i2]])
                        tt("dve", dst, HTMP[i2], MO[:, c, h * 64:(h + 1) * 64], ALU.mult, [r_htmp[i2], r_mo[c]], [r_mix_m[c]])
                if ci_ < NB - 1:
                    for kc in range(2):
                        i2 = uu % 2
                        uu += 1
                        for hh in range(2):
                            h = kc * 2 + hh
                            ts("dve", VS2[i2][:, hh * 65:(hh + 1) * 65], MV1[:, c, h, :], EA[:, d, c, h:h + 1], None, ALU.mult, None, [r_mv[c], r_gates], [r_vs[i2]])
                        pu, pru = bank()
                        mm(pu[:, 0:130], KTOK[:, c, kc * 128:(kc + 1) * 128], VS2[i2], True, True, [r_ktok[c], r_vs[i2]], [pru])
                        for hh in range(2):
                            h = kc * 2 + hh
                            ch = d * 4 + h
                            pbs = hh * 64
                            tt("dve", CTMP[pbs:pbs + 64, i2, :], C32[pbs:pbs + 64, ch, :], pu[pbs:pbs + 64, hh * 65:(hh + 1) * 65], ALU.add, [r_c[ch], pru], [r_ctmp[i2]])
                            ts("dve", C32[pbs:pbs + 64, ch, :], CTMP[pbs:pbs + 64, i2, :], EBL[pbs:pbs + 64, d, c, h:h + 1], None, ALU.mult, None,
                               [r_ctmp[i2], r_gates], [r_c[ch]])
                            cp("act", CBF[pbs:pbs + 64, ch, :], C32[pbs:pbs + 64, ch, :], [r_c[ch]], [r_c[ch]])
        P.barrier()
        chk("mls")

        WSF = XTF[:, 0:512].rearrange("p (g s) -> p g s", g=4)
        WSB = XTB[:, 1024:1536].rearrange("p (g s) -> p g s", g=4)
        WST = XTB[:, 1536:2048].rearrange("p (g s) -> p g s", g=4)
        GV = [XTF[:, 1024 + i * 256: 1024 + (i + 1) * 256] for i in range(2)]
        VB = [XTB[:, 3072 + i * 256: 3072 + (i + 1) * 256] for i in range(2)]
        r_ws = Res("ws")
        r_gv = [Res("gv0"), Res("gv1")]
        r_vb = [Res("vb0"), Res("vb1")]
        load_piece(0, 1808, 2320)
        dma("act", WSF, ws_d[l].rearrange("g t s -> t g s"), [], [r_ws])
        cp("dve", WSB, WSF, [r_ws], [r_ws])
        pb, pr = bank()
        pbb = pb[:].bitcast(BF16)
        for g in range(4):
            tr(pbb[:, g * 128:(g + 1) * 128], WSB[:, g, :], IDB[:], [r_ws, r_cst], [pr])
        cp("dve", WST.rearrange("p g s -> p (g s)"), pbb[:, 0:512], [pr], [r_ws])
        for b in range(NB):
            j = b % 2
            pb, pr = bank()
            proj_tm(0, b, 0, 512, pb, pr)
            act(MIX[:, b, 768:1024], pb[:, 0:256], AF.Gelu, [pr], [r_mix_g[b]])
            act(GV[j], pb[:, 256:512], AF.Gelu, [pr], [r_gv[j]])
            rstd, nmr, rs = ln_stats(GV[j], r_gv[j], width=256)
            act(VB[j], GV[j], AF.Identity, [rs, r_gv[j]], [r_vb[j]], bias=nmr, scale=rstd)
            pg, prg = bank()
            for g in range(4):
                mm(pg[:, g * 64:(g + 1) * 64], WST[:, g, :], VB[j][:, g * 64:(g + 1) * 64], True, True, [r_ws, r_vb[j]], [prg])
            for g in range(4):
                dst = MIX[:, b, 768 + g * 64: 768 + (g + 1) * 64]
                stt(dst, pg[:, g * 64:(g + 1) * 64], BST[:, l, g:g + 1], dst, ALU.add, ALU.mult, [prg, r_small, r_mix_g[b]], [r_mix_g[b]])
        P.barrier()
        chk("gml")

        WO = WW[:, 0:8192].rearrange("p (k n) -> p k n", k=8)
        r_wo = Res("wo")
        MTB = [AA[:, i * 1024:(i + 1) * 1024].rearrange("p (k t) -> p k t", k=8) for i in range(2)]
        TMPF = [AA[:, 2048 + i * 1024: 2048 + (i + 1) * 1024].bitcast(F32) for i in range(2)]
        r_mtb = [Res("mtb0"), Res("mtb1")]
        r_tmpf = [Res("tf0"), Res("tf1")]
        dma("pool", WO, wout_d[l].rearrange("(k p) n -> p k n", p=128), [], [r_wo])
        load_mod(0, seq, l, 2)
        load_row(1, lng_d[l, 0:1, :])
        load_row(2, lnb_d[l, 0:1, :])
        for b in range(NB):
            j = b % 2
            dma("sp", XT[:, b, :], xsrc[:, b, :], xsr, [r_x[b]])
            pb, pr = bank()
            pbb = pb[:].bitcast(BF16)
            for k in range(8):
                tr(pbb[:, k * 128:(k + 1) * 128], MIX[:, b, k * 128:(k + 1) * 128], IDB[:], [r_mix_a[b], r_mix_m[b], r_mix_g[b], r_cst], [pr])
            cp("act", MTB[j].rearrange("p k t -> p (k t)"), pbb, [pr], [r_mtb[j]])
            for dh in range(2):
                po, pro = bank()
                for k in range(8):
                    mm(po[:, :], MTB[j][:, k, :], WO[:, k, dh * 512:(dh + 1) * 512], k == 0, k == 7, [r_mtb[j], r_wo], [pro])
                tt("dve", TMPF[dh], po[:, :], MOD[0][:, dh * 512:(dh + 1) * 512], ALU.mult, [pro, r_mod[0]], [r_tmpf[dh]])
                stt(XT[:, b, dh * 512:(dh + 1) * 512], XT[:, b, dh * 512:(dh + 1) * 512], ALPHA, TMPF[dh], ALU.mult, ALU.add,
                    [r_tmpf[dh], r_x[b]], [r_x[b]])
        ln_stats_batch()
        for b in range(NB):
            act(XT[:, b, :], XT[:, b, :], AF.Identity, [r_stb, r_x[b]], [r_x[b]], bias=NMRB[:, b:b + 1], scale=RSTDB[:, b:b + 1])
            tt("dve", XT[:, b, :], XT[:, b, :], MOD[1][:], ALU.mult, [r_x[b], r_mod[1]], [r_x[b]])
            tt("pool", XT[:, b, :], XT[:, b, :], MOD[2][:], ALU.add, [r_x[b], r_mod[2]], [r_x[b]])
        P.barrier()

    def moe(seq, l, last):
        load_mod(0, seq, l, 4)
        load_mod(1, seq, l, 3)
        r_wrt = Res("wrt")
        dma("act", WRT[:], wr_d[l].rearrange("(k p) e -> p k e", p=128), [], [r_wrt])
        r_h2b = [Res("h2b%d" % b) for b in range(NB)]
        r_aff = Res("aff")
        H2T = WW[:, 0:2048].bitcast(F32).rearrange("p (k t) -> p k t", k=8)
        r_h2t = Res("h2t")
        LVEC = SM[:, 200:264]
        r_lvec = [Res("lvec%d" % i) for i in range(4)]
        LG = [SM[:, 264 + i * 16: 264 + (i + 1) * 16] for i in range(4)]
        r_lg = [Res("lg%d" % i) for i in range(4)]
        ln_stats_batch()
        for b in range(NB):
            j = b % 2
            i4 = b % 4
            act(LNX[j], XT[:, b, :], AF.Identity, [r_stb, r_x[b]], [r_lnx[j]], bias=NMRB[:, b:b + 1], scale=RSTDB[:, b:b + 1])
            tt("dve", LNX[j], LNX[j], MOD[0][:], ALU.mult, [r_lnx[j], r_mod[0]], [r_lnx[j]])
            tt("pool", LNX[j], LNX[j], MOD[1][:], ALU.add, [r_lnx[j], r_mod[1]], [r_lnx[j]])
            cp("act", H2B[:, b, :], LNX[j], [r_lnx[j]], [r_h2b[b]])
            act(XT[:, b, :], XT[:, b, :], AF.Copy, [r_x[b]], [r_x[b]], scale=ALPHA)
            for half in range(2):
                pb, pr = bank()
                for kk in range(4):
                    k = half * 4 + kk
                    tr(pb[:, kk * 128:(kk + 1) * 128], LNX[j][:, k * 128:(k + 1) * 128], ident_f, [r_lnx[j], r_cst], [pr])
                cp("dve", H2T[:, half * 4:(half + 1) * 4, :].rearrange("p k t -> p (k t)"), pb[:, :], [pr], [r_h2t])
            pl, prl = bank()
            for k in range(8):
                mm(pl[:, 0:16], H2T[:, k, :], WRT[:, k, :], k == 0, k == 7, [r_h2t, r_wrt], [prl])
            vec = LVEC[:, i4 * 8:(i4 + 1) * 8]
            P.op("dve", lambda e, vec=vec, pl=pl: e.tensor_reduce(out=vec[:, 0:1], in_=pl[:, 0:16], axis=AX.X, op=ALU.max, negate=True), [prl], [r_lvec[i4]])
            act(LG[i4], pl[:, 0:16], AF.Exp, [prl, r_lvec[i4]], [r_lg[i4]], bias=vec[:, 0:1], scale=1.0)
            P.op("dve", lambda e, vec=vec, i4=i4: e.tensor_reduce(out=vec[:, 1:2], in_=LG[i4], axis=AX.X, op=ALU.add), [r_lg[i4]], [r_lvec[i4]])
            P.op("dve", lambda e, vec=vec: e.reciprocal(out=vec[:, 2:3], in_=vec[:, 1:2]), [r_lvec[i4]], [r_lvec[i4]])
            ts("dve", AFF[:, b, :], LG[i4], vec[:, 2:3], None, ALU.mult, None, [r_lg[i4], r_lvec[i4]], [r_aff])
        chk("E")
        r_aft = Res("aft")
        for q4 in range(4):
            pb, pr = bank()
            for bb in range(4):
                b = q4 * 4 + bb
                tr(pb[0:16, bb * 128:(bb + 1) * 128], AFF[:, b, :], ident_f, [r_aff, r_cst], [pr])
            cp("dve", AFT[:, q4 * 512:(q4 + 1) * 512], pb[0:16, :], [pr], [r_aft])
        M8 = SM[0:16, 400:408]
        r_m8 = Res("m8")
        WORK = WW[0:16, 4096:8192].bitcast(F32)
        r_work = Res("work")
        for r in range(32):
            src = AFT[:, :] if r == 0 else WORK
            P.op("dve", lambda e, src=src: e.max(out=M8, in_=src), [r_aft, r_work], [r_m8])
            if r < 31:
                P.op("dve", lambda e, src=src: e.match_replace(out=WORK, in_to_replace=M8, in_values=src, imm_value=-1.0), [r_aft, r_m8, r_work], [r_work])
        ts("dve", WORK, AFT[:, :], M8[:, 7:8], None, ALU.is_ge, None, [r_aft, r_m8, r_work], [r_work])
        r_sel_meta = Res("selmeta")
        for q4 in range(4):
            pb, pr = bank()
            for bb in range(4):
                b = q4 * 4 + bb
                tr(pb[:, bb * 16:(bb + 1) * 16], WORK[:, b * 128:(b + 1) * 128], ident_f[0:16, 0:16], [r_work, r_cst], [pr])
            cp("dve", MSK[:, q4 * 4:(q4 + 1) * 4, :].rearrange("p b e -> p (b e)"), pb[:, 0:64], [pr], [r_sel_meta])
        tt("dve", GTM[:].rearrange("p b e -> p (b e)"), MSK[:].rearrange("p b e -> p (b e)"), AFF[:].rearrange("p b e -> p (b e)"), ALU.mult,
           [r_sel_meta, r_aff], [r_sel_meta])
        pp, prp = bank()
        mm(pp[:, 0:256], tri_s, MSK[:].rearrange("p b e -> p (b e)"), True, True, [r_sel_meta, r_cst], [prp])
        pq, prq = bank()
        mm(pq[:, 0:256], ones_f, MSK[:].rearrange("p b e -> p (b e)"), True, True, [r_sel_meta, r_cst], [prq])
        cp("dve", TOT[:].rearrange("p b e -> p (b e)"), pq[:, 0:256], [prq], [r_sel_meta])
        CAR = SLT
        P.op("dve", lambda e: e.memset(CAR[:, 0, :], 0.0), [], [r_sel_meta])
        for b in range(1, NB):
            tt("dve", CAR[:, b, :], CAR[:, b - 1, :], TOT[:, b - 1, :], ALU.add, [r_sel_meta], [r_sel_meta])
        tt("dve", SLT[:].rearrange("p b e -> p (b e)"), SLT[:].rearrange("p b e -> p (b e)"), pp[:, 0:256], ALU.add, [r_sel_meta, prp], [r_sel_meta])
        P.barrier()
        chk("F")
        load_mod(0, seq, l, 5)
        SEL = BB[:, 0:4096].rearrange("p (b j) -> p b j", b=NB)
        SELT = BB[:, 4096:8192].rearrange("p (c t) -> p c t", c=2)
        XGT = BB[:, 8192:10240].rearrange("p (k j) -> p k j", k=8)
        HID = BB[:, 10240:14336].rearrange("p (f j) -> p f j", f=16)
        YE = BB[:, 14336:16384].rearrange("p (c d) -> p c d", c=2)
        WSL = [WW[:, i * 4096:(i + 1) * 4096] for i in range(4)]
        SGT = [WW[:, 16384 + i * 512: 16384 + (i + 1) * 512].bitcast(F32) for i in range(2)]
        r_wsl = [Res("wsl%d" % i) for i in range(4)]
        r_sgt = [Res("sgt0"), Res("sgt1")]
        r_sel = Res("sel")
        r_selt = Res("selt")
        r_xgt = Res("xgt")
        r_hid = [Res("hid%d" % i) for i in range(16)]
        r_ye = Res("ye")
        wsi = {"i": 0}

        def wslice(src_ap, pat, **kw):
            i = wsi["i"] % 4
            wsi["i"] += 1
            v = WSL[i].rearrange(pat, **kw)
            dma("pool", v, src_ap, [], [r_wsl[i]])
            return v, r_wsl[i]

        r_selb = [Res("selb%d" % b) for b in range(NB)]
        r_xg = [Res("xg%d" % i) for i in range(4)]
        r_st4 = [Res("selt%d" % i) for i in range(4)]
        r_ye4 = [Res("ye%d" % i) for i in range(4)]

        def sel_build(e_):
            for b in range(NB):
                ts("dve", SEL[:, b, :], iota_f, SLT[:, b, e_:e_ + 1], MSK[:, b, e_:e_ + 1],
                   ALU.is_equal, ALU.mult, [r_sel_meta, r_cst], [r_selb[b]])

        def gather(e_):
            for kp in range(4):
                pb, pr = bank()
                for kk in range(2):
                    k = kp * 2 + kk
                    for b in range(NB):
                        mm(pb[:, kk * 256:(kk + 1) * 256], H2B[:, b, k * 128:(k + 1) * 128], SEL[:, b, :], b == 0, b == NB - 1, [r_h2b[b], r_selb[b]], [pr])
                cp("act" if kp % 2 else "dve", XGT[:, kp * 2:(kp + 1) * 2, :].rearrange("p k j -> p (k j)"), pb[:, :], [pr], [r_xg[kp]])

        def sel_t(e_):
            for c in range(2):
                for half in range(2):
                    pb, pr = bank()
                    pbb = pb[:].bitcast(BF16)
                    for bb in range(8):
                        b = half * 8 + bb
                        tr(pbb[:, bb * 128:(bb + 1) * 128], SEL[:, b, c * 128:(c + 1) * 128], IDB[:], [r_selb[b], r_cst], [pr])
                    cp("act", SELT[:, c, half * 1024:(half + 1) * 1024], pbb, [pr], [r_st4[c * 2 + half]])

        def hid_ye(e_):
            for js in range(4):
                wgv, rwg = wslice(wg_d[l, e_, :, js * 512:(js + 1) * 512].rearrange("(k p) n -> p k n", p=128), "p (k n) -> p k n", k=8)
                wuv, rwu = wslice(wu_d[l, e_, :, js * 512:(js + 1) * 512].rearrange("(k p) n -> p k n", p=128), "p (k n) -> p k n", k=8)
                for fl in range(4):
                    fc = js * 4 + fl
                    pb, pr = bank()
                    for k in range(8):
                        mm(pb[:, 0:256], wgv[:, k, fl * 128:(fl + 1) * 128], XGT[:, k, :], k == 0, k == 7, [rwg, r_xg[k // 2]], [pr])
                    for k in range(8):
                        mm(pb[:, 256:512], wuv[:, k, fl * 128:(fl + 1) * 128], XGT[:, k, :], k == 0, k == 7, [rwu, r_xg[k // 2]], [pr])
                    j2 = fc % 2
                    act(SGT[j2], pb[:, 0:256], AF.Silu, [pr], [r_sgt[j2]])
                    tt("dve", HID[:, fc, :], SGT[j2], pb[:, 256:512], ALU.mult, [r_sgt[j2], pr], [r_hid[fc]])
            yb = [bank() for _ in range(4)]
            for js in range(4):
                wdv, rwd = wslice(wd_d[l, e_, js * 512:(js + 1) * 512, :].rearrange("(f p) n -> p f n", p=128), "p (f n) -> p f n", f=4)
                for fl in range(4):
                    fc = js * 4 + fl
                    for c in range(2):
                        for dh in range(2):
                            pb, pr = yb[c * 2 + dh]
                            mm(pb[:, :], HID[:, fc, c * 128:(c + 1) * 128], wdv[:, fl, dh * 512:(dh + 1) * 512], fc == 0, fc == 15, [r_hid[fc], rwd], [pr])
            for c in range(2):
                for dh in range(2):
                    pb, pr = yb[c * 2 + dh]
                    tt("dve", YE[:, c, dh * 512:(dh + 1) * 512], pb[:, :], MOD[0][:, dh * 512:(dh + 1) * 512], ALU.mult, [pr, r_mod[0]], [r_ye4[c * 2 + dh]])

        def scatter(e_):
            for b in range(NB):
                for dh in range(2):
                    pb, pr = bank()
                    for c in range(2):
                        mm(pb[:, :], SELT[:, c, b * 128:(b + 1) * 128], YE[:, c, dh * 512:(dh + 1) * 512], c == 0, c == 1,
                           [r_st4[c * 2 + b // 8], r_ye4[c * 2 + dh]], [pr])
                    stt(XT[:, b, dh * 512:(dh + 1) * 512], pb[:, :], GTM[:, b, e_:e_ + 1], XT[:, b, dh * 512:(dh + 1) * 512], ALU.mult, ALU.add,
                        [pr, r_sel_meta, r_x[b]], [r_x[b]])

        sel_build(0)
        gather(0)
        sel_t(0)
        for e_ in range(16):
            if e_ + 1 < 16:
                sel_build(e_ + 1)
            hid_ye(e_)
            if e_ + 1 < 16:
                gather(e_ + 1)
            scatter(e_)
            if e_ + 1 < 16:
                sel_t(e_ + 1)
        load_row(1, lng_d[l, 1:2, :])
        load_row(2, lnb_d[l, 1:2, :])
        outs = []
        dst = (out_d[seq] if last else xs_d).rearrange("(b p) d -> p b d", p=128)
        ln_stats_batch()
        for b in range(NB):
            act(XT[:, b, :], XT[:, b, :], AF.Identity, [r_stb, r_x[b]], [r_x[b]], bias=NMRB[:, b:b + 1], scale=RSTDB[:, b:b + 1])
            tt("dve", XT[:, b, :], XT[:, b, :], MOD[1][:], ALU.mult, [r_x[b], r_mod[1]], [r_x[b]])
            tt("pool", XT[:, b, :], XT[:, b, :], MOD[2][:], ALU.add, [r_x[b], r_mod[2]], [r_x[b]])
            outs.append(dma("sp", dst[:, b, :], XT[:, b, :], [r_x[b]], [] if last else [r_xs]))
        P.barrier()
        return outs

    final = []
    try:
        chk("pro")
        for seq in range(n_seq):
            for l in range(n_layers):
                mixer(seq, l)
                if "x1" in dbg_d and seq == 0 and l == 0:
                    for b in range(NB):
                        final.append(dma("sp", dbg_d["x1"].rearrange("(b p) d -> p b d", p=128)[:, b, :], XT[:, b, :], [r_x[b]], []))
                    P.barrier()
                chk("D")
                final += moe(seq, l, l == n_layers - 1)
    except Stop:
        P.barrier()
    P.op("sp", None, extra_deps=final)
    P.emit()
    es.close()
    return nc


_CACHE = {}


def host_inputs(inp, core):
    f = lambda a: np.ascontiguousarray(np.asarray(a, dtype=np.float32))
    sl = slice(2 * core, 2 * core + 2)
    q = np.arange(128)[:, None]
    sj = np.arange(384)[None, :]
    bk = t5_buckets(sj - 128 - q)
    btab = np.asarray(inp["rel_bias"], np.float32)[bk]
    m = {
        "x": f(inp["x"][sl]),
        "cT": f(np.asarray(inp["c"])[sl].T.reshape(8, 128, 2).transpose(1, 0, 2)),
        "w_ada": f(inp["w_ada"]), "b_ada": f(inp["b_ada"]), "w_in": f(inp["w_in"]),
        "conv_wT": f(np.asarray(inp["conv_w"]).transpose(0, 2, 1)), "conv_b": f(np.asarray(inp["conv_b"]).reshape(DEPTH, 4, 128).transpose(2, 0, 1)),
        "gate_b": f(inp["gate_b"]), "sink": f(inp["sink"]),
        "bias_tab": f(btab.transpose(0, 2, 1)),
        "w_s": f(inp["w_s"]), "b_sT": f(np.asarray(inp["b_s"]).transpose(0, 2, 1)),
        "w_out": f(inp["w_out"]), "w_router": f(inp["w_router"]),
        "w_gate": f(inp["w_gate"]), "w_up": f(inp["w_up"]), "w_down": f(inp["w_down"]),
        "ln_g": f(inp["ln_g"]), "ln_b": f(inp["ln_b"]),
        "cst": const_pack(),
    }
    return m


def kernel(**inputs):
    if "nc" not in _CACHE:
        _CACHE["nc"] = build()
    nc = _CACHE["nc"]
    shared = host_inputs(inputs, 0)
    in_maps = []
    for core in range(8):
        m = dict(shared)
        sl = slice(2 * core, 2 * core + 2)
        m["x"] = np.ascontiguousarray(np.asarray(inputs["x"], np.float32)[sl])
        m["cT"] = np.ascontiguousarray(np.asarray(inputs["c"], np.float32)[sl].T.reshape(8, 128, 2).transpose(1, 0, 2))
        in_maps.append(m)
    res = run_bass_kernel_spmd(nc, in_maps, core_ids=list(range(8)))
    return np.concatenate([r["out"] for r in res.results], axis=0).astype(np.float32)
```

```python
import numpy as np
import concourse.bass as bass
import concourse.mybir as mybir
from concourse.bass_utils import run_bass_kernel_spmd
from contextlib import ExitStack

F32 = mybir.dt.float32
BF16 = mybir.dt.bfloat16
AF = mybir.ActivationFunctionType
ALU = mybir.AluOpType
AX = mybir.AxisListType

ENGS = ["pe", "act", "dve", "pool", "sp"]
N_DMA_SEMS = 24
D = 1024
S = 2048
NB = 16
DEPTH = 4
ALPHA = float((2 * DEPTH) ** 0.25)
EPS = 1e-5


class Res:
    __slots__ = ("name", "w", "rs", "excl")

    def __init__(self, name="", excl=False):
        self.name = name
        self.w = None
        self.rs = []
        self.excl = excl


class Op:
    __slots__ = ("eng", "key", "pos", "fn", "waits", "signal", "sigval", "is_dma")


class Prog:
    def __init__(self, nc):
        self.nc = nc
        self.ops = {e: [] for e in ENGS}
        self.waited = {e: {} for e in ENGS}
        self.cnt = {}
        self.dma_rr = 0
        self.dma_last = [None] * N_DMA_SEMS
        self.last = {e: None for e in ENGS}

    def _dep(self, o, d):
        if d is None or d is o:
            return
        E = o.eng
        if (not d.is_dma) and d.eng == E and E == "pe":
            return
        w = self.waited[E]
        if w.get(d.key, 0) >= d.pos:
            return
        w[d.key] = d.pos
        d.signal = True
        o.waits.append(d)

    def op(self, eng, fn, reads=(), writes=(), dma=False, extra_deps=()):
        o = Op()
        o.eng = eng
        o.fn = fn
        o.waits = []
        o.signal = False
        o.sigval = None
        o.is_dma = dma
        prev = None
        if dma:
            si = self.dma_rr
            self.dma_rr = (self.dma_rr + 1) % N_DMA_SEMS
            o.key = ("dma", si)
            o.signal = True
            prev = self.dma_last[si]
            self.dma_last[si] = o
        else:
            o.key = eng
        self.cnt[o.key] = self.cnt.get(o.key, 0) + 1
        o.pos = self.cnt[o.key]
        if prev is not None:
            self._dep(o, prev)
        for d in extra_deps:
            self._dep(o, d)
        for r in reads:
            self._dep(o, r.w)
            if r.excl:
                for rd in r.rs:
                    if rd.eng != eng:
                        self._dep(o, rd)
        for wr in writes:
            self._dep(o, wr.w)
            for rd in wr.rs:
                self._dep(o, rd)
        for wr in writes:
            wr.w = o
            wr.rs = []
        for r in reads:
            if r.w is not o:
                r.rs.append(o)
        self.ops[eng].append(o)
        if fn is not None and not dma:
            self.last[eng] = o
        return o

    def barrier(self):
        lasts = [self.last[e] for e in ENGS if self.last[e] is not None]
        dmas = [d for d in self.dma_last if d is not None]
        for e in ENGS:
            self.op(e, None, extra_deps=lasts + dmas)

    def emit(self):
        nc = self.nc
        with ExitStack() as es:
            sems = {}
            for e in ENGS:
                sems[e] = es.enter_context(nc.semaphore("s_" + e))
            for i in range(N_DMA_SEMS):
                sems[("dma", i)] = es.enter_context(nc.semaphore("s_dma%d" % i))
            for e in ENGS:
                c = 0
                for o in self.ops[e]:
                    if o.is_dma:
                        o.sigval = 16 * o.pos
                    elif o.signal:
                        c += 1
                        o.sigval = c
            import sys as _s
            print("SIGCOUNTS", {e: (len(self.ops[e]), max([o.sigval or 0 for o in self.ops[e] if not o.is_dma] + [0])) for e in ENGS},
                  "dma", max([o.sigval or 0 for e in ENGS for o in self.ops[e] if o.is_dma] + [0]), file=_s.stderr)
            block = es.enter_context(nc.Block())

            def run(ename):
                def body(eng):
                    for o in self.ops[ename]:
                        for d in o.waits:
                            eng.wait_ge(sems[d.key], d.sigval)
                        if o.fn is None:
                            continue
                        inst = o.fn(eng)
                        if o.signal:
                            inst.then_inc(sems[o.key], 16 if o.is_dma else 1)
                return body

            block.sync(run("sp"))
            block.tensor(run("pe"))
            block.scalar(run("act"))
            block.vector(run("dve"))
            block.gpsimd(run("pool"))


def t5_buckets(rel):
    nb = 16
    max_exact = 8
    ret = np.where(rel > 0, nb, 0)
    n = np.abs(rel)
    large = max_exact + (np.log(np.maximum(n, 1) / max_exact) / np.log(128 / max_exact) * (nb - max_exact)).astype(np.int32)
    large = np.minimum(large, nb - 1)
    return (ret + np.where(n < max_exact, n, large)).astype(np.int32)


C_ID, C_TRF, C_TRB, C_ONE, C_TRS, C_IOTA, C_MNEG, C_END = 0, 128, 256, 384, 512, 640, 896, 1280


def const_pack():
    c = np.zeros((128, C_END), np.float32)
    u = np.arange(128)[:, None]
    t = np.arange(128)[None, :]
    c[:, C_ID:C_ID + 128] = (u == t)
    c[:, C_TRF:C_TRF + 128] = (u <= t)
    c[:, C_TRB:C_TRB + 128] = (u >= t)
    c[:, C_ONE:C_ONE + 128] = 1.0
    c[:, C_TRS:C_TRS + 128] = (u < t)
    c[:, C_IOTA:C_IOTA + 256] = np.arange(256)[None, :]
    q = np.arange(128)[:, None]
    sj = np.arange(384)[None, :]
    rel = sj - 128 - q
    c[:, C_MNEG:C_MNEG + 384] = np.where(np.abs(rel) <= 128, 0.0, -1e30)
    return c


class Stop(Exception):
    pass


def build(n_layers=DEPTH, n_seq=2, dbg=(), stop=None):
    nc = bass.Bass("TRN2", target_bir_lowering=False)

    def din(name, shape):
        return nc.dram_tensor(name, list(shape), F32, kind="ExternalInput").ap()

    x_d = din("x", [2, S, D])
    cT_d = din("cT", [128, 8, 2])
    wada_d = din("w_ada", [DEPTH, D, 6 * D])
    bada_d = din("b_ada", [DEPTH, 6 * D])
    win_d = din("w_in", [DEPTH, D, 2320])
    cw_d = din("conv_wT", [DEPTH, 512, 3])
    cb_d = din("conv_b", [128, DEPTH, 4])
    gb_d = din("gate_b", [DEPTH, 16])
    sink_d = din("sink", [DEPTH, 8])
    btab_d = din("bias_tab", [128, 8, 384])
    ws_d = din("w_s", [DEPTH, 4, 128, 128])
    bsT_d = din("b_sT", [DEPTH, 128, 4])
    wout_d = din("w_out", [DEPTH, D, D])
    wr_d = din("w_router", [DEPTH, D, 16])
    wg_d = din("w_gate", [DEPTH, 16, D, 2 * D])
    wu_d = din("w_up", [DEPTH, 16, D, 2 * D])
    wd_d = din("w_down", [DEPTH, 16, 2 * D, D])
    lng_d = din("ln_g", [DEPTH, 2, D])
    lnb_d = din("ln_b", [DEPTH, 2, D])
    cst_d = din("cst", [128, C_END])
    out_d = nc.dram_tensor("out", [2, S, D], F32, kind="ExternalOutput").ap()
    xs_d = nc.dram_tensor("xs", [S, D], F32, kind="Internal").ap()
    modd = nc.dram_tensor("modd", [2, DEPTH * 6 * D], F32, kind="Internal").ap()
    dbg_d = {}
    for nm in dbg:
        dbg_d[nm] = nc.dram_tensor("dbg_" + nm, [S, D], F32, kind="ExternalOutput").ap()

    es = ExitStack()
    P = Prog(nc)

    def sb(name, shape, dt):
        return es.enter_context(nc.sbuf_tensor(name, list(shape), dt))

    XT = sb("XT", [128, NB, D], F32)
    AA = sb("AA", [128, 16384], BF16)
    BB = sb("BB", [128, 16384], BF16)
    WW = sb("WW", [128, 20480], BF16)
    MOD = [sb("MOD%d" % i, [128, D], F32) for i in range(3)]
    CST = sb("CST", [128, C_END], F32)
    IDB = sb("IDB", [128, 128], BF16)
    ONEB = sb("ONEB", [128, 128], BF16)
    SM = sb("SM", [128, 1024], F32)
    CWA = sb("CWA", [128, DEPTH, 4, 3], F32)
    CBA = sb("CBA", [128, DEPTH, 4], F32)
    BST = sb("BST", [128, DEPTH, 4], F32)
    GBB = sb("GBB", [128, DEPTH, 16], F32)
    SNK = sb("SNK", [128, DEPTH, 8], F32)
    AFF = sb("AFF", [128, NB, 16], F32)
    MSK = sb("MSK", [128, NB, 16], F32)
    GTM = sb("GTM", [128, NB, 16], F32)
    SLT = sb("SLT", [128, NB, 16], F32)
    TOT = sb("TOT", [128, NB, 16], F32)
    AFT = sb("AFT", [16, S], F32)
    WRT = sb("WRT", [128, 8, 16], F32)
    PS = [es.enter_context(nc.psum_tensor("ps%d" % i, [128, 512], F32)) for i in range(8)]
    PR = [Res("ps%d" % i, excl=True) for i in range(8)]
    pstate = {"i": 0}

    def bank():
        i = pstate["i"]
        pstate["i"] = (i + 1) % 8
        return PS[i], PR[i]

    def carve(arena, off, dt, pat=None, **kw):
        return arena

    ident_f = CST[:, C_ID:C_ID + 128]
    tri_f = CST[:, C_TRF:C_TRF + 128]
    tri_b = CST[:, C_TRB:C_TRB + 128]
    ones_f = CST[:, C_ONE:C_ONE + 128]
    tri_s = CST[:, C_TRS:C_TRS + 128]
    iota_f = CST[:, C_IOTA:C_IOTA + 256]
    mneg = CST[:, C_MNEG:C_MNEG + 384]

    sm_off = {"i": 0}
    smr = {}

    def smslot(n):
        o = sm_off["i"]
        sm_off["i"] += n
        assert sm_off["i"] <= 1024
        return SM[:, o:o + n]

    EPSV = smslot(1)
    STAT = [smslot(12) for _ in range(4)]
    MVv = [smslot(2) for _ in range(4)]
    RSTD = [smslot(1) for _ in range(4)]
    NMR = [smslot(1) for _ in range(4)]
    r_stat = [Res("stat%d" % i) for i in range(4)]
    stat_i = {"i": 0}
    r_cst = Res("cst")
    r_mod = [Res("mod%d" % i) for i in range(3)]
    r_x = [Res("x%d" % b) for b in range(NB)]
    r_modd = Res("modd")
    r_xs = Res("xs")

    def mm(out, lhsT, rhs, start, stop, R, W):
        return P.op("pe", lambda e: e.matmul(out, lhsT=lhsT, rhs=rhs, start=start, stop=stop), R, W)

    def tr(out, in_, ident, R, W):
        return P.op("pe", lambda e: e.transpose(out=out, in_=in_, identity=ident), R, W)

    def act(out, in_, func, R, W, bias=None, scale=None, accum_out=None):
        kw = {}
        if bias is not None:
            kw["bias"] = bias
        if scale is not None:
            kw["scale"] = scale
        if accum_out is not None:
            kw["accum_out"] = accum_out
        return P.op("act", lambda e: e.activation(out=out, in_=in_, func=func, **kw), R, W)

    def ts(eng, out, in0, s1, s2, op0, op1, R, W):
        if op1 is None:
            return P.op(eng, lambda e: e.tensor_scalar(out=out, in0=in0, scalar1=s1, scalar2=None, op0=op0), R, W)
        return P.op(eng, lambda e: e.tensor_scalar(out=out, in0=in0, scalar1=s1, scalar2=s2, op0=op0, op1=op1), R, W)

    def tt(eng, out, in0, in1, op, R, W):
        return P.op(eng, lambda e: e.tensor_tensor(out=out, in0=in0, in1=in1, op=op), R, W)

    def stt(out, in0, scalar, in1, op0, op1, R, W):
        return P.op("dve", lambda e: e.scalar_tensor_tensor(out=out, in0=in0, scalar=scalar, in1=in1, op0=op0, op1=op1), R, W)

    def cp(eng, out, in_, R, W):
        if eng == "act":
            return act(out, in_, AF.Copy, R, W)
        return P.op(eng, lambda e: e.tensor_copy(out=out, in_=in_), R, W)

    def dma(q, out, in_, R, W):
        return P.op(q, lambda e: e.dma_start(out=out, in_=in_), R, W, dma=True)

    def ln_stats(xap, Rx, width=1024):
        i = stat_i["i"]
        stat_i["i"] = (i + 1) % 4
        rs = r_stat[i]
        nchunk = width // 512 if width >= 512 else 1
        cw = width // nchunk
        for j in range(nchunk):
            P.op("dve", lambda e, j=j: e.bn_stats(out=STAT[i][:, j * 6:(j + 1) * 6], in_=xap[:, j * cw:(j + 1) * cw]), [Rx], [rs])
        P.op("dve", lambda e: e.bn_aggr(out=MVv[i], in_=STAT[i][:, 0:6 * nchunk]), [rs], [rs])
        act(RSTD[i], MVv[i][:, 1:2], AF.Sqrt, [rs, r_cst], [rs], bias=EPSV, scale=1.0)
        P.op("dve", lambda e: e.reciprocal(out=RSTD[i], in_=RSTD[i]), [rs], [rs])
        stt(NMR[i], MVv[i][:, 0:1], -1.0, RSTD[i], ALU.mult, ALU.mult, [rs], [rs])
        return RSTD[i], NMR[i], rs

    STATB = SM[:, 448:640].rearrange("p (b s) -> p b s", b=NB)
    MVB = SM[:, 640:672].rearrange("p (b s) -> p b s", b=NB)
    RSTDB = SM[:, 672:688]
    NMRB = SM[:, 688:704]
    r_sb = [Res("sb%d" % b) for b in range(NB)]
    r_stb = Res("stb")

    def ln_stats_batch():
        for b in range(NB):
            for j in range(2):
                P.op("dve", lambda e, b=b, j=j: e.bn_stats(out=STATB[:, b, j * 6:(j + 1) * 6], in_=XT[:, b, j * 512:(j + 1) * 512]), [r_x[b], r_stb], [r_sb[b]])
        for b in range(NB):
            P.op("dve", lambda e, b=b: e.bn_aggr(out=MVB[:, b, :], in_=STATB[:, b, :]), [r_sb[b]], [r_sb[b]])
        act(RSTDB, MVB[:, :, 1], AF.Sqrt, r_sb + [r_cst], [r_stb], bias=EPSV, scale=1.0)
        P.op("dve", lambda e: e.reciprocal(out=RSTDB, in_=RSTDB), [r_stb], [r_stb])
        stt(NMRB, MVB[:, :, 0], -1.0, RSTDB, ALU.mult, ALU.mult, r_sb + [r_stb], [r_stb])

    def load_mod(slot, seq, l, which):
        off = l * 6 * D + which * D
        return dma("sp", MOD[slot][:], modd[seq:seq + 1, off:off + D].broadcast_to([128, D]), [r_modd], [r_mod[slot]])

    def load_row(slot, row_ap):
        return dma("sp", MOD[slot][:], row_ap.broadcast_to([128, D]), [], [r_mod[slot]])

    dma("sp", CST[:], cst_d, [], [r_cst])
    P.op("dve", lambda e: e.memset(EPSV, EPS), [], [r_cst])
    cp("dve", IDB[:], ident_f, [r_cst], [r_cst])
    cp("dve", ONEB[:], ones_f, [r_cst], [r_cst])
    r_small = Res("small")
    dma("act", CWA[:], cw_d.rearrange("l (f p) j -> p l f j", p=128), [], [r_small])
    dma("act", CBA[:], cb_d, [], [r_small])
    dma("act", BST[:], bsT_d.rearrange("l p g -> p l g"), [], [r_small])
    dma("act", GBB[:].rearrange("p l g -> p (l g)"), gb_d.rearrange("l g -> (l g)").unsqueeze(0).broadcast_to([128, DEPTH * 16]), [], [r_small])
    dma("act", SNK[:].rearrange("p l g -> p (l g)"), sink_d.rearrange("l g -> (l g)").unsqueeze(0).broadcast_to([128, DEPTH * 8]), [], [r_small])

    CT = sb("CTs", [128, 8, 2], F32)
    CTB = sb("CTB", [128, 8, 2], BF16)
    r_ct = Res("ct")
    dma("sp", CT[:], cT_d, [], [r_ct])
    act(CTB[:], CT[:], AF.Silu, [r_ct], [r_ct])
    WAD = [WW[:, i * 4096:(i + 1) * 4096].rearrange("p (k n) -> p k n", k=8) for i in range(4)]
    r_wad = [Res("wad%d" % i) for i in range(4)]
    MROW = XT[0:2, 0, :]
    BROW = XT[0:2, 1, 0:512]
    r_mrow = Res("mrow")
    r_brow = Res("brow")
    wi = 0
    for l in range(n_layers):
        for j in range(12):
            w = WAD[wi % 4]
            rw = r_wad[wi % 4]
            wi += 1
            dma("pool", w, wada_d[l, :, j * 512:(j + 1) * 512].rearrange("(k p) n -> p k n", p=128), [], [rw])
            pb, pr = bank()
            for k in range(8):
                mm(pb[0:2, :], CTB[:, k, :], w[:, k, :], k == 0, k == 7, [r_ct, rw], [pr])
            dma("sp", BROW, bada_d[l:l + 1, j * 512:(j + 1) * 512].broadcast_to([2, 512]), [], [r_brow])
            plus1 = 1.0 if (j // 2) in (1, 2, 4, 5) else 0.0
            stt(MROW[:, (j % 2) * 512:(j % 2) * 512 + 512], pb[0:2, :], plus1, BROW, ALU.add, ALU.add, [pr, r_brow], [r_mrow])
            if j % 2 == 1:
                dma("sp", modd[:, l * 6 * D + (j // 2) * D: l * 6 * D + (j // 2 + 1) * D], MROW, [r_mrow], [r_modd])
    P.barrier()
    def chk(name):
        if stop == name:
            raise Stop()

    HT = AA[:].rearrange("p (k t) -> p k t", k=8)
    H2B = AA[:].rearrange("p (b d) -> p b d", b=NB)
    MIX = BB[:].rearrange("p (b d) -> p b d", b=NB)
    r_ht = [Res("ht%d" % b) for b in range(NB)]
    r_mix_a = [Res("mixa%d" % b) for b in range(NB)]
    r_mix_m = [Res("mixm%d" % b) for b in range(NB)]
    r_mix_g = [Res("mixg%d" % b) for b in range(NB)]
    XTB = XT[:].bitcast(BF16).rearrange("p b d -> p (b d)")
    XTF = XT[:].rearrange("p b d -> p (b d)")

    LNX = [WW[:, 14336 + i * 2048: 14336 + (i + 1) * 2048].bitcast(F32) for i in range(2)]
    LNH = [WW[:, 18432 + i * 1024: 18432 + (i + 1) * 1024] for i in range(2)]
    r_lnx = [Res("lnx0"), Res("lnx1")]
    r_lnh = [Res("lnh0"), Res("lnh1")]
    WR = [WW[:, i * 4224:(i + 1) * 4224].rearrange("p (k n) -> p k n", k=8) for i in range(2)]
    r_wr = [Res("wr0"), Res("wr1")]

    def x_src(seq, l):
        src = x_d[seq] if l == 0 else xs_d
        return src.rearrange("(b p) d -> p b d", p=128), ([] if l == 0 else [r_xs])

    def mixer(seq, l):
        xsrc, xsr = x_src(seq, l)
        load_mod(0, seq, l, 1)
        load_mod(1, seq, l, 0)
        for b in range(NB):
            j = b % 2
            dma("sp", LNX[j], xsrc[:, b, :], xsr, [r_lnx[j]])
            rstd, nmr, rs = ln_stats(LNX[j], r_lnx[j])
            act(LNX[j], LNX[j], AF.Identity, [rs, r_lnx[j]], [r_lnx[j]], bias=nmr, scale=rstd)
            tt("dve", LNX[j], LNX[j], MOD[0][:], ALU.mult, [r_lnx[j], r_mod[0]], [r_lnx[j]])
            tt("pool", LNH[j], LNX[j], MOD[1][:], ALU.add, [r_lnx[j], r_mod[1]], [r_lnh[j]])
            pb, pr = bank()
            pbb = pb[:].bitcast(BF16)
            for k in range(8):
                tr(pbb[:, k * 128:(k + 1) * 128], LNH[j][:, k * 128:(k + 1) * 128], IDB[:], [r_lnh[j], r_cst], [pr])
            cp("act", HT[:, :, b * 128:(b + 1) * 128], pbb.rearrange("p (k t) -> p k t", k=8), [pr], [r_ht[b]])

        chk("A")

        def load_piece(slot, c0, c1):
            return dma("pool", WR[slot][:, :, 0:c1 - c0], win_d[l, :, c0:c1].rearrange("(k p) n -> p k n", p=128), [], [r_wr[slot]])

        def proj_fm(slot, lhs_fn, evac):
            for tq in range(4):
                pb, pr = bank()
                for k in range(8):
                    mm(pb[:, :], lhs_fn(k), HT[:, k, tq * 512:(tq + 1) * 512], k == 0, k == 7,
                       [r_wr[slot]] + r_ht[tq * 4:(tq + 1) * 4], [pr])
                evac(tq, pb, pr)

        def proj_tm(slot, b, c0, n, pb, pr):
            for k in range(8):
                mm(pb[:, 0:n], HT[:, k, b * 128:(b + 1) * 128], WR[slot][:, k, c0:c0 + n], k == 0, k == 7, [r_wr[slot], r_ht[b]], [pr])

        AQT = XTB[:, 0:8192].rearrange("p (c t) -> p c t", c=4)
        AKT = XTB[:, 8192:10240]
        AV1 = XTB[:, 10240:12320].rearrange("p (b g e) -> p b g e", b=NB, g=2)
        BIASM = XTF[:, 6400:9472].rearrange("p (h s) -> p h s", h=8)
        r_aq = [Res("aq%d" % i) for i in range(4)]
        r_ak = [Res("ak%d" % i) for i in range(4)]
        r_av = [Res("av%d" % b) for b in range(NB)]
        r_bm = Res("biasm")
        dma("act", BIASM, btab_d, [], [r_bm])
        for h in range(8):
            tt("pool", BIASM[:, h, :], BIASM[:, h, :], mneg, ALU.add, [r_bm, r_cst], [r_bm])
        P.op("pool", lambda e: e.memset(AV1[:, :, :, 64:65], 1.0), [], r_av)
        for g_ in range(2):
            for c_ in range(4):
                dma("pool", WR[0][:, :, c_ * 128 + g_ * 64: c_ * 128 + (g_ + 1) * 64],
                    win_d[l, :, (g_ * 4 + c_) * 64:(g_ * 4 + c_ + 1) * 64].rearrange("(k p) e -> p k e", p=128), [], [r_wr[0]])
        load_piece(1, 512, 768)
        for c in range(4):
            def ev(tq, pb, pr, c=c):
                act(AQT[:, c, tq * 512:(tq + 1) * 512], pb[:, :], AF.Copy, [pr], [r_aq[tq]], scale=0.125)
            proj_fm(0, lambda k, c=c: WR[0][:, k, c * 128:(c + 1) * 128], ev)

        def evk(tq, pb, pr):
            cp("dve", AKT[:, tq * 512:(tq + 1) * 512], pb[:, :], [pr], [r_ak[tq]])
        proj_fm(1, lambda k: WR[1][:, k, 0:128], evk)
        for b in range(NB):
            pb, pr = bank()
            proj_tm(1, b, 128, 128, pb, pr)
            cp("dve", AV1[:, b, :, 0:64], pb[:, 0:128].rearrange("p (g e) -> p g e", g=2), [pr], [r_av[b]])
        S1 = [XTF[:, 9472 + i * 384: 9472 + (i + 1) * 384] for i in range(8)]
        AVEC = XTF[:, 12544:12672]
        PBF = [XTB[:, 25344 + i * 384: 25344 + (i + 1) * 384] for i in range(8)]
        PTB = [XTB[:, 28416 + i * 384: 28416 + (i + 1) * 384] for i in range(8)]
        r_s1 = [Res("s1_%d" % i) for i in range(8)]
        r_pb = [Res("pb_%d" % i) for i in range(8)]
        r_pt = [Res("pt_%d" % i) for i in range(8)]
        r_vec = [Res("vec0"), Res("vec1")]
        bi = 0
        for n in range(NB):
            lo, hi = max(n - 1, 0), min(n + 1, NB - 1)
            nblk = hi - lo + 1
            ncol = nblk * 128
            j0 = (lo - (n - 1)) * 128
            for g in range(2):
                st = bi % 2
                bi += 1
                V = AVEC[:, st * 64:(st + 1) * 64].rearrange("p (r c) -> p r c", c=4)
                rv = r_vec[st]
                snk = SNK[:, l, g * 4:(g + 1) * 4]
                pss = []
                for c in range(4):
                    ps, prs = bank()
                    pss.append((ps, prs))
                    mm(ps[:, 0:ncol], AQT[g * 64:(g + 1) * 64, c, n * 128:(n + 1) * 128], AKT[g * 64:(g + 1) * 64, lo * 128:(hi + 1) * 128],
                       True, True, [r_aq[n // 4]] + [r_ak[bb // 4] for bb in range(lo, hi + 1)], [prs])
                for c in range(4):
                    s_ = st * 4 + c
                    h = g * 4 + c
                    ps, prs = pss[c]
                    tt("dve", S1[s_][:, 0:ncol], ps[:, 0:ncol], BIASM[:, h, j0:j0 + ncol], ALU.add, [prs, r_bm], [r_s1[s_]])
                    P.op("dve", lambda e, s_=s_, ncol=ncol, V=V, c=c: e.tensor_reduce(out=V[:, 0, c:c + 1], in_=S1[s_][:, 0:ncol], axis=AX.X, op=ALU.max),
                         [r_s1[s_]], [rv])
                tt("dve", V[:, 1, :], V[:, 0, :], snk, ALU.max, [rv, r_small], [rv])
                ts("dve", V[:, 2, :], V[:, 1, :], -1.0, None, ALU.mult, None, [rv], [rv])
                tt("dve", V[:, 3, :], snk, V[:, 1, :], ALU.subtract, [rv, r_small], [rv])
                for c in range(4):
                    s_ = st * 4 + c
                    act(PBF[s_][:, 0:ncol], S1[s_][:, 0:ncol], AF.Exp, [r_s1[s_], rv], [r_pb[s_]], bias=V[:, 2, c:c + 1], scale=1.0)
                act(V[:, 4, :], V[:, 3, :], AF.Exp, [rv], [rv])
                for half in range(2):
                    pt, prt = bank()
                    ptb = pt[:].bitcast(BF16)
                    for cc in range(2):
                        s_ = st * 4 + half * 2 + cc
                        for jb in range(nblk):
                            tr(ptb[:, cc * 384 + jb * 128: cc * 384 + (jb + 1) * 128], PBF[s_][:, jb * 128:(jb + 1) * 128], IDB[:], [r_pb[s_], r_cst], [prt])
                    s0 = st * 4 + half * 2
                    for cc in range(2):
                        cp("act", PTB[s0 + cc][:, 0:ncol], ptb[:, cc * 384: cc * 384 + ncol], [prt], [r_pt[s0 + cc]])
                po, pro = bank()
                for c in range(4):
                    s_ = st * 4 + c
                    for jb in range(nblk):
                        mm(po[:, c * 65:(c + 1) * 65], PTB[s_][:, jb * 128:(jb + 1) * 128], AV1[:, lo + jb, g, :], jb == 0, jb == nblk - 1,
                           [r_pt[s_], r_av[lo + jb]], [pro])
                tt("dve", V[:, 5, :], po[:, 0:260].rearrange("p (c e) -> p c e", c=4)[:, :, 64], V[:, 4, :], ALU.add, [pro, rv], [rv])
                P.op("dve", lambda e, V=V: e.reciprocal(out=V[:, 6, :], in_=V[:, 5, :]), [rv], [rv])
                for c in range(4):
                    h = g * 4 + c
                    ts("dve", MIX[:, n, h * 64:(h + 1) * 64], po[:, c * 65: c * 65 + 64], V[:, 6, c:c + 1], None, ALU.mult, None, [pro, rv], [r_mix_a[n]])
        P.barrier()
        chk("att")

        PRE = XTB[:, 0:8200].rearrange("p (c t) -> p c t", c=4)
        CQ = XTB[:, 8200:16392].rearrange("p (c t) -> p c t", c=4)
        KTOK = XTB[:, 16392:20488].rearrange("p (b d) -> p b d", b=NB)
        MV1 = XTB[:, 20488:24648].rearrange("p (b h e) -> p b h e", b=NB, h=4)
        MO = XTB[:, 24648:28744].rearrange("p (b d) -> p b d", b=NB)
        fo = 14400

        def falloc(n):
            nonlocal fo
            a = XTF[:, fo:fo + n]
            fo += n
            assert fo <= 16384
            return a
        GT = falloc(256).rearrange("p (b g) -> p b g", b=NB)
        LF = falloc(128).rearrange("p (d b h) -> p d b h", d=2, b=NB)
        EA = falloc(128).rearrange("p (d b h) -> p d b h", d=2, b=NB)
        EB = falloc(128).rearrange("p (d b h) -> p d b h", d=2, b=NB)
        EBL = falloc(128).rearrange("p (d b h) -> p d b h", d=2, b=NB)
        TMPG = falloc(128).rearrange("p (d b h) -> p d b h", d=2, b=NB)
        C32 = falloc(8 * 65).rearrange("p (c e) -> p c e", c=8)
        CTMP = falloc(2 * 65).rearrange("p (c e) -> p c e", c=2)
        ND = [falloc(65) for _ in range(4)]
        MVEC = falloc(32)
        HTMP = [falloc(64) for _ in range(2)]
        bo = 8448

        def balloc(n):
            nonlocal bo
            a = WW[:, bo:bo + n]
            bo += n
            assert bo <= 14336
            return a
        CONVT = [balloc(1024).bitcast(F32) for _ in range(2)]
        STM = [balloc(128) for _ in range(4)]
        VS2 = [balloc(130) for _ in range(2)]
        CBF = balloc(8 * 66).rearrange("p (c e) -> p c e", c=8)[:, :, 0:65]
        r_pre = [Res("pre%d" % i) for i in range(4)]
        r_cq = [[Res("cq%d_%d" % (f, i)) for i in range(4)] for f in range(4)]
        r_ktok = [Res("ktok%d" % b) for b in range(NB)]
        r_mv = [Res("mv%d" % b) for b in range(NB)]
        r_mo = [Res("mo%d" % b) for b in range(NB)]
        r_gt = Res("gt")
        r_gates = Res("gates")
        r_convt = [Res("cva"), Res("cvb")]
        r_stm = [Res("stm%d" % i) for i in range(4)]
        r_vs = [Res("vs%d" % i) for i in range(4)]
        r_c = [Res("c%d" % i) for i in range(8)]
        r_ctmp = [Res("ctmp0"), Res("ctmp1")]
        r_nd = [Res("nd%d" % i) for i in range(4)]
        r_mvec = [Res("mvec%d" % i) for i in range(4)]
        r_htmp = [Res("htmp0"), Res("htmp1")]
        load_piece(0, 768, 1280)
        load_piece(1, 1280, 1808)
        P.op("pool", lambda e: e.memset(PRE[:, :, 0:1], 0.0), [], r_pre)
        P.op("pool", lambda e: e.memset(PRE[:, :, 2049:2050], 0.0), [], r_pre)
        P.op("pool", lambda e: e.memset(MV1[:, :, :, 64:65], 1.0), [], r_mv)
        P.op("pool", lambda e: e.memset(C32[:, :, :], 0.0), [], r_c)
        P.op("pool", lambda e: e.memset(CBF, 0.0), [], r_c)
        for fc in range(4):
            def ev(tq, pb, pr, fc=fc):
                cp("act", PRE[:, fc, 1 + tq * 512: 1 + (tq + 1) * 512], pb[:, :], [pr], [r_pre[tq]])
            proj_fm(0, lambda k, fc=fc: WR[0][:, k, fc * 128:(fc + 1) * 128], ev)
        ci = 0
        for fc in range(4):
            for tq in range(4):
                t0 = tq * 512
                cv = CONVT[ci % 2]
                rc = r_convt[ci % 2]
                ci += 1
                rd = [r_pre[i] for i in range(max(tq - 1, 0), min(tq + 1, 3) + 1)] + [r_small]
                ts("dve", cv, PRE[:, fc, t0:t0 + 512], CWA[:, l, fc, 0:1], None, ALU.mult, None, rd, [rc])
                stt(cv, PRE[:, fc, t0 + 1:t0 + 513], CWA[:, l, fc, 1:2], cv, ALU.mult, ALU.add, rd + [rc], [rc])
                stt(cv, PRE[:, fc, t0 + 2:t0 + 514], CWA[:, l, fc, 2:3], cv, ALU.mult, ALU.add, rd + [rc], [rc])
                act(CQ[:, fc, t0:t0 + 512], cv, AF.Silu, [rc, r_small], [r_cq[fc][tq]], bias=CBA[:, l, fc:fc + 1], scale=1.0)
                if fc >= 2:
                    ts("dve", CQ[:, fc, t0:t0 + 512], CQ[:, fc, t0:t0 + 512], 0.125, None, ALU.mult, None, [r_cq[fc][tq]], [r_cq[fc][tq]])
        chk("m1")
        for b in range(NB):
            pb, pr = bank()
            pbb = pb[:].bitcast(BF16)
            for kc in range(2):
                tr(pbb[:, kc * 128:(kc + 1) * 128], CQ[:, 2 + kc, b * 128:(b + 1) * 128], IDB[:], [r_cq[2 + kc][b // 4], r_cst], [pr])
            cp("dve", KTOK[:, b, :], pbb[:, 0:256], [pr], [r_ktok[b]])
        chk("m1b")
        for b in range(NB):
            pb, pr = bank()
            proj_tm(1, b, 0, 512, pb, pr)
            pb2, pr2 = bank()
            proj_tm(1, b, 496, 32, pb2, pr2)
            cp("dve", MV1[:, b, :, 0:64], pb[:, 0:256].rearrange("p (h e) -> p h e", h=4), [pr], [r_mv[b]])
            sgt_ = CONVT[b % 2][:, 0:256]
            act(sgt_, pb[:, 256:512], AF.Exp, [pr], [r_convt[b % 2]], scale=-1.0)
            ts("dve", sgt_, sgt_, 1.0, None, ALU.add, None, [r_convt[b % 2]], [r_convt[b % 2]])
            P.op("dve", lambda e, sgt_=sgt_: e.reciprocal(out=sgt_, in_=sgt_), [r_convt[b % 2]], [r_convt[b % 2]])
            cp("dve", MO[:, b, :], sgt_, [r_convt[b % 2]], [r_mo[b]])
            tt("dve", GT[:, b, :], pb2[:, 16:32], GBB[:, l, :], ALU.add, [pr2, r_small], [r_gt])
        chk("m2")
        for d in range(2):
            act(TMPG[:, d], GT[:, :, (2 * d + 1) * 4:(2 * d + 2) * 4], AF.Exp, [r_gt], [r_gates], scale=-1.0)
            act(LF[:, d], TMPG[:, d], AF.Ln, [r_gates], [r_gates], bias=1.0, scale=1.0)
        for d in range(2):
            pb, pr = bank()
            mm(pb[:, 0:64], tri_f if d == 0 else tri_b, LF[:, d].rearrange("p b h -> p (b h)"), True, True, [r_gates, r_cst], [pr])
            pb2, pr2 = bank()
            mm(pb2[:, 0:64], ones_f, LF[:, d].rearrange("p b h -> p (b h)"), True, True, [r_gates, r_cst], [pr2])
            tt("dve", TMPG[:, d], pb[:, 0:64].rearrange("p (b h) -> p b h", b=NB), GT[:, :, (2 * d) * 4:(2 * d + 1) * 4], ALU.add, [pr, r_gt], [r_gates])
            act(EA[:, d], TMPG[:, d], AF.Exp, [r_gates], [r_gates])
            act(EB[:, d], pb[:, 0:64].rearrange("p (b h) -> p b h", b=NB), AF.Exp, [pr], [r_gates], scale=-1.0)
            act(EBL[:, d], pb2[:, 0:64].rearrange("p (b h) -> p b h", b=NB), AF.Exp, [pr2], [r_gates], scale=-1.0)
        chk("m3")
        uu = 0
        for ci_ in range(NB):
            for d in range(2):
                c = ci_ if d == 0 else NB - 1 - ci_
                msk = tri_f if d == 0 else tri_b
                info = []
                for h in range(4):
                    kc, pbs = h // 2, (h % 2) * 64
                    QT = CQ[pbs:pbs + 64, kc, c * 128:(c + 1) * 128]
                    KT = CQ[pbs:pbs + 64, 2 + kc, c * 128:(c + 1) * 128]
                    rq = [r_cq[kc][c // 4], r_cq[2 + kc][c // 4]]
                    ps, prs = bank()
                    mm(ps[:, 0:128], KT, QT, True, True, rq, [prs])
                    info.append((kc, rq, ps, prs))
                for h in range(4):
                    kc, rq, ps, prs = info[h]
                    stt(STM[h], ps[:, 0:128], EA[:, d, c, h:h + 1], msk, ALU.mult, ALU.mult, [prs, r_gates, r_cst], [r_stm[h]])
                pns = []
                for h in range(4):
                    kc, rq, ps, prs = info[h]
                    ch = d * 4 + h
                    pn, prn = bank()
                    pns.append((pn, prn))
                    mm(pn[:, 0:65], STM[h], MV1[:, c, h, :], True, False, [r_stm[h], r_mv[c]], [prn])
                    mm(pn[:, 0:65], CQ[:, kc, c * 128:(c + 1) * 128], CBF[:, ch, :], False, True, rq + [r_c[ch]], [prn])
                for h in range(4):
                    pn, prn = pns[h]
                    ts("dve", ND[h], pn[:, 0:65], EB[:, d, c, h:h + 1], None, ALU.mult, None, [prn, r_gates], [r_nd[h]])
                for h in range(4):
                    mv = MVEC[:, h * 8:(h + 1) * 8]
                    ts("dve", mv[:, 0:1], ND[h][:, 64:65], -1.0, None, ALU.mult, None, [r_nd[h]], [r_mvec[h]])
                for h in range(4):
                    mv = MVEC[:, h * 8:(h + 1) * 8]
                    ts("dve", mv[:, 0:1], mv[:, 0:1], ND[h][:, 64:65], 1.0, ALU.max, ALU.max, [r_nd[h], r_mvec[h]], [r_mvec[h]])
                for h in range(4):
                    mv = MVEC[:, h * 8:(h + 1) * 8]
                    P.op("dve", lambda e, mv=mv: e.reciprocal(out=mv[:, 1:2], in_=mv[:, 0:1]), [r_mvec[h]], [r_mvec[h]])
                for h in range(4):
                    mv = MVEC[:, h * 8:(h + 1) * 8]
                    i2 = h % 2
                    dst = MIX[:, c, 512 + h * 64: 512 + (h + 1) * 64]
                    if (d == 0) == (c <= 7):
                        ts("dve", dst, ND[h][:, 0:64], mv[:, 1:2], None, ALU.mult, None, [r_nd[h], r_mvec[h]], [r_mix_m[c]])
                    else:
                        stt(HTMP[i2], ND[h][:, 0:64], mv[:, 1:2], dst, ALU.mult, ALU.add, [r_nd[h], r_mvec[h], r_mix_m[c]], [r_htmp[i2]])
                        tt("dve", dst, HTMP[i2], MO[:, c, h * 64:(h + 1) * 64], ALU.mult, [r_htmp[i2], r_mo[c]], [r_mix_m[c]])
                if ci_ < NB - 1:
                    for kc in range(2):
                        i2 = uu % 2
                        uu += 1
                        for hh in range(2):
                            h = kc * 2 + hh
                            ts("dve", VS2[i2][:, hh * 65:(hh + 1) * 65], MV1[:, c, h, :], EA[:, d, c, h:h + 1], None, ALU.mult, None, [r_mv[c], r_gates], [r_vs[i2]])
                        pu, pru = bank()
                        mm(pu[:, 0:130], KTOK[:, c, kc * 128:(kc + 1) * 128], VS2[i2], True, True, [r_ktok[c], r_vs[i2]], [pru])
                        for hh in range(2):
                            h = kc * 2 + hh
                            ch = d * 4 + h
                            pbs = hh * 64
                            tt("dve", CTMP[pbs:pbs + 64, i2, :], C32[pbs:pbs + 64, ch, :], pu[pbs:pbs + 64, hh * 65:(hh + 1) * 65], ALU.add, [r_c[ch], pru], [r_ctmp[i2]])
                            ts("dve", C32[pbs:pbs + 64, ch, :], CTMP[pbs:pbs + 64, i2, :], EBL[pbs:pbs + 64, d, c, h:h + 1], None, ALU.mult, None,
                               [r_ctmp[i2], r_gates], [r_c[ch]])
                            cp("act", CBF[pbs:pbs + 64, ch, :], C32[pbs:pbs + 64, ch, :], [r_c[ch]], [r_c[ch]])
        P.barrier()
        chk("mls")

        WSF = XTF[:, 0:512].rearrange("p (g s) -> p g s", g=4)
        WSB = XTB[:, 1024:1536].rearrange("p (g s) -> p g s", g=4)
        WST = XTB[:, 1536:2048].rearrange("p (g s) -> p g s", g=4)
        GV = [XTF[:, 1024 + i * 256: 1024 + (i + 1) * 256] for i in range(2)]
        VB = [XTB[:, 3072 + i * 256: 3072 + (i + 1) * 256] for i in range(2)]
        r_ws = Res("ws")
        r_gv = [Res("gv0"), Res("gv1")]
        r_vb = [Res("vb0"), Res("vb1")]
        load_piece(0, 1808, 2320)
        dma("act", WSF, ws_d[l].rearrange("g t s -> t g s"), [], [r_ws])
        cp("dve", WSB, WSF, [r_ws], [r_ws])
        pb, pr = bank()
        pbb = pb[:].bitcast(BF16)
        for g in range(4):
            tr(pbb[:, g * 128:(g + 1) * 128], WSB[:, g, :], IDB[:], [r_ws, r_cst], [pr])
        cp("dve", WST.rearrange("p g s -> p (g s)"), pbb[:, 0:512], [pr], [r_ws])
        for b in range(NB):
            j = b % 2
            pb, pr = bank()
            proj_tm(0, b, 0, 512, pb, pr)
            act(MIX[:, b, 768:1024], pb[:, 0:256], AF.Gelu, [pr], [r_mix_g[b]])
            act(GV[j], pb[:, 256:512], AF.Gelu, [pr], [r_gv[j]])
            rstd, nmr, rs = ln_stats(GV[j], r_gv[j], width=256)
            act(VB[j], GV[j], AF.Identity, [rs, r_gv[j]], [r_vb[j]], bias=nmr, scale=rstd)
            pg, prg = bank()
            for g in range(4):
                mm(pg[:, g * 64:(g + 1) * 64], WST[:, g, :], VB[j][:, g * 64:(g + 1) * 64], True, True, [r_ws, r_vb[j]], [prg])
            for g in range(4):
                dst = MIX[:, b, 768 + g * 64: 768 + (g + 1) * 64]
                stt(dst, pg[:, g * 64:(g + 1) * 64], BST[:, l, g:g + 1], dst, ALU.add, ALU.mult, [prg, r_small, r_mix_g[b]], [r_mix_g[b]])
        P.barrier()
        chk("gml")

        WO = WW[:, 0:8192].rearrange("p (k n) -> p k n", k=8)
        r_wo = Res("wo")
        MTB = [AA[:, i * 1024:(i + 1) * 1024].rearrange("p (k t) -> p k t", k=8) for i in range(2)]
        TMPF = [AA[:, 2048 + i * 1024: 2048 + (i + 1) * 1024].bitcast(F32) for i in range(2)]
        r_mtb = [Res("mtb0"), Res("mtb1")]
        r_tmpf = [Res("tf0"), Res("tf1")]
        dma("pool", WO, wout_d[l].rearrange("(k p) n -> p k n", p=128), [], [r_wo])
        load_mod(0, seq, l, 2)
        load_row(1, lng_d[l, 0:1, :])
        load_row(2, lnb_d[l, 0:1, :])
        for b in range(NB):
            j = b % 2
            dma("sp", XT[:, b, :], xsrc[:, b, :], xsr, [r_x[b]])
            pb, pr = bank()
            pbb = pb[:].bitcast(BF16)
            for k in range(8):
                tr(pbb[:, k * 128:(k + 1) * 128], MIX[:, b, k * 128:(k + 1) * 128], IDB[:], [r_mix_a[b], r_mix_m[b], r_mix_g[b], r_cst], [pr])
            cp("act", MTB[j].rearrange("p k t -> p (k t)"), pbb, [pr], [r_mtb[j]])
            for dh in range(2):
                po, pro = bank()
                for k in range(8):
                    mm(po[:, :], MTB[j][:, k, :], WO[:, k, dh * 512:(dh + 1) * 512], k == 0, k == 7, [r_mtb[j], r_wo], [pro])
                tt("dve", TMPF[dh], po[:, :], MOD[0][:, dh * 512:(dh + 1) * 512], ALU.mult, [pro, r_mod[0]], [r_tmpf[dh]])
                stt(XT[:, b, dh * 512:(dh + 1) * 512], XT[:, b, dh * 512:(dh + 1) * 512], ALPHA, TMPF[dh], ALU.mult, ALU.add,
                    [r_tmpf[dh], r_x[b]], [r_x[b]])
        ln_stats_batch()
        for b in range(NB):
            act(XT[:, b, :], XT[:, b, :], AF.Identity, [r_stb, r_x[b]], [r_x[b]], bias=NMRB[:, b:b + 1], scale=RSTDB[:, b:b + 1])
            tt("dve", XT[:, b, :], XT[:, b, :], MOD[1][:], ALU.mult, [r_x[b], r_mod[1]], [r_x[b]])
            tt("pool", XT[:, b, :], XT[:, b, :], MOD[2][:], ALU.add, [r_x[b], r_mod[2]], [r_x[b]])
        P.barrier()

    def moe(seq, l, last):
        load_mod(0, seq, l, 4)
        load_mod(1, seq, l, 3)
        r_wrt = Res("wrt")
        dma("act", WRT[:], wr_d[l].rearrange("(k p) e -> p k e", p=128), [], [r_wrt])
        r_h2b = [Res("h2b%d" % b) for b in range(NB)]
        r_aff = Res("aff")
        H2T = WW[:, 0:2048].bitcast(F32).rearrange("p (k t) -> p k t", k=8)
        r_h2t = Res("h2t")
        LVEC = SM[:, 200:264]
        r_lvec = [Res("lvec%d" % i) for i in range(4)]
        LG = [SM[:, 264 + i * 16: 264 + (i + 1) * 16] for i in range(4)]
        r_lg = [Res("lg%d" % i) for i in range(4)]
        ln_stats_batch()
        for b in range(NB):
            j = b % 2
            i4 = b % 4
            act(LNX[j], XT[:, b, :], AF.Identity, [r_stb, r_x[b]], [r_lnx[j]], bias=NMRB[:, b:b + 1], scale=RSTDB[:, b:b + 1])
            tt("dve", LNX[j], LNX[j], MOD[0][:], ALU.mult, [r_lnx[j], r_mod[0]], [r_lnx[j]])
            tt("pool", LNX[j], LNX[j], MOD[1][:], ALU.add, [r_lnx[j], r_mod[1]], [r_lnx[j]])
            cp("act", H2B[:, b, :], LNX[j], [r_lnx[j]], [r_h2b[b]])
            act(XT[:, b, :], XT[:, b, :], AF.Copy, [r_x[b]], [r_x[b]], scale=ALPHA)
            for half in range(2):
                pb, pr = bank()
                for kk in range(4):
                    k = half * 4 + kk
                    tr(pb[:, kk * 128:(kk + 1) * 128], LNX[j][:, k * 128:(k + 1) * 128], ident_f, [r_lnx[j], r_cst], [pr])
                cp("dve", H2T[:, half * 4:(half + 1) * 4, :].rearrange("p k t -> p (k t)"), pb[:, :], [pr], [r_h2t])
            pl, prl = bank()
            for k in range(8):
                mm(pl[:, 0:16], H2T[:, k, :], WRT[:, k, :], k == 0, k == 7, [r_h2t, r_wrt], [prl])
            vec = LVEC[:, i4 * 8:(i4 + 1) * 8]
            P.op("dve", lambda e, vec=vec, pl=pl: e.tensor_reduce(out=vec[:, 0:1], in_=pl[:, 0:16], axis=AX.X, op=ALU.max, negate=True), [prl], [r_lvec[i4]])
            act(LG[i4], pl[:, 0:16], AF.Exp, [prl, r_lvec[i4]], [r_lg[i4]], bias=vec[:, 0:1], scale=1.0)
            P.op("dve", lambda e, vec=vec, i4=i4: e.tensor_reduce(out=vec[:, 1:2], in_=LG[i4], axis=AX.X, op=ALU.add), [r_lg[i4]], [r_lvec[i4]])
            P.op("dve", lambda e, vec=vec: e.reciprocal(out=vec[:, 2:3], in_=vec[:, 1:2]), [r_lvec[i4]], [r_lvec[i4]])
            ts("dve", AFF[:, b, :], LG[i4], vec[:, 2:3], None, ALU.mult, None, [r_lg[i4], r_lvec[i4]], [r_aff])
        chk("E")
        r_aft = Res("aft")
        for q4 in range(4):
            pb, pr = bank()
            for bb in range(4):
                b = q4 * 4 + bb
                tr(pb[0:16, bb * 128:(bb + 1) * 128], AFF[:, b, :], ident_f, [r_aff, r_cst], [pr])
            cp("dve", AFT[:, q4 * 512:(q4 + 1) * 512], pb[0:16, :], [pr], [r_aft])
        M8 = SM[0:16, 400:408]
        r_m8 = Res("m8")
        WORK = WW[0:16, 4096:8192].bitcast(F32)
        r_work = Res("work")
        for r in range(32):
            src = AFT[:, :] if r == 0 else WORK
            P.op("dve", lambda e, src=src: e.max(out=M8, in_=src), [r_aft, r_work], [r_m8])
            if r < 31:
                P.op("dve", lambda e, src=src: e.match_replace(out=WORK, in_to_replace=M8, in_values=src, imm_value=-1.0), [r_aft, r_m8, r_work], [r_work])
        ts("dve", WORK, AFT[:, :], M8[:, 7:8], None, ALU.is_ge, None, [r_aft, r_m8, r_work], [r_work])
        r_sel_meta = Res("selmeta")
        for q4 in range(4):
            pb, pr = bank()
            for bb in range(4):
                b = q4 * 4 + bb
                tr(pb[:, bb * 16:(bb + 1) * 16], WORK[:, b * 128:(b + 1) * 128], ident_f[0:16, 0:16], [r_work, r_cst], [pr])
            cp("dve", MSK[:, q4 * 4:(q4 + 1) * 4, :].rearrange("p b e -> p (b e)"), pb[:, 0:64], [pr], [r_sel_meta])
        tt("dve", GTM[:].rearrange("p b e -> p (b e)"), MSK[:].rearrange("p b e -> p (b e)"), AFF[:].rearrange("p b e -> p (b e)"), ALU.mult,
           [r_sel_meta, r_aff], [r_sel_meta])
        pp, prp = bank()
        mm(pp[:, 0:256], tri_s, MSK[:].rearrange("p b e -> p (b e)"), True, True, [r_sel_meta, r_cst], [prp])
        pq, prq = bank()
        mm(pq[:, 0:256], ones_f, MSK[:].rearrange("p b e -> p (b e)"), True, True, [r_sel_meta, r_cst], [prq])
        cp("dve", TOT[:].rearrange("p b e -> p (b e)"), pq[:, 0:256], [prq], [r_sel_meta])
        CAR = SLT
        P.op("dve", lambda e: e.memset(CAR[:, 0, :], 0.0), [], [r_sel_meta])
        for b in range(1, NB):
            tt("dve", CAR[:, b, :], CAR[:, b - 1, :], TOT[:, b - 1, :], ALU.add, [r_sel_meta], [r_sel_meta])
        tt("dve", SLT[:].rearrange("p b e -> p (b e)"), SLT[:].rearrange("p b e -> p (b e)"), pp[:, 0:256], ALU.add, [r_sel_meta, prp], [r_sel_meta])
        P.barrier()
        chk("F")
        load_mod(0, seq, l, 5)
        SEL = BB[:, 0:4096].rearrange("p (b j) -> p b j", b=NB)
        SELT = BB[:, 4096:8192].rearrange("p (c t) -> p c t", c=2)
        XGT = BB[:, 8192:10240].rearrange("p (k j) -> p k j", k=8)
        HID = BB[:, 10240:14336].rearrange("p (f j) -> p f j", f=16)
        YE = BB[:, 14336:16384].rearrange("p (c d) -> p c d", c=2)
        WSL = [WW[:, i * 4096:(i + 1) * 4096] for i in range(4)]
        SGT = [WW[:, 16384 + i * 512: 16384 + (i + 1) * 512].bitcast(F32) for i in range(2)]
        r_wsl = [Res("wsl%d" % i) for i in range(4)]
        r_sgt = [Res("sgt0"), Res("sgt1")]
        r_sel = Res("sel")
        r_selt = Res("selt")
        r_xgt = Res("xgt")
        r_hid = [Res("hid%d" % i) for i in range(16)]
        r_ye = Res("ye")
        wsi = {"i": 0}

        def wslice(src_ap, pat, **kw):
            i = wsi["i"] % 4
            wsi["i"] += 1
            v = WSL[i].rearrange(pat, **kw)
            dma("pool", v, src_ap, [], [r_wsl[i]])
            return v, r_wsl[i]

        r_selb = [Res("selb%d" % b) for b in range(NB)]
        r_xg = [Res("xg%d" % i) for i in range(4)]
        r_st4 = [Res("selt%d" % i) for i in range(4)]
        r_ye4 = [Res("ye%d" % i) for i in range(4)]

        def sel_build(e_):
            for b in range(NB):
                ts("dve", SEL[:, b, :], iota_f, SLT[:, b, e_:e_ + 1], MSK[:, b, e_:e_ + 1],
                   ALU.is_equal, ALU.mult, [r_sel_meta, r_cst], [r_selb[b]])

        def gather(e_):
            for kp in range(4):
                pb, pr = bank()
                for kk in range(2):
                    k = kp * 2 + kk
                    for b in range(NB):
                        mm(pb[:, kk * 256:(kk + 1) * 256], H2B[:, b, k * 128:(k + 1) * 128], SEL[:, b, :], b == 0, b == NB - 1, [r_h2b[b], r_selb[b]], [pr])
                cp("act" if kp % 2 else "dve", XGT[:, kp * 2:(kp + 1) * 2, :].rearrange("p k j -> p (k j)"), pb[:, :], [pr], [r_xg[kp]])

        def sel_t(e_):
            for c in range(2):
                for half in range(2):
                    pb, pr = bank()
                    pbb = pb[:].bitcast(BF16)
                    for bb in range(8):
                        b = half * 8 + bb
                        tr(pbb[:, bb * 128:(bb + 1) * 128], SEL[:, b, c * 128:(c + 1) * 128], IDB[:], [r_selb[b], r_cst], [pr])
                    cp("act", SELT[:, c, half * 1024:(half + 1) * 1024], pbb, [pr], [r_st4[c * 2 + half]])

        def hid_ye(e_):
            for js in range(4):
                wgv, rwg = wslice(wg_d[l, e_, :, js * 512:(js + 1) * 512].rearrange("(k p) n -> p k n", p=128), "p (k n) -> p k n", k=8)
                wuv, rwu = wslice(wu_d[l, e_, :, js * 512:(js + 1) * 512].rearrange("(k p) n -> p k n", p=128), "p (k n) -> p k n", k=8)
                for fl in range(4):
                    fc = js * 4 + fl
                    pb, pr = bank()
                    for k in range(8):
                        mm(pb[:, 0:256], wgv[:, k, fl * 128:(fl + 1) * 128], XGT[:, k, :], k == 0, k == 7, [rwg, r_xg[k // 2]], [pr])
                    for k in range(8):
                        mm(pb[:, 256:512], wuv[:, k, fl * 128:(fl + 1) * 128], XGT[:, k, :], k == 0, k == 7, [rwu, r_xg[k // 2]], [pr])
                    j2 = fc % 2
                    act(SGT[j2], pb[:, 0:256], AF.Silu, [pr], [r_sgt[j2]])
                    tt("dve", HID[:, fc, :], SGT[j2], pb[:, 256:512], ALU.mult, [r_sgt[j2], pr], [r_hid[fc]])
            yb = [bank() for _ in range(4)]
            for js in range(4):
                wdv, rwd = wslice(wd_d[l, e_, js * 512:(js + 1) * 512, :].rearrange("(f p) n -> p f n", p=128), "p (f n) -> p f n", f=4)
                for fl in range(4):
                    fc = js * 4 + fl
                    for c in range(2):
                        for dh in range(2):
                            pb, pr = yb[c * 2 + dh]
                            mm(pb[:, :], HID[:, fc, c * 128:(c + 1) * 128], wdv[:, fl, dh * 512:(dh + 1) * 512], fc == 0, fc == 15, [r_hid[fc], rwd], [pr])
            for c in range(2):
                for dh in range(2):
                    pb, pr = yb[c * 2 + dh]
                    tt("dve", YE[:, c, dh * 512:(dh + 1) * 512], pb[:, :], MOD[0][:, dh * 512:(dh + 1) * 512], ALU.mult, [pr, r_mod[0]], [r_ye4[c * 2 + dh]])

        def scatter(e_):
            for b in range(NB):
                for dh in range(2):
                    pb, pr = bank()
                    for c in range(2):
                        mm(pb[:, :], SELT[:, c, b * 128:(b + 1) * 128], YE[:, c, dh * 512:(dh + 1) * 512], c == 0, c == 1,
                           [r_st4[c * 2 + b // 8], r_ye4[c * 2 + dh]], [pr])
                    stt(XT[:, b, dh * 512:(dh + 1) * 512], pb[:, :], GTM[:, b, e_:e_ + 1], XT[:, b, dh * 512:(dh + 1) * 512], ALU.mult, ALU.add,
                        [pr, r_sel_meta, r_x[b]], [r_x[b]])

        sel_build(0)
        gather(0)
        sel_t(0)
        for e_ in range(16):
            if e_ + 1 < 16:
                sel_build(e_ + 1)
            hid_ye(e_)
            if e_ + 1 < 16:
                gather(e_ + 1)
            scatter(e_)
            if e_ + 1 < 16:
                sel_t(e_ + 1)
        load_row(1, lng_d[l, 1:2, :])
        load_row(2, lnb_d[l, 1:2, :])
        outs = []
        dst = (out_d[seq] if last else xs_d).rearrange("(b p) d -> p b d", p=128)
        ln_stats_batch()
        for b in range(NB):
            act(XT[:, b, :], XT[:, b, :], AF.Identity, [r_stb, r_x[b]], [r_x[b]], bias=NMRB[:, b:b + 1], scale=RSTDB[:, b:b + 1])
            tt("dve", XT[:, b, :], XT[:, b, :], MOD[1][:], ALU.mult, [r_x[b], r_mod[1]], [r_x[b]])
            tt("pool", XT[:, b, :], XT[:, b, :], MOD[2][:], ALU.add, [r_x[b], r_mod[2]], [r_x[b]])
            outs.append(dma("sp", dst[:, b, :], XT[:, b, :], [r_x[b]], [] if last else [r_xs]))
        P.barrier()
        return outs

    final = []
    try:
        chk("pro")
        for seq in range(n_seq):
            for l in range(n_layers):
                mixer(seq, l)
                if "x1" in dbg_d and seq == 0 and l == 0:
                    for b in range(NB):
                        final.append(dma("sp", dbg_d["x1"].rearrange("(b p) d -> p b d", p=128)[:, b, :], XT[:, b, :], [r_x[b]], []))
                    P.barrier()
                chk("D")
                final += moe(seq, l, l == n_layers - 1)
    except Stop:
        P.barrier()
    P.op("sp", None, extra_deps=final)
    P.emit()
    es.close()
    return nc


_CACHE = {}


def host_inputs(inp, core):
    f = lambda a: np.ascontiguousarray(np.asarray(a, dtype=np.float32))
    sl = slice(2 * core, 2 * core + 2)
    q = np.arange(128)[:, None]
    sj = np.arange(384)[None, :]
    bk = t5_buckets(sj - 128 - q)
    btab = np.asarray(inp["rel_bias"], np.float32)[bk]
    m = {
        "x": f(inp["x"][sl]),
        "cT": f(np.asarray(inp["c"])[sl].T.reshape(8, 128, 2).transpose(1, 0, 2)),
        "w_ada": f(inp["w_ada"]), "b_ada": f(inp["b_ada"]), "w_in": f(inp["w_in"]),
        "conv_wT": f(np.asarray(inp["conv_w"]).transpose(0, 2, 1)), "conv_b": f(np.asarray(inp["conv_b"]).reshape(DEPTH, 4, 128).transpose(2, 0, 1)),
        "gate_b": f(inp["gate_b"]), "sink": f(inp["sink"]),
        "bias_tab": f(btab.transpose(0, 2, 1)),
        "w_s": f(inp["w_s"]), "b_sT": f(np.asarray(inp["b_s"]).transpose(0, 2, 1)),
        "w_out": f(inp["w_out"]), "w_router": f(inp["w_router"]),
        "w_gate": f(inp["w_gate"]), "w_up": f(inp["w_up"]), "w_down": f(inp["w_down"]),
        "ln_g": f(inp["ln_g"]), "ln_b": f(inp["ln_b"]),
        "cst": const_pack(),
    }
    return m


def kernel(**inputs):
    if "nc" not in _CACHE:
        _CACHE["nc"] = build()
    nc = _CACHE["nc"]
    shared = host_inputs(inputs, 0)
    in_maps = []
    for core in range(8):
        m = dict(shared)
        sl = slice(2 * core, 2 * core + 2)
        m["x"] = np.ascontiguousarray(np.asarray(inputs["x"], np.float32)[sl])
        m["cT"] = np.ascontiguousarray(np.asarray(inputs["c"], np.float32)[sl].T.reshape(8, 128, 2).transpose(1, 0, 2))
        in_maps.append(m)
    res = run_bass_kernel_spmd(nc, in_maps, core_ids=list(range(8)))
    return np.concatenate([r["out"] for r in res.results], axis=0).astype(np.float32)
```
